# Optimizing a Trainium2 kernel written in Bass

```python
import jax, jax.numpy as jnp
from jax import lax
import numpy as np


D_MODEL = 1024
BATCH = 8
SEQ = 8192
DEPTH = 2

PLE_DIM = 256
D_FF = 2816
EPS = 1e-6
ROPE_THETA = 10000.0
RET_THETA = 10000.0
N_NORMS = 7

A_HEADS = 4
A_DK = 128
A_DV = 128
A_CHUNK = 64
B_Q_HEADS = 8
B_KV_HEADS = 2
B_HEAD_DIM = 64
B_WINDOW = 128
B_BLOCK = 128
C_HEADS = 4
C_DK = 256
C_DV = 512
C_CHUNK = 128

N_EVEN = (DEPTH + 1) // 2
N_ODD = DEPTH // 2

A_KW = A_HEADS * A_DK
A_VW = A_HEADS * A_DV
B_QW = B_Q_HEADS * B_HEAD_DIM
B_KW = B_KV_HEADS * B_HEAD_DIM
EVEN_SPLITS = [A_KW, 2 * A_KW, 2 * A_KW + A_VW, 2 * A_KW + 2 * A_VW, 2 * A_KW + 2 * A_VW + B_QW,
               2 * A_KW + 2 * A_VW + B_QW + B_KW]
EVEN_IN = 2 * A_KW + 2 * A_VW + B_QW + 2 * B_KW
EVEN_OUT = A_VW + B_QW
C_KW = C_HEADS * C_DK
C_VW = C_HEADS * C_DV
ODD_SPLITS = [C_KW, 2 * C_KW, 2 * C_KW + C_VW]
ODD_IN = 2 * C_KW + 2 * C_VW
ODD_OUT = C_VW

kernel_name = 'hybrid_hgrn2_swa_sink_retention_macaron'


def rmsnorm(x, g):
    xf = x.astype(jnp.float32)
    y = xf * lax.rsqrt(jnp.mean(xf * xf, axis=-1, keepdims=True) + EPS)
    return (y * g.astype(jnp.float32)).astype(x.dtype)


def swiglu(x, w_in, w_out):
    gate, up = jnp.split(x @ w_in, 2, axis=-1)
    return (jax.nn.silu(gate) * up) @ w_out


def rope_tables(positions, inv_freq):
    ang = positions.astype(jnp.float32)[:, :, None] * inv_freq[None, None, :]
    return jnp.cos(ang)[:, :, None, :], jnp.sin(ang)[:, :, None, :]


def apply_rope(x, cos, sin):
    xf = x.astype(jnp.float32)
    x1, x2 = jnp.split(xf, 2, axis=-1)
    return jnp.concatenate([x1 * cos - x2 * sin, x2 * cos + x1 * sin], axis=-1).astype(x.dtype)


def hgrn2_chunkwise(q, f_logit, i, lb):
    B, S, H, K = q.shape
    V = i.shape[-1]
    C = A_CHUNK
    n = S // C

    def chunks(t):
        return t.astype(jnp.float32).reshape(B, n, C, H, -1).transpose(0, 3, 1, 2, 4)

    lb5 = lb[None, :, None, None, :]
    qf = jax.nn.silu(chunks(q))
    f = lb5 + (1.0 - lb5) * jax.nn.sigmoid(chunks(f_logit))
    k = 1.0 - f
    v = chunks(i)
    b = jnp.cumsum(jnp.log(f), axis=3)
    b_last = b[:, :, :, -1:, :]
    q_s = qf * jnp.exp(b)
    k_s = k * jnp.exp(-b)
    k_d = k * jnp.exp(b_last - b)
    causal = jnp.tril(jnp.ones((C, C), dtype=bool))
    att = jnp.where(causal, jnp.einsum('bhnck,bhnsk->bhncs', q_s, k_s), 0.0)
    intra = jnp.einsum('bhncs,bhnsv->bhncv', att, v)
    dec = jnp.exp(b_last[:, :, :, 0, :])

    def step(state, xs):
        qc, kc, vc, dc = xs
        o = jnp.einsum('bhck,bhkv->bhcv', qc, state)
        state = state * dc[..., None] + jnp.einsum('bhck,bhcv->bhkv', kc, vc)
        return state, o

    xs = (jnp.moveaxis(q_s, 2, 0), jnp.moveaxis(k_d, 2, 0), jnp.moveaxis(v, 2, 0), jnp.moveaxis(dec, 2, 0))
    _, inter = lax.scan(step, jnp.zeros((B, H, K, V), jnp.float32), xs)
    o = intra + jnp.moveaxis(inter, 0, 2)
    return o.transpose(0, 2, 3, 1, 4).reshape(B, S, H, V)


def swa_with_sinks(q, k, v, sinks):
    B, S, Hq, D = q.shape
    G = k.shape[2]
    R = Hq // G
    L = B_BLOCK
    nb = S // L
    qb = q.reshape(B, nb, L, G, R, D)
    kb = k.reshape(B, nb, L, G, D)
    vb = v.reshape(B, nb, L, G, D)
    pad = ((0, 0), (1, 0), (0, 0), (0, 0), (0, 0))
    kk = jnp.concatenate([jnp.pad(kb[:, :-1], pad), kb], axis=2)
    vv = jnp.concatenate([jnp.pad(vb[:, :-1], pad), vb], axis=2)
    s = jnp.einsum('bnqgrd,bnkgd->bngrqk', qb, kk).astype(jnp.float32) * (D ** -0.5)
    qi = jnp.arange(L)[:, None]
    kj = jnp.arange(2 * L)[None, :]
    diff = qi + L - kj
    band = (diff >= 0) & (diff < B_WINDOW)
    key_pos = jnp.arange(nb)[:, None, None] * L - L + kj[None]
    valid = band[None] & (key_pos >= 0)
    s = jnp.where(valid[None, :, None, None], s, -jnp.inf)
    sink = sinks.astype(jnp.float32).reshape(G, R)[None, None, :, :, None, None]
    m = jnp.maximum(jnp.max(s, axis=-1, keepdims=True), sink)
    pr = jnp.exp(s - m)
    denom = jnp.sum(pr, axis=-1, keepdims=True) + jnp.exp(sink - m)
    o = jnp.einsum('bngrqk,bnkgd->bnqgrd', (pr / denom).astype(vv.dtype), vv)
    return o.reshape(B, S, Hq * D)


def retention_chunkwise(q, k, v):
    B, S, H, K = q.shape
    V = v.shape[-1]
    C = C_CHUNK
    n = S // C
    lg = jnp.log1p(-jnp.exp2(-5.0 - jnp.arange(H, dtype=jnp.float32)))

    def chunks(t):
        return t.astype(jnp.float32).reshape(B, n, C, H, -1).transpose(0, 3, 1, 2, 4)

    qc = chunks(q)
    kc = chunks(k) * (K ** -0.5)
    vc = chunks(v)
    idx = jnp.arange(C, dtype=jnp.float32)
    diff = idx[:, None] - idx[None, :]
    decay = jnp.where(diff >= 0, jnp.exp(jnp.maximum(diff, 0.0)[None] * lg[:, None, None]), 0.0)
    att = jnp.einsum('bhnck,bhnsk->bhncs', qc, kc) * decay[None, :, None]
    intra = jnp.einsum('bhncs,bhnsv->bhncv', att, vc)
    q_in = qc * jnp.exp((idx + 1.0)[None, :] * lg[:, None])[None, :, None, :, None]
    k_in = kc * jnp.exp((C - 1.0 - idx)[None, :] * lg[:, None])[None, :, None, :, None]
    chunk_dec = jnp.exp(C * lg)[None, :, None, None]

    def step(state, xs):
        qx, kx, vx = xs
        o = jnp.einsum('bhck,bhkv->bhcv', qx, state)
        state = state * chunk_dec + jnp.einsum('bhck,bhcv->bhkv', kx, vx)
        return state, o

    xs = (jnp.moveaxis(q_in, 2, 0), jnp.moveaxis(k_in, 2, 0), jnp.moveaxis(vc, 2, 0))
    _, inter = lax.scan(step, jnp.zeros((B, H, K, V), jnp.float32), xs)
    o = intra + jnp.moveaxis(inter, 0, 2)
    return o.transpose(0, 2, 3, 1, 4).reshape(B, S, H, V)


def even_mixer(u, w_in, w_out, lb, onorm_g, sinks, cos_b, sin_b):
    B, S, _ = u.shape
    qa, fa, ia, ga, qb, kb, vb = jnp.split(u @ w_in, EVEN_SPLITS, axis=-1)
    oa = hgrn2_chunkwise(qa.reshape(B, S, A_HEADS, A_DK), fa.reshape(B, S, A_HEADS, A_DK),
                         ia.reshape(B, S, A_HEADS, A_DV), lb.reshape(A_HEADS, A_DK))
    oa = rmsnorm(oa, onorm_g) * jax.nn.silu(ga.reshape(B, S, A_HEADS, A_DV).astype(jnp.float32))
    oa = oa.reshape(B, S, A_VW).astype(u.dtype)
    qh = apply_rope(qb.reshape(B, S, B_Q_HEADS, B_HEAD_DIM), cos_b, sin_b)
    kh = apply_rope(kb.reshape(B, S, B_KV_HEADS, B_HEAD_DIM), cos_b, sin_b)
    ob = swa_with_sinks(qh, kh, vb.reshape(B, S, B_KV_HEADS, B_HEAD_DIM), sinks).astype(u.dtype)
    return jnp.concatenate([oa, ob], axis=-1) @ w_out


def odd_mixer(u, w_in, w_out, onorm_g, cos_c, sin_c):
    B, S, _ = u.shape
    qc, kc, vc, gc = jnp.split(u @ w_in, ODD_SPLITS, axis=-1)
    qh = apply_rope(qc.reshape(B, S, C_HEADS, C_DK), cos_c, sin_c)
    kh = apply_rope(kc.reshape(B, S, C_HEADS, C_DK), cos_c, sin_c)
    o = retention_chunkwise(qh, kh, vc.reshape(B, S, C_HEADS, C_DV))
    o = rmsnorm(o, onorm_g) * jax.nn.silu(gc.reshape(B, S, C_HEADS, C_DV).astype(jnp.float32))
    return o.reshape(B, S, C_VW).astype(u.dtype) @ w_out


def setup_inputs(seed: int = 0) -> dict:
    key = jax.random.key(seed)
    ks = jax.random.split(key, 18)
    f32 = jnp.float32

    def w(k, shape, fan_in):
        return jax.random.normal(k, shape, f32) * (fan_in ** -0.5)

    x = jax.random.normal(ks[0], (BATCH, SEQ, D_MODEL), f32)
    p = jax.random.normal(ks[1], (DEPTH, BATCH, SEQ, PLE_DIM), f32)
    offsets = jax.random.randint(ks[2], (BATCH, 1), 0, 4096, dtype=jnp.int32)
    positions = (offsets + jnp.arange(SEQ, dtype=jnp.int32)[None, :]).astype(jnp.int32)
    norm_g = 1.0 + 0.05 * jax.random.normal(ks[3], (DEPTH, N_NORMS, D_MODEL), f32)
    ffn1_w_in = w(ks[4], (DEPTH, D_MODEL, 2 * D_FF), D_MODEL)
    ffn1_w_out = w(ks[5], (DEPTH, D_FF, D_MODEL), D_FF)
    ffn2_w_in = w(ks[6], (DEPTH, D_MODEL, 2 * D_FF), D_MODEL)
    ffn2_w_out = w(ks[7], (DEPTH, D_FF, D_MODEL), D_FF)
    ple_w_proj = w(ks[8], (DEPTH, PLE_DIM, D_MODEL), PLE_DIM)
    ple_w_gate = w(ks[9], (DEPTH, D_MODEL, D_MODEL), D_MODEL)
    even_w_in = w(ks[10], (N_EVEN, D_MODEL, EVEN_IN), D_MODEL)
    even_w_out = w(ks[11], (N_EVEN, EVEN_OUT, D_MODEL), EVEN_OUT)
    hgrn_lb = 0.1 * jax.random.normal(ks[12], (N_EVEN + 1, A_KW), f32)
    hgrn_onorm_g = 1.0 + 0.05 * jax.random.normal(ks[13], (N_EVEN, A_DV), f32)
    attn_sinks = jax.random.normal(ks[14], (N_EVEN, B_Q_HEADS), f32)
    odd_w_in = w(ks[15], (N_ODD, D_MODEL, ODD_IN), D_MODEL)
    odd_w_out = w(ks[16], (N_ODD, ODD_OUT, D_MODEL), ODD_OUT)
    ret_onorm_g = 1.0 + 0.05 * jax.random.normal(ks[17], (N_ODD, C_DV), f32)
    return {'x': x, 'p': p, 'positions': positions, 'norm_g': norm_g,
            'ffn1_w_in': ffn1_w_in, 'ffn1_w_out': ffn1_w_out, 'ffn2_w_in': ffn2_w_in, 'ffn2_w_out': ffn2_w_out,
            'ple_w_proj': ple_w_proj, 'ple_w_gate': ple_w_gate,
            'even_w_in': even_w_in, 'even_w_out': even_w_out, 'hgrn_lb': hgrn_lb, 'hgrn_onorm_g': hgrn_onorm_g,
            'attn_sinks': attn_sinks, 'odd_w_in': odd_w_in, 'odd_w_out': odd_w_out, 'ret_onorm_g': ret_onorm_g}


def reference(x, p, positions, norm_g, ffn1_w_in, ffn1_w_out, ffn2_w_in, ffn2_w_out, ple_w_proj, ple_w_gate,
              even_w_in, even_w_out, hgrn_lb, hgrn_onorm_g, attn_sinks, odd_w_in, odd_w_out, ret_onorm_g):
    inv_b = ROPE_THETA ** (-jnp.arange(0, B_HEAD_DIM, 2, dtype=jnp.float32) / B_HEAD_DIM)
    inv_c = RET_THETA ** (-jnp.linspace(0.0, 1.0, C_DK // 2, dtype=jnp.float32))
    cos_b, sin_b = rope_tables(positions, inv_b)
    cos_c, sin_c = rope_tables(positions, inv_c)
    lb_all = jnp.cumsum(jax.nn.softmax(hgrn_lb.astype(jnp.float32), axis=0), axis=0)
    h = x
    for i in range(DEPTH):
        g = norm_g[i]
        h = h + 0.5 * rmsnorm(swiglu(rmsnorm(h, g[0]), ffn1_w_in[i], ffn1_w_out[i]), g[1])
        u = rmsnorm(h, g[2])
        j = i // 2
        if i % 2 == 0:
            mix = even_mixer(u, even_w_in[j], even_w_out[j], lb_all[j], hgrn_onorm_g[j], attn_sinks[j], cos_b, sin_b)
        else:
            mix = odd_mixer(u, odd_w_in[j], odd_w_out[j], ret_onorm_g[j], cos_c, sin_c)
        h = h + rmsnorm(mix, g[3])
        h = h + 0.5 * rmsnorm(swiglu(rmsnorm(h, g[4]), ffn2_w_in[i], ffn2_w_out[i]), g[5])
        gate = jax.nn.sigmoid(h @ ple_w_gate[i])
        h = h + rmsnorm(gate * (p[i] @ ple_w_proj[i]), g[6])
    return h
```

```python
from contextlib import ExitStack
import numpy as np
import concourse.bass as bass
import concourse.mybir as mybir
from concourse.bass_utils import run_bass_kernel_spmd

F32 = mybir.dt.float32
BF16 = mybir.dt.bfloat16
I32 = mybir.dt.int32
AF = mybir.ActivationFunctionType
ALU = mybir.AluOpType
AX = mybir.AxisListType

_DSZ = {F32: 4, BF16: 2, I32: 4}
COMPUTE = ("pe", "act", "dve", "pool")
SAME_ENGINE_SYNC = True


def _rng(ap):
    sz = _DSZ[ap.dtype]
    steps = ap.ap
    off = int(ap.offset)
    if str(ap.space) == "DRAM":
        ext = sum((c - 1) * s for s, c in steps)
        return (ap.name, off * sz, (off + ext + 1) * sz)
    if str(ap.space) == "PSUM":
        return (ap.name, 0, 2048)
    pstep = steps[0][0]
    lo = off % pstep if pstep > 0 else off
    ext = sum((c - 1) * s for s, c in steps[1:])
    return (ap.name, lo * sz, (lo + ext + 1) * sz)


class Op:
    __slots__ = ("eng", "fn", "deps", "sem", "cnt", "needs_inc", "inc_idx", "is_dma", "seq", "epoch")

    def __init__(self, eng, fn, is_dma=False):
        self.seq = 0
        self.epoch = 0
        self.eng = eng
        self.fn = fn
        self.deps = set()
        self.sem = None
        self.cnt = 0
        self.needs_inc = False
        self.inc_idx = 0
        self.is_dma = is_dma


class DmaSem:
    def __init__(self, handle):
        self.h = handle
        self.n = 0
        self.last = None


class Prog:
    def __init__(self, nc):
        self.nc = nc
        self.streams = {e: [] for e in ("pe", "act", "dve", "pool", "sp")}
        self.acc = {}
        self.dma_sems = []
        self.epoch = 0

    def _add(self, op):
        op.seq = len(self.streams[op.eng])
        op.epoch = self.epoch
        last = {}
        keep = set()
        for d in op.deps:
            if d.is_dma:
                keep.add(d)
            elif d.eng not in last or d.seq > last[d.eng].seq:
                last[d.eng] = d
        op.deps = keep | set(last.values())
        self.streams[op.eng].append(op)

    def new_dma_sem(self, stack, name):
        s = DmaSem(stack.enter_context(self.nc.semaphore(name)))
        self.dma_sems.append(s)
        return s

    def _track(self, op, reads, writes):
        writes = list(writes) + [ap for ap in reads if str(ap.space) == "PSUM"]
        reads = [ap for ap in reads if str(ap.space) != "PSUM"]
        for ap in reads:
            n, lo, hi = _rng(ap)
            a = self.acc.setdefault(n, ([], []))
            for (l, h, o) in a[0]:
                if l < hi and lo < h:
                    op.deps.add(o)
            a[1].append((lo, hi, op))
        for ap in writes:
            n, lo, hi = _rng(ap)
            a = self.acc.setdefault(n, ([], []))
            for (l, h, o) in a[0]:
                if l < hi and lo < h and o is not op:
                    op.deps.add(o)
            for (l, h, o) in a[1]:
                if l < hi and lo < h and o is not op:
                    op.deps.add(o)
            a[0][:] = [t for t in a[0] if not (lo <= t[0] and t[1] <= hi)]
            a[1][:] = [t for t in a[1] if not (lo <= t[0] and t[1] <= hi)]
            a[0].append((lo, hi, op))

    def emit(self, eng, fn, reads=(), writes=()):
        op = Op(eng, fn)
        self._track(op, reads, writes)
        self._add(op)
        return op

    def dma(self, queue, out, in_, sem):
        op = Op(queue, lambda e: e.dma_start(out=out, in_=in_), is_dma=True)
        if sem.last is not None:
            op.deps.add(sem.last)
        sem.n += 1
        sem.last = op
        op.sem = sem
        op.cnt = sem.n
        self._track(op, [in_], [out])
        self._add(op)
        return op

    def mm(self, out, lhsT, rhs, start=True, stop=True):
        return self.emit("pe", lambda e: e.matmul(out, lhsT, rhs, start=start, stop=stop),
                         reads=[lhsT, rhs], writes=[out])

    def tr(self, out, in_, ident):
        return self.emit("pe", lambda e: e.transpose(out, in_, ident), reads=[in_, ident], writes=[out])

    def actf(self, out, in_, func, bias=None, scale=None, accum_out=None):
        kw = {}
        rd = [in_]
        wr = [out]
        if bias is not None:
            kw["bias"] = bias
            if not isinstance(bias, (int, float)):
                rd.append(bias)
        if scale is not None:
            kw["scale"] = scale
            if not isinstance(scale, (int, float)):
                rd.append(scale)
        if accum_out is not None:
            kw["accum_out"] = accum_out
            wr.append(accum_out)
        return self.emit("act", lambda e: e.activation(out, in_, func, **kw), reads=rd, writes=wr)

    def tt(self, eng, out, in0, in1, op):
        return self.emit(eng, lambda e: e.tensor_tensor(out, in0, in1, op), reads=[in0, in1], writes=[out])

    def ts(self, eng, out, in0, s1, op0, s2=None, op1=None):
        rd = [in0] + [s for s in (s1, s2) if s is not None and not isinstance(s, (int, float))]
        kw = {}
        if op1 is not None:
            kw["op1"] = op1
        return self.emit(eng, lambda e: e.tensor_scalar(out, in0, s1, s2, op0, **kw), reads=rd, writes=[out])

    def stt(self, eng, out, in0, scalar, in1, op0, op1):
        rd = [in0, in1] + ([scalar] if not isinstance(scalar, (int, float)) else [])
        return self.emit(eng, lambda e: e.scalar_tensor_tensor(out, in0, scalar, in1, op0, op1),
                         reads=rd, writes=[out])

    def copy(self, eng, out, in_):
        if eng == "act":
            return self.emit("act", lambda e: e.copy(out, in_), reads=[in_], writes=[out])
        return self.emit(eng, lambda e: e.tensor_copy(out, in_), reads=[in_], writes=[out])

    def memset(self, eng, ap, val):
        return self.emit(eng, lambda e: e.memset(ap, val), writes=[ap])

    def finalize(self, sems, block):
        def needs_sem(op, d):
            if d.is_dma:
                return True
            if d.eng == op.eng and not op.is_dma:
                if d.eng == "pe" or not SAME_ENGINE_SYNC:
                    return False
            return True

        for st in self.streams.values():
            for op in st:
                for d in op.deps:
                    if not d.is_dma and needs_sem(op, d):
                        d.needs_inc = True
        self.max_inc = {}
        for eng in COMPUTE:
            k = {}
            for op in self.streams[eng]:
                if not op.is_dma and op.needs_inc:
                    k[op.epoch] = k.get(op.epoch, 0) + 1
                    op.inc_idx = k[op.epoch]
            self.max_inc[eng] = max(list(k.values()) + [0])
        stats = {}
        nc = self.nc
        engobj = {"pe": nc.tensor, "act": nc.scalar, "dve": nc.vector, "pool": nc.gpsimd, "sp": nc.sync}

        def run(eng):
            e = engobj[eng]
            waited = {}
            nw = 0
            for op in self.streams[eng]:
                need = {}
                for d in op.deps:
                    if not needs_sem(op, d):
                        continue
                    if d.is_dma:
                        key, v, h = ("d", id(d.sem)), d.cnt * 16, d.sem.h
                    else:
                        key, v, h = ("c", d.eng, d.epoch), d.inc_idx, sems[d.eng][d.epoch]
                    if v > need.get(key, (0, None))[0]:
                        need[key] = (v, h)
                for key, (v, h) in need.items():
                    if v > waited.get(key, 0):
                        e.wait_ge(h, v)
                        waited[key] = v
                        nw += 1
                ins = op.fn(e)
                if op.is_dma:
                    ins.then_inc(op.sem.h, 16)
                elif op.needs_inc:
                    ins.then_inc(sems[eng][op.epoch], 1)
            if eng == "sp":
                for s in self.dma_sems:
                    if s.n:
                        e.wait_ge(s.h, s.n * 16)
            stats[eng] = (len(self.streams[eng]), nw)

        block.tensor(lambda _e: run("pe"))
        block.scalar(lambda _e: run("act"))
        block.vector(lambda _e: run("dve"))
        block.gpsimd(lambda _e: run("pool"))
        block.sync(lambda _e: run("sp"))
        return stats


D = 1024
T = 512
DFF = 2816
EPS = 1e-6
NJ = DFF // 128
R_SLOTS = 4
EP_TILES = 2
WCOLS = 4096


class Cols:
    def __init__(self):
        self.n = 0
        self.d = {}

    def add(self, name, w):
        self.d[name] = (self.n, w)
        self.n += w


CST = Cols()
for _n, _w in (("gT", 112), ("lbT", 8), ("ogh", 1), ("ogr", 4), ("sinks", 8), ("invb", 1), ("invc", 1),
               ("ident", 128), ("maskU", 64), ("scanmask", 512), ("swam", 256), ("swam0", 256),
               ("decayT", 512), ("gq", 512), ("gk", 512)):
    CST.add(_n, _w)
NCST = CST.n


def unit_list():
    u = []
    for l in range(2):
        u += [(("ffn1_in", l, i), 4096) for i in range(11)]
        u += [(("ffn1_out", l, i), 2816) for i in range(8)]
        if l == 0:
            u += [(("hg", i), 4096) for i in range(4)]
            u += [(("swa", i), 3072) for i in range(2)]
            u += [(("ewout", i), 3072) for i in range(4)]
        else:
            for hh in range(4):
                u += [(("oq", hh), 2048), (("ok", hh), 2048), (("ov", hh), 4096), (("og", hh), 4096)]
            u += [(("owout", i), 4096) for i in range(4)]
        u += [(("ffn2_in", l, i), 4096) for i in range(11)]
        u += [(("ffn2_out", l, i), 2816) for i in range(8)]
        u += [(("pproj", l), 2048), (("pgate", l, 0), 4096), (("pgate", l, 1), 4096)]
    return u


UNITS = unit_list()
NU = len(UNITS)
UIDX = {k: i for i, (k, _) in enumerate(UNITS)}


def _kmaj(W, cols):
    kc = W.shape[0] // 128
    return np.ascontiguousarray(W.reshape(kc, 128, W.shape[1])[:, :, cols].transpose(1, 0, 2)).reshape(128, -1)


def pack_weights(inp):
    wall = np.zeros((NU, 128, WCOLS), np.float32)
    ar = np.arange
    for (key, n) in UNITS:
        i = UIDX[key]
        k0 = key[0]
        if k0 in ("ffn1_in", "ffn2_in"):
            W = inp[k0[:4] + "_w_in"][key[1]]
            u = key[2]
            cols = np.concatenate([ar(256 * u, 256 * u + 256), DFF + ar(256 * u, 256 * u + 256)])
            blk = _kmaj(W, cols)
        elif k0 in ("ffn1_out", "ffn2_out"):
            W = inp[k0[:4] + "_w_out"][key[1]]
            blk = _kmaj(W, ar(128 * key[2], 128 * key[2] + 128))
        elif k0 == "hg":
            W = inp["even_w_in"][0]
            hh = key[1]
            cols = np.concatenate([g * 512 + hh * 128 + ar(128) for g in range(4)])
            blk = _kmaj(W, cols)
        elif k0 == "swa":
            W = inp["even_w_in"][0]
            g = key[1]
            cols = np.concatenate([2048 + 256 * g + ar(256), 2560 + 64 * g + ar(64), 2688 + 64 * g + ar(64)])
            blk = _kmaj(W, cols)
        elif k0 == "ewout":
            W = inp["even_w_out"][0]
            cols = ar(256 * key[1], 256 * key[1] + 256)
            a = _kmaj(W[0:512], cols)
            b = np.zeros((128, 8, 256), np.float32)
            b[0:64] = W[512:1024].reshape(8, 64, 1024)[:, :, cols].transpose(1, 0, 2)
            blk = np.concatenate([a, b.reshape(128, -1)], axis=1)
        elif k0 in ("oq", "ok", "ov", "og"):
            W = inp["odd_w_in"][0]
            hh = key[1]
            base = {"oq": 0, "ok": 1024, "ov": 2048, "og": 4096}[k0]
            wd = 256 if k0 in ("oq", "ok") else 512
            blk = _kmaj(W, base + hh * wd + ar(wd))
        elif k0 == "owout":
            W = inp["odd_w_out"][0]
            blk = _kmaj(W, ar(256 * key[1], 256 * key[1] + 256))
        elif k0 == "pproj":
            blk = _kmaj(inp["ple_w_proj"][key[1]], ar(1024))
        elif k0 == "pgate":
            blk = _kmaj(inp["ple_w_gate"][key[1]], ar(512 * key[2], 512 * key[2] + 512))
        assert blk.shape == (128, n), (key, blk.shape, n)
        wall[i, :, :n] = blk
    return wall


def pack_consts(inp):
    c = np.zeros((128, NCST), np.float32)

    def put(name, arr):
        o, w = CST.d[name]
        arr = np.asarray(arr, np.float32)
        c[: arr.shape[0], o:o + w] = arr.reshape(arr.shape[0], w)

    put("gT", inp["norm_g"].reshape(2, 7, 8, 128).transpose(3, 0, 1, 2).reshape(128, 112))
    put("lbT", inp["hgrn_lb"].reshape(2, 4, 128).transpose(2, 0, 1).reshape(128, 8))
    put("ogh", inp["hgrn_onorm_g"].reshape(128, 1))
    put("ogr", inp["ret_onorm_g"].reshape(4, 128).T)
    put("sinks", np.broadcast_to(inp["attn_sinks"].reshape(1, 8), (128, 8)))
    inv_b = (np.float32(10000.0) ** (-np.arange(0, 64, 2, dtype=np.float32) / np.float32(64))).astype(np.float32)
    inv_c = (np.float32(10000.0) ** (-np.linspace(0.0, 1.0, 128, dtype=np.float32))).astype(np.float32)
    put("invb", np.concatenate([inv_b, inv_b]).reshape(64, 1))
    put("invc", inv_c.reshape(128, 1))
    put("ident", np.eye(128, dtype=np.float32))
    s = np.arange(64)
    put("maskU", (s[:, None] <= s[None, :]).astype(np.float32))
    sm = np.ones(512, np.float32)
    sm[::64] = 0.0
    put("scanmask", np.broadcast_to(sm, (128, 512)))
    qi = np.arange(128)[:, None]
    kj = np.arange(256)[None, :]
    diff = qi + 128 - kj
    valid = (diff >= 0) & (diff < 128)
    put("swam", np.where(valid, 0.0, -30000.0))
    put("swam0", np.where(valid & (kj >= 128), 0.0, -30000.0))
    hh = np.arange(4, dtype=np.float64)
    lg = np.log1p(-np.exp2(-5.0 - hh))
    idx = np.arange(128, dtype=np.float64)
    dcs = idx[None, :] - idx[:, None]
    dec = np.where(dcs[None] >= 0, np.exp(np.maximum(dcs, 0.0)[None] * lg[:, None, None]), 0.0)
    put("decayT", dec.transpose(1, 0, 2).reshape(128, 512))
    put("gq", np.broadcast_to(np.exp((idx + 1.0)[None, :] * lg[:, None]).reshape(1, 512), (128, 512)))
    put("gk", np.broadcast_to(np.exp((127.0 - idx)[None, :] * lg[:, None]).reshape(1, 512), (128, 512)))
    return c


GAMMA128 = [float(np.exp(128.0 * np.log1p(-np.exp2(-5.0 - h)))) for h in range(4)]


def build_nc(NT, nstage=8):
    S = NT * T
    nc = bass.Bass("TRN2", target_bir_lowering=False)
    x_d = nc.dram_tensor("x", [S, D], F32, kind="ExternalInput").ap()
    p_d = nc.dram_tensor("p", [2, S, 256], F32, kind="ExternalInput").ap()
    pos_d = nc.dram_tensor("pos", [1, S], I32, kind="ExternalInput").ap()
    wall_d = nc.dram_tensor("wall", [NU, 128, WCOLS], F32, kind="ExternalInput").ap()
    cst_d = nc.dram_tensor("cst", [128, NCST], F32, kind="ExternalInput").ap()
    out_d = nc.dram_tensor("out", [S, D], F32, kind="ExternalOutput").ap()
    wbf_d = nc.dram_tensor("wbf", [NU, 128, WCOLS], BF16, kind="Internal").ap()

    with ExitStack() as st:
        P = Prog(nc)
        sb = lambda n, s, d: st.enter_context(nc.sbuf_tensor(n, s, d))
        h = sb("h", [128, 8, T], F32)
        xn = sb("xn", [128, 8, T], BF16)
        ybuf = sb("ybuf", [128, 8, T], F32)
        sqb = sb("sqb", [128, 2, T], BF16)
        rstd = sb("rstd", [128, T], F32)
        wring = sb("wring", [128, R_SLOTS, WCOLS], BF16)
        ptile = sb("ptile", [128, 4, 256], F32)
        pTb = sb("pTb", [128, 2, T], BF16)
        cosb = sb("cosb", [64, T], F32)
        sinb = sb("sinb", [64, T], F32)
        cosc = sb("cosc", [128, T], F32)
        sinc = sb("sinc", [128, T], F32)
        retS = sb("retS", [128, 8, 512], F32)
        retSb = sb("retSb", [128, 8, 512], BF16)
        hgS = sb("hgS", [128, 4, 128], F32)
        hgSb = sb("hgSb", [128, 4, 128], BF16)
        kT = sb("kT", [64, 2, 640], BF16)
        vswa = sb("vswa", [128, 5, 128], BF16)
        csb = sb("csb", [128, NCST], F32)
        gsc = sb("gsc", [128, 112], F32)
        lbs = sb("lbs", [128, 12], F32)
        identb = sb("identb", [128, 128], BF16)
        onesb = sb("onesb", [128, 128], BF16)
        smallc = sb("smallc", [128, 16], F32)
        ARENA = 72 * 1024
        arena = sb("arena", [128, ARENA // 2], BF16)
        pb = [st.enter_context(nc.psum_tensor(f"pb{i}", [128, 512], F32)) for i in range(8)]
        NEP = (NT + EP_TILES - 1) // EP_TILES
        sems = {e: [st.enter_context(nc.semaphore(f"s_{e}{i}")) for i in range(NEP)] for e in COMPUTE}
        wsem_all = [[P.new_dma_sem(st, f"w{j}_{i}") for i in range(R_SLOTS)] for j in range(NEP)]
        csem = [P.new_dma_sem(st, f"c{i}") for i in range(8)]
        xsem = P.new_dma_sem(st, "xs")
        osem = P.new_dma_sem(st, "os")
        psem = P.new_dma_sem(st, "ps")
        qsem = P.new_dma_sem(st, "qs")
        ksem = P.new_dma_sem(st, "ks")
        blk = st.enter_context(nc.Block())

        def av(lo, nbytes, dt, inner=None, parts=128):
            a = arena[0:parts, lo // 2:(lo + nbytes) // 2]
            if dt != BF16:
                a = a.bitcast(dt)
            if inner is not None:
                a = a.rearrange("p (a b) -> p a b", b=inner)
            return a

        K = 1024

        def cc(name, lo=0, n=None, parts=128):
            o, w = CST.d[name]
            n = w - lo if n is None else n
            return csb[0:parts, o + lo:o + lo + n]

        identf = cc("ident")
        bank_ctr = [0]

        def nb():
            b = pb[bank_ctr[0] % 7]
            bank_ctr[0] += 1
            return b

        ss_bank = pb[7]

        wctr = [0]

        def wload(key):
            i = UIDX[key]
            n = UNITS[i][1]
            s = wctr[0] % R_SLOTS
            wctr[0] += 1
            P.dma("sp", wring[:, s, 0:n], wbf_d[i, :, 0:n], wsem_all[P.epoch][s])
            return wring[:, s, 0:n]

        P.dma("sp", csb[:], cst_d, ksem)
        for i, (key, n) in enumerate(UNITS):
            P.dma("pool", wbf_d[i, :, 0:n], wall_d[i, :, 0:n], csem[i % 8])
        P.copy("dve", identb[:], identf)
        P.memset("dve", onesb[:], 1.0)
        P.ts("dve", gsc[:], cc("gT"), 0.5, ALU.mult)
        P.tt("dve", lbs[:, 0:4], cc("lbT", 0, 4), cc("lbT", 4, 4), ALU.subtract)
        P.actf(lbs[:, 4:8], lbs[:, 0:4], AF.Sigmoid)
        P.ts("dve", lbs[:, 8:12], lbs[:, 4:8], -1.0, ALU.mult, 1.0, ALU.add)
        for tns in (retS, retSb, hgS, hgSb, vswa):
            P.memset("dve", tns[:], 0.0)
        P.memset("dve", kT[:], 0.0)

        def gcol(l, n, k, scaled=False):
            c = (l * 7 + n) * 8 + k
            return gsc[:, c:c + 1] if scaled else cc("gT", c, 1)

        def rstd_from_ss(ss_ap, out_ap, inv_n):
            P.actf(out_ap, ss_ap, AF.Ln, bias=EPS, scale=inv_n)
            P.actf(out_ap, out_ap, AF.Exp, scale=-0.5)

        def prenorm(l, n):
            for k in range(8):
                P.actf(sqb[:, k % 2, :], h[:, k, :], AF.Square)
                P.mm(ss_bank[:], onesb[:], sqb[:, k % 2, :], start=(k == 0), stop=(k == 7))
            rstd_from_ss(ss_bank[:], rstd[:], 1.0 / D)
            for k in range(8):
                P.stt("dve", xn[:, k, :], h[:, k, :], gcol(l, n, k), rstd[:], ALU.mult, ALU.mult)

        def evac_y(py, o):
            P.copy("dve", ybuf[:, o, :], py)
            P.actf(sqb[:, o % 2, :], ybuf[:, o, :], AF.Square)
            P.mm(ss_bank[:], onesb[:], sqb[:, o % 2, :], start=(o == 0), stop=(o == 7))

        def postnorm_res(l, n, half):
            rstd_from_ss(ss_bank[:], rstd[:], 1.0 / D)
            for k in range(8):
                P.tt("pool", ybuf[:, k, :], ybuf[:, k, :], rstd[:], ALU.mult)
                P.stt("dve", h[:, k, :], ybuf[:, k, :], gcol(l, n, k, scaled=half), h[:, k, :], ALU.mult, ALU.add)

        def ffn(l, which):
            n0 = 0 if which == 1 else 4
            hid = av(0, 22 * K, BF16, inner=T)
            sg = av(22 * K, 4 * K, F32, inner=T)
            prenorm(l, n0)
            for u in range(11):
                w = wload((f"ffn{which}_in", l, u)).rearrange("p (k c) -> p k c", c=512)
                for jj in range(2):
                    j = 2 * u + jj
                    pg = nb()
                    pu = nb()
                    for k in range(8):
                        P.mm(pg[:], w[:, k, 128 * jj:128 * jj + 128], xn[:, k, :], start=(k == 0), stop=(k == 7))
                    for k in range(8):
                        P.mm(pu[:], w[:, k, 256 + 128 * jj:256 + 128 * jj + 128], xn[:, k, :], start=(k == 0), stop=(k == 7))
                    P.actf(sg[:, j % 2, :], pg[:], AF.Silu)
                    P.tt("dve", hid[:, j, :], sg[:, j % 2, :], pu[:], ALU.mult)
            for o in range(8):
                w = wload((f"ffn{which}_out", l, o)).rearrange("p (k c) -> p k c", c=128)
                py = nb()
                for j in range(NJ):
                    P.mm(py[:], w[:, j, :], hid[:, j, :], start=(j == 0), stop=(j == NJ - 1))
                evac_y(py[:], o)
            postnorm_res(l, n0 + 1, True)

        def ple(l, t):
            for k in range(8):
                P.copy("act" if k % 2 else "dve", xn[:, k, :], h[:, k, :])
            P.dma("sp", ptile[:], p_d[l, t * T:(t + 1) * T, :].rearrange("(b p) f -> p b f", p=128), psem)
            for c in range(2):
                bk = nb()
                for b in range(4):
                    P.tr(bk[:, 128 * b:128 * b + 128], ptile[:, b, 128 * c:128 * c + 128], identf)
                P.copy("act", pTb[:, c, :], bk[:])
            w = wload(("pproj", l)).rearrange("p (k c) -> p k c", c=1024)
            for o in range(8):
                bk = nb()
                for c in range(2):
                    P.mm(bk[:], w[:, c, 128 * o:128 * o + 128], pTb[:, c, :], start=(c == 0), stop=(c == 1))
                P.copy("dve", ybuf[:, o, :], bk[:])
            sg = av(22 * K, 4 * K, F32, inner=T)
            for u in range(2):
                w = wload(("pgate", l, u)).rearrange("p (k c) -> p k c", c=512)
                for o4 in range(4):
                    o = 4 * u + o4
                    bk = nb()
                    for k in range(8):
                        P.mm(bk[:], w[:, k, 128 * o4:128 * o4 + 128], xn[:, k, :], start=(k == 0), stop=(k == 7))
                    P.actf(sg[:, o % 2, :], bk[:], AF.Sigmoid)
                    P.tt("dve", ybuf[:, o, :], ybuf[:, o, :], sg[:, o % 2, :], ALU.mult)
                    P.actf(sqb[:, o % 2, :], ybuf[:, o, :], AF.Square)
                    P.mm(ss_bank[:], onesb[:], sqb[:, o % 2, :], start=(o == 0), stop=(o == 7))
            postnorm_res(l, 6, False)

        xio = av(0, 16 * K, F32, inner=D)

        def load_x(t):
            P.dma("sp", xio, x_d[t * T:(t + 1) * T, :].rearrange("(b p) f -> p b f", p=128), xsem)
            for k in range(8):
                bk = nb()
                for b in range(4):
                    P.tr(bk[:, 128 * b:128 * b + 128], xio[:, b, 128 * k:128 * k + 128], identf)
                P.copy("act" if k % 2 else "dve", h[:, k, :], bk[:])

        def store_out(t):
            for b in range(4):
                for hf in range(2):
                    bk = nb()
                    for kk in range(4):
                        k = 4 * hf + kk
                        P.tr(bk[:, 128 * kk:128 * kk + 128], h[:, k, 128 * b:128 * b + 128], identf)
                    P.copy("act" if hf else "dve", xio[:, b, 512 * hf:512 * hf + 512], bk[:])
            P.dma("sp", out_d[t * T:(t + 1) * T, :].rearrange("(b p) f -> p b f", p=128), xio, osem)

        TWO_PI = float(2 * np.pi)
        MAGIC = 12582912.0
        C1 = 6.28125
        C2 = float(2 * np.pi - 6.28125)
        PI_LO = 3.1415925

        def trig_tables(t):
            posi = av(16 * K, 2 * K, I32)
            posf = av(18 * K, 2 * K, F32)
            ang = av(20 * K, 2 * K, F32)
            nn = av(22 * K, 2 * K, F32)
            r2 = av(24 * K, 2 * K, F32)
            P.dma("sp", posi, pos_d[:, t * T:(t + 1) * T].partition_broadcast(128), qsem)
            P.copy("dve", posf, posi)
            for (inv, parts, cs, sn) in ((cc("invb", parts=64), 64, cosb, sinb), (cc("invc"), 128, cosc, sinc)):
                a = ang[0:parts]
                n_ = nn[0:parts]
                r = r2[0:parts]
                P.ts("dve", a, posf[0:parts], inv, ALU.mult)
                P.ts("dve", n_, a, float(1.0 / TWO_PI), ALU.mult, MAGIC, ALU.add)
                P.ts("dve", n_, n_, -MAGIC, ALU.add)
                P.stt("dve", a, n_, -C1, a, ALU.mult, ALU.add)
                P.stt("dve", a, n_, -C2, a, ALU.mult, ALU.add)
                P.ts("dve", a, a, -PI_LO, ALU.max, PI_LO, ALU.min)
                P.actf(sn[:], a, AF.Sin)
                P.ts("dve", r, a, float(np.pi / 2), ALU.add)
                P.ts("dve", n_, r, PI_LO, ALU.is_gt, TWO_PI, ALU.mult)
                P.tt("dve", r, r, n_, ALU.subtract)
                P.ts("dve", r, r, -PI_LO, ALU.max, PI_LO, ALU.min)
                P.actf(cs[:], r, AF.Sin)

        def even_mixer(t):
            l = 0
            prenorm(l, 2)
            catA = av(0, 4 * K, BF16, inner=T)
            catB = av(4 * K, 8 * K, BF16, inner=T, parts=64)
            qf = av(12 * K, 2 * K, F32)
            fb = av(14 * K, 2 * K, F32)
            kk_ = av(16 * K, 2 * K, F32)
            bb = av(18 * K, 2 * K, F32)
            e1 = av(20 * K, 2 * K, F32)
            e2 = av(22 * K, 2 * K, F32)
            qs = av(24 * K, 1 * K, BF16)
            ks = av(25 * K, 1 * K, BF16)
            kdT = av(26 * K, 1 * K, BF16)
            vtok = av(27 * K, 2 * K, BF16, inner=128, parts=64)
            gs = av(29 * K, 2 * K, F32)
            obuf = av(31 * K, 2 * K, F32)
            decb = av(33 * K, 32, F32)
            kdtok = av(33 * K + 512, 512, BF16, inner=128, parts=64)
            attb = av(34 * K, 256, BF16, inner=64, parts=64)
            lb = lbs[:, 4:8]
            omlb = lbs[:, 8:12]
            maskU = cc("maskU", parts=64)
            for hh in range(4):
                w = wload(("hg", hh)).rearrange("p (k c) -> p k c", c=512)
                pq = nb()
                for k in range(8):
                    P.mm(pq[:], w[:, k, 0:128], xn[:, k, :], start=(k == 0), stop=(k == 7))
                P.actf(qf, pq[:], AF.Silu)
                pf = nb()
                for k in range(8):
                    P.mm(pf[:], w[:, k, 128:256], xn[:, k, :], start=(k == 0), stop=(k == 7))
                P.actf(fb, pf[:], AF.Sigmoid)
                P.ts("dve", fb, fb, omlb[:, hh:hh + 1], ALU.mult, lb[:, hh:hh + 1], ALU.add)
                P.ts("dve", kk_, fb, -1.0, ALU.mult, 1.0, ALU.add)
                P.actf(e1, fb, AF.Ln)
                P.emit("dve", lambda e, o=bb, m=cc("scanmask"), d=e1: e.tensor_tensor_scan(o, m, d, 0.0, ALU.mult, ALU.add),
                       reads=[cc("scanmask"), e1], writes=[bb])
                b3 = bb.rearrange("p (c t) -> p c t", t=64)
                P.actf(e1, bb, AF.Exp)
                P.tt("dve", qs, qf, e1, ALU.mult)
                P.actf(e2, bb, AF.Exp, scale=-1.0)
                P.tt("dve", ks, kk_, e2, ALU.mult)
                P.actf(decb, b3[:, :, 63], AF.Exp)
                e13 = e1.rearrange("p (c t) -> p c t", t=64)
                P.tt("dve", e13, b3[:, :, 63:64].to_broadcast([128, 8, 64]), b3, ALU.subtract)
                P.actf(e1, e1, AF.Exp)
                P.tt("dve", kdT, kk_, e1, ALU.mult)
                for c in range(8):
                    pv = nb()
                    for k in range(8):
                        P.mm(pv[0:64, 0:128], xn[:, k, 64 * c:64 * c + 64], w[:, k, 256:384], start=(k == 0), stop=(k == 7))
                    P.copy("act", vtok[:, c, :], pv[0:64, 0:128])
                pgt = nb()
                for k in range(8):
                    P.mm(pgt[:], w[:, k, 384:512], xn[:, k, :], start=(k == 0), stop=(k == 7))
                P.actf(gs, pgt[:], AF.Silu)
                for c in range(8):
                    cs_ = slice(64 * c, 64 * c + 64)
                    r = c % 2
                    pt_ = nb()
                    ptb = pt_[:].bitcast(BF16)
                    P.tr(ptb[0:64, 0:128], kdT[:, cs_], identb[:])
                    P.copy("act", kdtok[:, r, :], ptb[0:64, 0:128])
                    pa = nb()
                    P.mm(pa[0:64, 0:64], ks[:, cs_], qs[:, cs_])
                    P.tt("dve", attb[:, r, :], pa[0:64, 0:64], maskU, ALU.mult)
                    po = nb()
                    P.mm(po[:, 0:64], vtok[:, c, :], attb[:, r, :], start=True, stop=False)
                    P.mm(po[:, 0:64], hgSb[:, hh, :], qs[:, cs_], start=False, stop=True)
                    P.copy("act", obuf[:, cs_], po[:, 0:64])
                    pu_ = nb()
                    P.mm(pu_[:, 0:128], kdtok[:, r, :], vtok[:, c, :])
                    P.stt("dve", hgS[:, hh, :], hgS[:, hh, :], decb[:, c:c + 1], pu_[:, 0:128], ALU.mult, ALU.add)
                    P.copy("act", hgSb[:, hh, :], hgS[:, hh, :])
                P.actf(sqb[:, 0, :], obuf, AF.Square)
                pn_ = nb()
                P.mm(pn_[:], onesb[:], sqb[:, 0, :])
                rstd_from_ss(pn_[:], e2, 1.0 / 128)
                P.stt("dve", obuf, obuf, cc("ogh"), e2, ALU.mult, ALU.mult)
                P.tt("dve", catA[:, hh, :], obuf, gs, ALU.mult)
            qraw = av(35 * K, 8 * K, F32, inner=T, parts=64)
            kraw = av(43 * K, 2 * K, F32, parts=64)
            ra = av(45 * K, 8 * K, F32, inner=T, parts=64)
            rb = av(53 * K, 8 * K, F32, inner=T, parts=64)
            qT = av(61 * K, 4 * K, BF16, inner=T, parts=64)
            smx = av(65 * K, 1 * K, F32)
            pexp = av(66 * K, 1 * K, F32)
            pnb = av(67 * K, 512, BF16)
            pTt = av(67 * K + 512, 512, BF16, inner=128)
            sc = smallc
            cos4 = cosb[:, :].unsqueeze(1).to_broadcast([64, 4, T])
            sin4 = sinb[:, :].unsqueeze(1).to_broadcast([64, 4, T])
            for g in range(2):
                w = wload(("swa", g)).rearrange("p (k c) -> p k c", c=384)
                for q4 in range(4):
                    pq = nb()
                    for k in range(8):
                        P.mm(pq[0:64, :], w[:, k, 64 * q4:64 * q4 + 64], xn[:, k, :], start=(k == 0), stop=(k == 7))
                    P.copy("act", qraw[:, q4, :], pq[0:64, :])
                pk = nb()
                for k in range(8):
                    P.mm(pk[0:64, :], w[:, k, 256:320], xn[:, k, :], start=(k == 0), stop=(k == 7))
                P.copy("act", kraw, pk[0:64, :])
                for b in range(4):
                    pv = nb()
                    for k in range(8):
                        P.mm(pv[:, 0:64], xn[:, k, 128 * b:128 * b + 128], w[:, k, 320:384], start=(k == 0), stop=(k == 7))
                    P.copy("act", vswa[:, 1 + b, 64 * g:64 * g + 64], pv[:, 0:64])
                P.tt("dve", ra[:], qraw[:], cos4, ALU.mult)
                P.tt("dve", rb[0:32], qraw[32:64], sin4[32:64], ALU.mult)
                P.tt("dve", rb[32:64], qraw[0:32], sin4[0:32], ALU.mult)
                P.tt("dve", qT[0:32], ra[0:32], rb[0:32], ALU.subtract)
                P.tt("dve", qT[32:64], ra[32:64], rb[32:64], ALU.add)
                ra1 = ra[:, 0, :]
                rb1 = rb[:, 0, :]
                P.tt("dve", ra1, kraw, cosb[:], ALU.mult)
                P.tt("dve", rb1[0:32], kraw[32:64], sinb[32:64], ALU.mult)
                P.tt("dve", rb1[32:64], kraw[0:32], sinb[0:32], ALU.mult)
                P.tt("dve", kT[0:32, g, 128:640], ra1[0:32], rb1[0:32], ALU.subtract)
                P.tt("dve", kT[32:64, g, 128:640], ra1[32:64], rb1[32:64], ALU.add)
                for b in range(4):
                    mask = cc("swam0") if (t == 0 and b == 0) else cc("swam")
                    for q4 in range(4):
                        hq = 4 * g + q4
                        sink = cc("sinks", hq, 1)
                        ps_ = nb()
                        P.mm(ps_[:, 0:256], qT[:, q4, 128 * b:128 * b + 128], kT[:, g, 128 * b:128 * b + 256])
                        P.stt("dve", smx, ps_[:, 0:256], 0.125, mask, ALU.mult, ALU.add)
                        P.emit("dve", lambda e, o=sc[:, 0:1], i=smx: e.reduce_max(o, i, AX.X),
                               reads=[smx], writes=[sc[:, 0:1]])
                        P.ts("dve", sc[:, 1:2], sc[:, 0:1], sink, ALU.max, -1.0, ALU.mult)
                        P.memset("dve", sc[:, 2:3], 0.0)
                        P.actf(pexp, smx, AF.Exp, bias=sc[:, 1:2], accum_out=sc[:, 2:3])
                        P.actf(sc[:, 3:4], sink, AF.Exp, bias=sc[:, 1:2])
                        P.tt("dve", sc[:, 4:5], sc[:, 2:3], sc[:, 3:4], ALU.add)
                        P.emit("dve", lambda e, o=sc[:, 5:6], i=sc[:, 4:5]: e.reciprocal(o, i),
                               reads=[sc[:, 4:5]], writes=[sc[:, 5:6]])
                        P.ts("dve", pnb, pexp, sc[:, 5:6], ALU.mult)
                        pt_ = nb()
                        ptb = pt_[:].bitcast(BF16)
                        for j in range(2):
                            P.tr(ptb[:, 128 * j:128 * j + 128], pnb[:, 128 * j:128 * j + 128], identb[:])
                        P.copy("act", pTt.rearrange("p a b -> p (a b)"), ptb[:, 0:256])
                        po = nb()
                        for j in range(2):
                            P.mm(po[0:64, 0:128], vswa[:, b + j, 64 * g:64 * g + 64], pTt[:, j, :], start=(j == 0), stop=(j == 1))
                        P.copy("act", catB[:, hq, 128 * b:128 * b + 128], po[0:64, 0:128])
                P.copy("dve", kT[:, g, 0:128], kT[:, g, 512:640])
            P.copy("dve", vswa[:, 0, :], vswa[:, 4, :])
            for u in range(4):
                w = wload(("ewout", u))
                wa = w[:, 0:1024].rearrange("p (k c) -> p k c", c=256)
                wb_ = w[0:64, 1024:3072].rearrange("p (k c) -> p k c", c=256)
                for o2 in range(2):
                    o = 2 * u + o2
                    py = nb()
                    for hh in range(4):
                        P.mm(py[:], wa[:, hh, 128 * o2:128 * o2 + 128], catA[:, hh, :], start=(hh == 0), stop=False)
                    for hq in range(8):
                        P.mm(py[:], wb_[:, hq, 128 * o2:128 * o2 + 128], catB[:, hq, :], start=False, stop=(hq == 7))
                    evac_y(py[:], o)
            postnorm_res(l, 3, False)

        def odd_mixer(t):
            l = 1
            prenorm(l, 2)
            cat = av(0, 16 * K, BF16, inner=T)
            qraw = av(16 * K, 4 * K, F32, inner=T)
            kraw = av(20 * K, 4 * K, F32, inner=T)
            ta = av(24 * K, 4 * K, F32, inner=T)
            tb = av(28 * K, 4 * K, F32, inner=T)
            rr = av(32 * K, 4 * K, F32, inner=T)
            qr = av(36 * K, 2 * K, BF16, inner=T)
            qin = av(38 * K, 2 * K, BF16, inner=T)
            krb = av(40 * K, 2 * K, BF16, inner=T)
            kinT = av(42 * K, 2 * K, BF16, inner=T)
            ktok = av(44 * K, 2 * K, BF16, inner=256)
            vtok = av(46 * K, 4 * K, BF16, inner=512)
            gs = av(50 * K, 4 * K, BF16, inner=T)
            obuf = av(54 * K, 8 * K, F32, inner=T)
            attb = av(62 * K, 512, BF16, inner=128)
            e2 = av(63 * K, 2 * K, F32)
            cos2 = cosc[:, :].unsqueeze(1).to_broadcast([128, 2, T])
            sin2 = sinc[:, :].unsqueeze(1).to_broadcast([128, 2, T])

            def rope(raw):
                P.tt("dve", ta[:], raw[:], cos2, ALU.mult)
                P.tt("dve", tb[:], raw[:], sin2, ALU.mult)
                P.tt("dve", rr[:, 0, :], ta[:, 0, :], tb[:, 1, :], ALU.subtract)
                P.tt("dve", rr[:, 1, :], ta[:, 1, :], tb[:, 0, :], ALU.add)

            for hh in range(4):
                gq4 = cc("gq", 128 * hh, 128).unsqueeze(1).to_broadcast([128, 4, 128])
                gk4 = cc("gk", 128 * hh, 128).unsqueeze(1).to_broadcast([128, 4, 128])
                w = wload(("oq", hh)).rearrange("p (k c) -> p k c", c=256)
                for hf in range(2):
                    pq = nb()
                    for k in range(8):
                        P.mm(pq[:], w[:, k, 128 * hf:128 * hf + 128], xn[:, k, :], start=(k == 0), stop=(k == 7))
                    P.copy("act", qraw[:, hf, :], pq[:])
                rope(qraw)
                P.copy("act", qr[:], rr[:])
                for hf in range(2):
                    P.tt("dve", qin[:, hf, :].rearrange("p (c t) -> p c t", t=128),
                         rr[:, hf, :].rearrange("p (c t) -> p c t", t=128), gq4, ALU.mult)
                w = wload(("ok", hh)).rearrange("p (k c) -> p k c", c=256)
                for hf in range(2):
                    pk = nb()
                    for k in range(8):
                        P.mm(pk[:], w[:, k, 128 * hf:128 * hf + 128], xn[:, k, :], start=(k == 0), stop=(k == 7))
                    P.copy("act", kraw[:, hf, :], pk[:])
                rope(kraw)
                P.ts("dve", rr[:], rr[:], 1.0 / 16.0, ALU.mult)
                P.copy("act", krb[:], rr[:])
                for hf in range(2):
                    P.tt("dve", kinT[:, hf, :].rearrange("p (c t) -> p c t", t=128),
                         rr[:, hf, :].rearrange("p (c t) -> p c t", t=128), gk4, ALU.mult)
                pt_ = nb()
                ptb = pt_[:].bitcast(BF16)
                for c in range(4):
                    for hf in range(2):
                        P.tr(ptb[:, 256 * c + 128 * hf:256 * c + 128 * hf + 128], kinT[:, hf, 128 * c:128 * c + 128], identb[:])
                P.copy("act", ktok.rearrange("p a b -> p (a b)"), ptb[:])
                w = wload(("ov", hh)).rearrange("p (k c) -> p k c", c=512)
                for c in range(4):
                    pv = nb()
                    for k in range(8):
                        P.mm(pv[:], xn[:, k, 128 * c:128 * c + 128], w[:, k, :], start=(k == 0), stop=(k == 7))
                    P.copy("act", vtok[:, c, :], pv[:])
                w = wload(("og", hh)).rearrange("p (k c) -> p k c", c=512)
                for vc in range(4):
                    pg = nb()
                    for k in range(8):
                        P.mm(pg[:], w[:, k, 128 * vc:128 * vc + 128], xn[:, k, :], start=(k == 0), stop=(k == 7))
                    P.actf(gs[:, vc, :], pg[:], AF.Silu)
                dT = cc("decayT", 128 * hh, 128)
                for c in range(4):
                    cs_ = slice(128 * c, 128 * c + 128)
                    r = c % 2
                    pa = nb()
                    for hf in range(2):
                        P.mm(pa[:, 0:128], krb[:, hf, cs_], qr[:, hf, cs_], start=(hf == 0), stop=(hf == 1))
                    P.tt("dve", attb[:, r, :], pa[:, 0:128], dT, ALU.mult)
                    po = nb()
                    for vc in range(4):
                        vs = slice(128 * vc, 128 * vc + 128)
                        P.mm(po[:, vs], vtok[:, c, vs], attb[:, r, :], start=True, stop=False)
                        for hf in range(2):
                            P.mm(po[:, vs], retSb[:, 2 * hh + hf, vs], qin[:, hf, cs_], start=False, stop=(hf == 1))
                    P.copy("act", obuf[:, :, cs_], po[:].rearrange("p (a b) -> p a b", b=128))
                    for hf in range(2):
                        pu_ = nb()
                        P.mm(pu_[:], ktok[:, c, 128 * hf:128 * hf + 128], vtok[:, c, :])
                        P.stt("dve", retS[:, 2 * hh + hf, :], retS[:, 2 * hh + hf, :], GAMMA128[hh], pu_[:], ALU.mult, ALU.add)
                        P.copy("act", retSb[:, 2 * hh + hf, :], retS[:, 2 * hh + hf, :])
                pn_ = nb()
                for vc in range(4):
                    P.actf(sqb[:, vc % 2, :], obuf[:, vc, :], AF.Square)
                    P.mm(pn_[:], onesb[:], sqb[:, vc % 2, :], start=(vc == 0), stop=(vc == 3))
                rstd_from_ss(pn_[:], e2, 1.0 / 512)
                for vc in range(4):
                    P.stt("dve", obuf[:, vc, :], obuf[:, vc, :], cc("ogr", vc, 1), e2, ALU.mult, ALU.mult)
                    P.tt("dve", cat[:, 4 * hh + vc, :], obuf[:, vc, :], gs[:, vc, :], ALU.mult)
            for u in range(4):
                w = wload(("owout", u)).rearrange("p (k c) -> p k c", c=256)
                for o2 in range(2):
                    o = 2 * u + o2
                    py = nb()
                    for j in range(16):
                        P.mm(py[:], w[:, j, 128 * o2:128 * o2 + 128], cat[:, j, :], start=(j == 0), stop=(j == 15))
                    evac_y(py[:], o)
            postnorm_res(l, 3, False)

        for t in range(NT):
            P.epoch = t // EP_TILES
            load_x(t)
            trig_tables(t)
            stage_fns = [lambda: ffn(0, 1), lambda: even_mixer(t), lambda: ffn(0, 2), lambda: ple(0, t),
                         lambda: ffn(1, 1), lambda: odd_mixer(t), lambda: ffn(1, 2), lambda: ple(1, t)]
            for f in stage_fns[:nstage]:
                f()
            store_out(t)
        stats = P.finalize(sems, blk)
    return nc, stats


_NC_CACHE = {}


def kernel(**inputs):
    NT = inputs["x"].shape[1] // T
    if NT not in _NC_CACHE:
        _NC_CACHE[NT] = build_nc(NT)[0]
    nc = _NC_CACHE[NT]
    wall = pack_weights(inputs)
    cst = pack_consts(inputs)
    x = np.ascontiguousarray(inputs["x"], dtype=np.float32)
    p = np.ascontiguousarray(inputs["p"], dtype=np.float32)
    pos = np.ascontiguousarray(inputs["positions"], dtype=np.int32)
    in_maps = []
    for c in range(8):
        in_maps.append({"x": x[c], "p": np.ascontiguousarray(p[:, c]), "pos": pos[c:c + 1],
                        "wall": wall, "cst": cst})
    res = run_bass_kernel_spmd(nc, in_maps, core_ids=list(range(8)))
    return np.stack([r["out"] for r in res.results], axis=0)
```

```python
from contextlib import ExitStack
import numpy as np
import concourse.bass as bass
import concourse.mybir as mybir
from concourse.bass_utils import run_bass_kernel_spmd

F32 = mybir.dt.float32
BF16 = mybir.dt.bfloat16
I32 = mybir.dt.int32
AF = mybir.ActivationFunctionType
ALU = mybir.AluOpType
AX = mybir.AxisListType

_DSZ = {F32: 4, BF16: 2, I32: 4}
COMPUTE = ("pe", "act", "dve", "pool")
SAME_ENGINE_SYNC = True


def _rng(ap):
    sz = _DSZ[ap.dtype]
    steps = ap.ap
    off = int(ap.offset)
    if str(ap.space) == "DRAM":
        ext = sum((c - 1) * s for s, c in steps)
        return (ap.name, off * sz, (off + ext + 1) * sz)
    if str(ap.space) == "PSUM":
        return (ap.name, 0, 2048)
    pstep = steps[0][0]
    lo = off % pstep if pstep > 0 else off
    ext = sum((c - 1) * s for s, c in steps[1:])
    return (ap.name, lo * sz, (lo + ext + 1) * sz)


class Op:
    __slots__ = ("eng", "fn", "deps", "sem", "cnt", "needs_inc", "inc_idx", "is_dma", "seq", "epoch")

    def __init__(self, eng, fn, is_dma=False):
        self.seq = 0
        self.epoch = 0
        self.eng = eng
        self.fn = fn
        self.deps = set()
        self.sem = None
        self.cnt = 0
        self.needs_inc = False
        self.inc_idx = 0
        self.is_dma = is_dma


class DmaSem:
    def __init__(self, handle):
        self.h = handle
        self.n = 0
        self.last = None


class Prog:
    def __init__(self, nc):
        self.nc = nc
        self.streams = {e: [] for e in ("pe", "act", "dve", "pool", "sp")}
        self.acc = {}
        self.dma_sems = []
        self.epoch = 0

    def _add(self, op):
        op.seq = len(self.streams[op.eng])
        op.epoch = self.epoch
        last = {}
        keep = set()
        for d in op.deps:
            if d.is_dma:
                keep.add(d)
            elif d.eng not in last or d.seq > last[d.eng].seq:
                last[d.eng] = d
        op.deps = keep | set(last.values())
        self.streams[op.eng].append(op)

    def new_dma_sem(self, stack, name):
        s = DmaSem(stack.enter_context(self.nc.semaphore(name)))
        self.dma_sems.append(s)
        return s

    def _track(self, op, reads, writes):
        writes = list(writes) + [ap for ap in reads if str(ap.space) == "PSUM"]
        reads = [ap for ap in reads if str(ap.space) != "PSUM"]
        for ap in reads:
            n, lo, hi = _rng(ap)
            a = self.acc.setdefault(n, ([], []))
            for (l, h, o) in a[0]:
                if l < hi and lo < h:
                    op.deps.add(o)
            a[1].append((lo, hi, op))
        for ap in writes:
            n, lo, hi = _rng(ap)
            a = self.acc.setdefault(n, ([], []))
            for (l, h, o) in a[0]:
                if l < hi and lo < h and o is not op:
                    op.deps.add(o)
            for (l, h, o) in a[1]:
                if l < hi and lo < h and o is not op:
                    op.deps.add(o)
            a[0][:] = [t for t in a[0] if not (lo <= t[0] and t[1] <= hi)]
            a[1][:] = [t for t in a[1] if not (lo <= t[0] and t[1] <= hi)]
            a[0].append((lo, hi, op))

    def emit(self, eng, fn, reads=(), writes=()):
        op = Op(eng, fn)
        self._track(op, reads, writes)
        self._add(op)
        return op

    def dma(self, queue, out, in_, sem):
        op = Op(queue, lambda e: e.dma_start(out=out, in_=in_), is_dma=True)
        if sem.last is not None:
            op.deps.add(sem.last)
        sem.n += 1
        sem.last = op
        op.sem = sem
        op.cnt = sem.n
        self._track(op, [in_], [out])
        self._add(op)
        return op

    def mm(self, out, lhsT, rhs, start=True, stop=True):
        return self.emit("pe", lambda e: e.matmul(out, lhsT, rhs, start=start, stop=stop),
                         reads=[lhsT, rhs], writes=[out])

    def tr(self, out, in_, ident):
        return self.emit("pe", lambda e: e.transpose(out, in_, ident), reads=[in_, ident], writes=[out])

    def actf(self, out, in_, func, bias=None, scale=None, accum_out=None):
        kw = {}
        rd = [in_]
        wr = [out]
        if bias is not None:
            kw["bias"] = bias
            if not isinstance(bias, (int, float)):
                rd.append(bias)
        if scale is not None:
            kw["scale"] = scale
            if not isinstance(scale, (int, float)):
                rd.append(scale)
        if accum_out is not None:
            kw["accum_out"] = accum_out
            wr.append(accum_out)
        return self.emit("act", lambda e: e.activation(out, in_, func, **kw), reads=rd, writes=wr)

    def tt(self, eng, out, in0, in1, op):
        return self.emit(eng, lambda e: e.tensor_tensor(out, in0, in1, op), reads=[in0, in1], writes=[out])

    def ts(self, eng, out, in0, s1, op0, s2=None, op1=None):
        rd = [in0] + [s for s in (s1, s2) if s is not None and not isinstance(s, (int, float))]
        kw = {}
        if op1 is not None:
            kw["op1"] = op1
        return self.emit(eng, lambda e: e.tensor_scalar(out, in0, s1, s2, op0, **kw), reads=rd, writes=[out])

    def stt(self, eng, out, in0, scalar, in1, op0, op1):
        rd = [in0, in1] + ([scalar] if not isinstance(scalar, (int, float)) else [])
        return self.emit(eng, lambda e: e.scalar_tensor_tensor(out, in0, scalar, in1, op0, op1),
                         reads=rd, writes=[out])

    def copy(self, eng, out, in_):
        if eng == "act":
            return self.emit("act", lambda e: e.copy(out, in_), reads=[in_], writes=[out])
        return self.emit(eng, lambda e: e.tensor_copy(out, in_), reads=[in_], writes=[out])

    def memset(self, eng, ap, val):
        return self.emit(eng, lambda e: e.memset(ap, val), writes=[ap])

    def finalize(self, sems, block):
        def needs_sem(op, d):
            if d.is_dma:
                return True
            if d.eng == op.eng and not op.is_dma:
                if d.eng == "pe" or not SAME_ENGINE_SYNC:
                    return False
            return True

        for st in self.streams.values():
            for op in st:
                for d in op.deps:
                    if not d.is_dma and needs_sem(op, d):
                        d.needs_inc = True
        self.max_inc = {}
        for eng in COMPUTE:
            k = {}
            for op in self.streams[eng]:
                if not op.is_dma and op.needs_inc:
                    k[op.epoch] = k.get(op.epoch, 0) + 1
                    op.inc_idx = k[op.epoch]
            self.max_inc[eng] = max(list(k.values()) + [0])
        stats = {}
        nc = self.nc
        engobj = {"pe": nc.tensor, "act": nc.scalar, "dve": nc.vector, "pool": nc.gpsimd, "sp": nc.sync}

        def run(eng):
            e = engobj[eng]
            waited = {}
            nw = 0
            for op in self.streams[eng]:
                need = {}
                for d in op.deps:
                    if not needs_sem(op, d):
                        continue
                    if d.is_dma:
                        key, v, h = ("d", id(d.sem)), d.cnt * 16, d.sem.h
                    else:
                        key, v, h = ("c", d.eng, d.epoch), d.inc_idx, sems[d.eng][d.epoch]
                    if v > need.get(key, (0, None))[0]:
                        need[key] = (v, h)
                for key, (v, h) in need.items():
                    if v > waited.get(key, 0):
                        e.wait_ge(h, v)
                        waited[key] = v
                        nw += 1
                ins = op.fn(e)
                if op.is_dma:
                    ins.then_inc(op.sem.h, 16)
                elif op.needs_inc:
                    ins.then_inc(sems[eng][op.epoch], 1)
            if eng == "sp":
                for s in self.dma_sems:
                    if s.n:
                        e.wait_ge(s.h, s.n * 16)
            stats[eng] = (len(self.streams[eng]), nw)

        block.tensor(lambda _e: run("pe"))
        block.scalar(lambda _e: run("act"))
        block.vector(lambda _e: run("dve"))
        block.gpsimd(lambda _e: run("pool"))
        block.sync(lambda _e: run("sp"))
        return stats


D = 1024
T = 512
DFF = 2816
EPS = 1e-6
NJ = DFF // 128
R_SLOTS = 4
EP_TILES = 2
WCOLS = 4096


class Cols:
    def __init__(self):
        self.n = 0
        self.d = {}

    def add(self, name, w):
        self.d[name] = (self.n, w)
        self.n += w


CST = Cols()
for _n, _w in (("gT", 112), ("lbT", 8), ("ogh", 1), ("ogr", 4), ("sinks", 8), ("invb", 1), ("invc", 1),
               ("ident", 128), ("maskU", 64), ("scanmask", 512), ("swam", 256), ("swam0", 256),
               ("decayT", 512), ("gq", 512), ("gk", 512)):
    CST.add(_n, _w)
NCST = CST.n


def unit_list():
    u = []
    for l in range(2):
        u += [(("ffn1_in", l, i), 4096) for i in range(11)]
        u += [(("ffn1_out", l, i), 2816) for i in range(8)]
        if l == 0:
            u += [(("hg", i), 4096) for i in range(4)]
            u += [(("swa", i), 3072) for i in range(2)]
            u += [(("ewout", i), 3072) for i in range(4)]
        else:
            for hh in range(4):
                u += [(("oq", hh), 2048), (("ok", hh), 2048), (("ov", hh), 4096), (("og", hh), 4096)]
            u += [(("owout", i), 4096) for i in range(4)]
        u += [(("ffn2_in", l, i), 4096) for i in range(11)]
        u += [(("ffn2_out", l, i), 2816) for i in range(8)]
        u += [(("pproj", l), 2048), (("pgate", l, 0), 4096), (("pgate", l, 1), 4096)]
    return u


UNITS = unit_list()
NU = len(UNITS)
UIDX = {k: i for i, (k, _) in enumerate(UNITS)}


def _kmaj(W, cols):
    kc = W.shape[0] // 128
    return np.ascontiguousarray(W.reshape(kc, 128, W.shape[1])[:, :, cols].transpose(1, 0, 2)).reshape(128, -1)


def pack_weights(inp):
    wall = np.zeros((NU, 128, WCOLS), np.float32)
    ar = np.arange
    for (key, n) in UNITS:
        i = UIDX[key]
        k0 = key[0]
        if k0 in ("ffn1_in", "ffn2_in"):
            W = inp[k0[:4] + "_w_in"][key[1]]
            u = key[2]
            cols = np.concatenate([ar(256 * u, 256 * u + 256), DFF + ar(256 * u, 256 * u + 256)])
            blk = _kmaj(W, cols)
        elif k0 in ("ffn1_out", "ffn2_out"):
            W = inp[k0[:4] + "_w_out"][key[1]]
            blk = _kmaj(W, ar(128 * key[2], 128 * key[2] + 128))
        elif k0 == "hg":
            W = inp["even_w_in"][0]
            hh = key[1]
            cols = np.concatenate([g * 512 + hh * 128 + ar(128) for g in range(4)])
            blk = _kmaj(W, cols)
        elif k0 == "swa":
            W = inp["even_w_in"][0]
            g = key[1]
            cols = np.concatenate([2048 + 256 * g + ar(256), 2560 + 64 * g + ar(64), 2688 + 64 * g + ar(64)])
            blk = _kmaj(W, cols)
        elif k0 == "ewout":
            W = inp["even_w_out"][0]
            cols = ar(256 * key[1], 256 * key[1] + 256)
            a = _kmaj(W[0:512], cols)
            b = np.zeros((128, 8, 256), np.float32)
            b[0:64] = W[512:1024].reshape(8, 64, 1024)[:, :, cols].transpose(1, 0, 2)
            blk = np.concatenate([a, b.reshape(128, -1)], axis=1)
        elif k0 in ("oq", "ok", "ov", "og"):
            W = inp["odd_w_in"][0]
            hh = key[1]
            base = {"oq": 0, "ok": 1024, "ov": 2048, "og": 4096}[k0]
            wd = 256 if k0 in ("oq", "ok") else 512
            blk = _kmaj(W, base + hh * wd + ar(wd))
        elif k0 == "owout":
            W = inp["odd_w_out"][0]
            blk = _kmaj(W, ar(256 * key[1], 256 * key[1] + 256))
        elif k0 == "pproj":
            blk = _kmaj(inp["ple_w_proj"][key[1]], ar(1024))
        elif k0 == "pgate":
            blk = _kmaj(inp["ple_w_gate"][key[1]], ar(512 * key[2], 512 * key[2] + 512))
        assert blk.shape == (128, n), (key, blk.shape, n)
        wall[i, :, :n] = blk
    return wall


def pack_consts(inp):
    c = np.zeros((128, NCST), np.float32)

    def put(name, arr):
        o, w = CST.d[name]
        arr = np.asarray(arr, np.float32)
        c[: arr.shape[0], o:o + w] = arr.reshape(arr.shape[0], w)

    put("gT", inp["norm_g"].reshape(2, 7, 8, 128).transpose(3, 0, 1, 2).reshape(128, 112))
    put("lbT", inp["hgrn_lb"].reshape(2, 4, 128).transpose(2, 0, 1).reshape(128, 8))
    put("ogh", inp["hgrn_onorm_g"].reshape(128, 1))
    put("ogr", inp["ret_onorm_g"].reshape(4, 128).T)
    put("sinks", np.broadcast_to(inp["attn_sinks"].reshape(1, 8), (128, 8)))
    inv_b = (np.float32(10000.0) ** (-np.arange(0, 64, 2, dtype=np.float32) / np.float32(64))).astype(np.float32)
    inv_c = (np.float32(10000.0) ** (-np.linspace(0.0, 1.0, 128, dtype=np.float32))).astype(np.float32)
    put("invb", np.concatenate([inv_b, inv_b]).reshape(64, 1))
    put("invc", inv_c.reshape(128, 1))
    put("ident", np.eye(128, dtype=np.float32))
    s = np.arange(64)
    put("maskU", (s[:, None] <= s[None, :]).astype(np.float32))
    sm = np.ones(512, np.float32)
    sm[::64] = 0.0
    put("scanmask", np.broadcast_to(sm, (128, 512)))
    qi = np.arange(128)[:, None]
    kj = np.arange(256)[None, :]
    diff = qi + 128 - kj
    valid = (diff >= 0) & (diff < 128)
    put("swam", np.where(valid, 0.0, -30000.0))
    put("swam0", np.where(valid & (kj >= 128), 0.0, -30000.0))
    hh = np.arange(4, dtype=np.float64)
    lg = np.log1p(-np.exp2(-5.0 - hh))
    idx = np.arange(128, dtype=np.float64)
    dcs = idx[None, :] - idx[:, None]
    dec = np.where(dcs[None] >= 0, np.exp(np.maximum(dcs, 0.0)[None] * lg[:, None, None]), 0.0)
    put("decayT", dec.transpose(1, 0, 2).reshape(128, 512))
    put("gq", np.broadcast_to(np.exp((idx + 1.0)[None, :] * lg[:, None]).reshape(1, 512), (128, 512)))
    put("gk", np.broadcast_to(np.exp((127.0 - idx)[None, :] * lg[:, None]).reshape(1, 512), (128, 512)))
    return c


GAMMA128 = [float(np.exp(128.0 * np.log1p(-np.exp2(-5.0 - h)))) for h in range(4)]


def build_nc(NT, nstage=8):
    S = NT * T
    nc = bass.Bass("TRN2", target_bir_lowering=False)
    x_d = nc.dram_tensor("x", [S, D], F32, kind="ExternalInput").ap()
    p_d = nc.dram_tensor("p", [2, S, 256], F32, kind="ExternalInput").ap()
    pos_d = nc.dram_tensor("pos", [1, S], I32, kind="ExternalInput").ap()
    wall_d = nc.dram_tensor("wall", [NU, 128, WCOLS], F32, kind="ExternalInput").ap()
    cst_d = nc.dram_tensor("cst", [128, NCST], F32, kind="ExternalInput").ap()
    out_d = nc.dram_tensor("out", [S, D], F32, kind="ExternalOutput").ap()
    wbf_d = nc.dram_tensor("wbf", [NU, 128, WCOLS], BF16, kind="Internal").ap()

    with ExitStack() as st:
        P = Prog(nc)
        sb = lambda n, s, d: st.enter_context(nc.sbuf_tensor(n, s, d))
        h = sb("h", [128, 8, T], F32)
        xn = sb("xn", [128, 8, T], BF16)
        ybuf = sb("ybuf", [128, 8, T], F32)
        sqb = sb("sqb", [128, 2, T], BF16)
        rstd = sb("rstd", [128, T], F32)
        wring = sb("wring", [128, R_SLOTS, WCOLS], BF16)
        ptile = sb("ptile", [128, 4, 256], F32)
        pTb = sb("pTb", [128, 2, T], BF16)
        cosb = sb("cosb", [64, T], F32)
        sinb = sb("sinb", [64, T], F32)
        cosc = sb("cosc", [128, T], F32)
        sinc = sb("sinc", [128, T], F32)
        retS = sb("retS", [128, 8, 512], F32)
        retSb = sb("retSb", [128, 8, 512], BF16)
        hgS = sb("hgS", [128, 4, 128], F32)
        hgSb = sb("hgSb", [128, 4, 128], BF16)
        kT = sb("kT", [64, 2, 640], BF16)
        vswa = sb("vswa", [128, 5, 128], BF16)
        csb = sb("csb", [128, NCST], F32)
        gsc = sb("gsc", [128, 112], F32)
        lbs = sb("lbs", [128, 12], F32)
        identb = sb("identb", [128, 128], BF16)
        onesb = sb("onesb", [128, 128], BF16)
        smallc = sb("smallc", [128, 16], F32)
        ARENA = 72 * 1024
        arena = sb("arena", [128, ARENA // 2], BF16)
        pb = [st.enter_context(nc.psum_tensor(f"pb{i}", [128, 512], F32)) for i in range(8)]
        NEP = (NT + EP_TILES - 1) // EP_TILES
        sems = {e: [st.enter_context(nc.semaphore(f"s_{e}{i}")) for i in range(NEP)] for e in COMPUTE}
        wsem_all = [[P.new_dma_sem(st, f"w{j}_{i}") for i in range(R_SLOTS)] for j in range(NEP)]
        csem = [P.new_dma_sem(st, f"c{i}") for i in range(8)]
        xsem = P.new_dma_sem(st, "xs")
        osem = P.new_dma_sem(st, "os")
        psem = P.new_dma_sem(st, "ps")
        qsem = P.new_dma_sem(st, "qs")
        ksem = P.new_dma_sem(st, "ks")
        blk = st.enter_context(nc.Block())

        def av(lo, nbytes, dt, inner=None, parts=128):
            a = arena[0:parts, lo // 2:(lo + nbytes) // 2]
            if dt != BF16:
                a = a.bitcast(dt)
            if inner is not None:
                a = a.rearrange("p (a b) -> p a b", b=inner)
            return a

        K = 1024

        def cc(name, lo=0, n=None, parts=128):
            o, w = CST.d[name]
            n = w - lo if n is None else n
            return csb[0:parts, o + lo:o + lo + n]

        identf = cc("ident")
        bank_ctr = [0]

        def nb():
            b = pb[bank_ctr[0] % 7]
            bank_ctr[0] += 1
            return b

        ss_bank = pb[7]

        wctr = [0]

        def wload(key):
            i = UIDX[key]
            n = UNITS[i][1]
            s = wctr[0] % R_SLOTS
            wctr[0] += 1
            P.dma("sp", wring[:, s, 0:n], wbf_d[i, :, 0:n], wsem_all[P.epoch][s])
            return wring[:, s, 0:n]

        P.dma("sp", csb[:], cst_d, ksem)
        for i, (key, n) in enumerate(UNITS):
            P.dma("pool", wbf_d[i, :, 0:n], wall_d[i, :, 0:n], csem[i % 8])
        P.copy("dve", identb[:], identf)
        P.memset("dve", onesb[:], 1.0)
        P.ts("dve", gsc[:], cc("gT"), 0.5, ALU.mult)
        P.tt("dve", lbs[:, 0:4], cc("lbT", 0, 4), cc("lbT", 4, 4), ALU.subtract)
        P.actf(lbs[:, 4:8], lbs[:, 0:4], AF.Sigmoid)
        P.ts("dve", lbs[:, 8:12], lbs[:, 4:8], -1.0, ALU.mult, 1.0, ALU.add)
        for tns in (retS, retSb, hgS, hgSb, vswa):
            P.memset("dve", tns[:], 0.0)
        P.memset("dve", kT[:], 0.0)

        def gcol(l, n, k, scaled=False):
            c = (l * 7 + n) * 8 + k
            return gsc[:, c:c + 1] if scaled else cc("gT", c, 1)

        def rstd_from_ss(ss_ap, out_ap, inv_n):
            P.actf(out_ap, ss_ap, AF.Ln, bias=EPS, scale=inv_n)
            P.actf(out_ap, out_ap, AF.Exp, scale=-0.5)

        def prenorm(l, n):
            for k in range(8):
                P.actf(sqb[:, k % 2, :], h[:, k, :], AF.Square)
                P.mm(ss_bank[:], onesb[:], sqb[:, k % 2, :], start=(k == 0), stop=(k == 7))
            rstd_from_ss(ss_bank[:], rstd[:], 1.0 / D)
            for k in range(8):
                P.stt("dve", xn[:, k, :], h[:, k, :], gcol(l, n, k), rstd[:], ALU.mult, ALU.mult)

        def evac_y(py, o):
            P.copy("dve", ybuf[:, o, :], py)
            P.actf(sqb[:, o % 2, :], ybuf[:, o, :], AF.Square)
            P.mm(ss_bank[:], onesb[:], sqb[:, o % 2, :], start=(o == 0), stop=(o == 7))

        def postnorm_res(l, n, half):
            rstd_from_ss(ss_bank[:], rstd[:], 1.0 / D)
            for k in range(8):
                P.tt("dve" if k % 2 else "pool", ybuf[:, k, :], ybuf[:, k, :], rstd[:], ALU.mult)
                P.stt("dve", h[:, k, :], ybuf[:, k, :], gcol(l, n, k, scaled=half), h[:, k, :], ALU.mult, ALU.add)

        def ffn(l, which):
            n0 = 0 if which == 1 else 4
            hid = av(0, 22 * K, BF16, inner=T)
            sg = av(22 * K, 4 * K, F32, inner=T)
            prenorm(l, n0)
            for u in range(11):
                w = wload((f"ffn{which}_in", l, u)).rearrange("p (k c) -> p k c", c=512)
                for jj in range(2):
                    j = 2 * u + jj
                    pg = nb()
                    pu = nb()
                    for k in range(8):
                        P.mm(pg[:], w[:, k, 128 * jj:128 * jj + 128], xn[:, k, :], start=(k == 0), stop=(k == 7))
                    for k in range(8):
                        P.mm(pu[:], w[:, k, 256 + 128 * jj:256 + 128 * jj + 128], xn[:, k, :], start=(k == 0), stop=(k == 7))
                    P.actf(sg[:, j % 2, :], pg[:], AF.Silu)
                    P.tt("dve", hid[:, j, :], sg[:, j % 2, :], pu[:], ALU.mult)
            for o in range(8):
                w = wload((f"ffn{which}_out", l, o)).rearrange("p (k c) -> p k c", c=128)
                py = nb()
                for j in range(NJ):
                    P.mm(py[:], w[:, j, :], hid[:, j, :], start=(j == 0), stop=(j == NJ - 1))
                evac_y(py[:], o)
            postnorm_res(l, n0 + 1, True)

        def ple(l, t):
            for k in range(8):
                P.copy("act" if k % 2 else "dve", xn[:, k, :], h[:, k, :])
            P.dma("sp", ptile[:], p_d[l, t * T:(t + 1) * T, :].rearrange("(b p) f -> p b f", p=128), psem)
            for c in range(2):
                bk = nb()
                for b in range(4):
                    P.tr(bk[:, 128 * b:128 * b + 128], ptile[:, b, 128 * c:128 * c + 128], identf)
                P.copy("act", pTb[:, c, :], bk[:])
            w = wload(("pproj", l)).rearrange("p (k c) -> p k c", c=1024)
            for o in range(8):
                bk = nb()
                for c in range(2):
                    P.mm(bk[:], w[:, c, 128 * o:128 * o + 128], pTb[:, c, :], start=(c == 0), stop=(c == 1))
                P.copy("dve", ybuf[:, o, :], bk[:])
            sg = av(22 * K, 4 * K, F32, inner=T)
            for u in range(2):
                w = wload(("pgate", l, u)).rearrange("p (k c) -> p k c", c=512)
                for o4 in range(4):
                    o = 4 * u + o4
                    bk = nb()
                    for k in range(8):
                        P.mm(bk[:], w[:, k, 128 * o4:128 * o4 + 128], xn[:, k, :], start=(k == 0), stop=(k == 7))
                    P.actf(sg[:, o % 2, :], bk[:], AF.Sigmoid)
                    P.tt("dve", ybuf[:, o, :], ybuf[:, o, :], sg[:, o % 2, :], ALU.mult)
                    P.actf(sqb[:, o % 2, :], ybuf[:, o, :], AF.Square)
                    P.mm(ss_bank[:], onesb[:], sqb[:, o % 2, :], start=(o == 0), stop=(o == 7))
            postnorm_res(l, 6, False)

        xio = av(0, 16 * K, F32, inner=D)

        def load_x(t):
            P.dma("sp", xio, x_d[t * T:(t + 1) * T, :].rearrange("(b p) f -> p b f", p=128), xsem)
            for k in range(8):
                bk = nb()
                for b in range(4):
                    P.tr(bk[:, 128 * b:128 * b + 128], xio[:, b, 128 * k:128 * k + 128], identf)
                P.copy("act" if k % 2 else "dve", h[:, k, :], bk[:])

        def store_out(t):
            for b in range(4):
                for hf in range(2):
                    bk = nb()
                    for kk in range(4):
                        k = 4 * hf + kk
                        P.tr(bk[:, 128 * kk:128 * kk + 128], h[:, k, 128 * b:128 * b + 128], identf)
                    P.copy("act" if hf else "dve", xio[:, b, 512 * hf:512 * hf + 512], bk[:])
            P.dma("sp", out_d[t * T:(t + 1) * T, :].rearrange("(b p) f -> p b f", p=128), xio, osem)

        TWO_PI = float(2 * np.pi)
        MAGIC = 12582912.0
        C1 = 6.28125
        C2 = float(2 * np.pi - 6.28125)
        PI_LO = 3.1415925

        def trig_tables(t):
            posi = av(16 * K, 2 * K, I32)
            posf = av(18 * K, 2 * K, F32)
            ang = av(20 * K, 2 * K, F32)
            nn = av(22 * K, 2 * K, F32)
            r2 = av(24 * K, 2 * K, F32)
            P.dma("sp", posi, pos_d[:, t * T:(t + 1) * T].partition_broadcast(128), qsem)
            P.copy("dve", posf, posi)
            for (inv, parts, cs, sn) in ((cc("invb", parts=64), 64, cosb, sinb), (cc("invc"), 128, cosc, sinc)):
                a = ang[0:parts]
                n_ = nn[0:parts]
                r = r2[0:parts]
                P.ts("dve", a, posf[0:parts], inv, ALU.mult)
                P.ts("dve", n_, a, float(1.0 / TWO_PI), ALU.mult, MAGIC, ALU.add)
                P.ts("dve", n_, n_, -MAGIC, ALU.add)
                P.stt("dve", a, n_, -C1, a, ALU.mult, ALU.add)
                P.stt("dve", a, n_, -C2, a, ALU.mult, ALU.add)
                P.ts("dve", a, a, -PI_LO, ALU.max, PI_LO, ALU.min)
                P.actf(sn[:], a, AF.Sin)
                P.ts("dve", r, a, float(np.pi / 2), ALU.add)
                P.ts("dve", n_, r, PI_LO, ALU.is_gt, TWO_PI, ALU.mult)
                P.tt("dve", r, r, n_, ALU.subtract)
                P.ts("dve", r, r, -PI_LO, ALU.max, PI_LO, ALU.min)
                P.actf(cs[:], r, AF.Sin)

        def even_mixer(t):
            l = 0
            prenorm(l, 2)
            catA = av(0, 4 * K, BF16, inner=T)
            catB = av(4 * K, 8 * K, BF16, inner=T, parts=64)
            qf = av(12 * K, 2 * K, F32)
            fb = av(14 * K, 2 * K, F32)
            kk_ = av(16 * K, 2 * K, F32)
            bb = av(18 * K, 2 * K, F32)
            e1 = av(20 * K, 2 * K, F32)
            e2 = av(22 * K, 2 * K, F32)
            qs = av(24 * K, 1 * K, BF16)
            ks = av(25 * K, 1 * K, BF16)
            kdT = av(26 * K, 1 * K, BF16)
            vtok = av(27 * K, 2 * K, BF16, inner=128, parts=64)
            gs = av(29 * K, 2 * K, F32)
            obuf = av(31 * K, 2 * K, F32)
            decb = av(33 * K, 32, F32)
            kdtok = av(33 * K + 512, 512, BF16, inner=128, parts=64)
            attb = av(34 * K, 256, BF16, inner=64, parts=64)
            lb = lbs[:, 4:8]
            omlb = lbs[:, 8:12]
            maskU = cc("maskU", parts=64)
            for hh in range(4):
                w = wload(("hg", hh)).rearrange("p (k c) -> p k c", c=512)
                pq = nb()
                for k in range(8):
                    P.mm(pq[:], w[:, k, 0:128], xn[:, k, :], start=(k == 0), stop=(k == 7))
                P.actf(qf, pq[:], AF.Silu)
                pf = nb()
                for k in range(8):
                    P.mm(pf[:], w[:, k, 128:256], xn[:, k, :], start=(k == 0), stop=(k == 7))
                P.actf(fb, pf[:], AF.Sigmoid)
                P.ts("dve", fb, fb, omlb[:, hh:hh + 1], ALU.mult, lb[:, hh:hh + 1], ALU.add)
                P.ts("dve", kk_, fb, -1.0, ALU.mult, 1.0, ALU.add)
                P.actf(e1, fb, AF.Ln)
                P.emit("dve", lambda e, o=bb, m=cc("scanmask"), d=e1: e.tensor_tensor_scan(o, m, d, 0.0, ALU.mult, ALU.add),
                       reads=[cc("scanmask"), e1], writes=[bb])
                b3 = bb.rearrange("p (c t) -> p c t", t=64)
                P.actf(e1, bb, AF.Exp)
                P.tt("dve", qs, qf, e1, ALU.mult)
                P.actf(e2, bb, AF.Exp, scale=-1.0)
                P.tt("dve", ks, kk_, e2, ALU.mult)
                P.actf(decb, b3[:, :, 63], AF.Exp)
                e13 = e1.rearrange("p (c t) -> p c t", t=64)
                P.tt("dve", e13, b3[:, :, 63:64].to_broadcast([128, 8, 64]), b3, ALU.subtract)
                P.actf(e1, e1, AF.Exp)
                P.tt("dve", kdT, kk_, e1, ALU.mult)
                for c in range(8):
                    pv = nb()
                    for k in range(8):
                        P.mm(pv[0:64, 0:128], xn[:, k, 64 * c:64 * c + 64], w[:, k, 256:384], start=(k == 0), stop=(k == 7))
                    P.copy("act", vtok[:, c, :], pv[0:64, 0:128])
                pgt = nb()
                for k in range(8):
                    P.mm(pgt[:], w[:, k, 384:512], xn[:, k, :], start=(k == 0), stop=(k == 7))
                P.actf(gs, pgt[:], AF.Silu)
                for c in range(8):
                    cs_ = slice(64 * c, 64 * c + 64)
                    r = c % 2
                    pt_ = nb()
                    ptb = pt_[:].bitcast(BF16)
                    P.tr(ptb[0:64, 0:128], kdT[:, cs_], identb[:])
                    P.copy("act", kdtok[:, r, :], ptb[0:64, 0:128])
                    pa = nb()
                    P.mm(pa[0:64, 0:64], ks[:, cs_], qs[:, cs_])
                    P.tt("dve", attb[:, r, :], pa[0:64, 0:64], maskU, ALU.mult)
                    po = nb()
                    P.mm(po[:, 0:64], vtok[:, c, :], attb[:, r, :], start=True, stop=False)
                    P.mm(po[:, 0:64], hgSb[:, hh, :], qs[:, cs_], start=False, stop=True)
                    P.copy("act", obuf[:, cs_], po[:, 0:64])
                    pu_ = nb()
                    P.mm(pu_[:, 0:128], kdtok[:, r, :], vtok[:, c, :])
                    P.stt("dve", hgS[:, hh, :], hgS[:, hh, :], decb[:, c:c + 1], pu_[:, 0:128], ALU.mult, ALU.add)
                    P.copy("act", hgSb[:, hh, :], hgS[:, hh, :])
                P.actf(sqb[:, 0, :], obuf, AF.Square)
                pn_ = nb()
                P.mm(pn_[:], onesb[:], sqb[:, 0, :])
                rstd_from_ss(pn_[:], e2, 1.0 / 128)
                P.stt("dve", obuf, obuf, cc("ogh"), e2, ALU.mult, ALU.mult)
                P.tt("dve", catA[:, hh, :], obuf, gs, ALU.mult)
            qraw = av(35 * K, 8 * K, F32, inner=T, parts=64)
            kraw = av(43 * K, 2 * K, F32, parts=64)
            ra = av(45 * K, 8 * K, F32, inner=T, parts=64)
            rb = av(53 * K, 8 * K, F32, inner=T, parts=64)
            qT = av(61 * K, 4 * K, BF16, inner=T, parts=64)
            smx_ = [av(65 * K, 1 * K, F32), av(68 * K, 1 * K, F32)]
            pexp_ = [av(66 * K, 1 * K, F32), av(69 * K, 1 * K, F32)]
            pnb_ = [av(67 * K, 512, BF16), av(70 * K, 512, BF16)]
            pTt_ = [av(67 * K + 512, 512, BF16, inner=128), av(70 * K + 512, 512, BF16, inner=128)]
            swa_it = [0]
            cos4 = cosb[:, :].unsqueeze(1).to_broadcast([64, 4, T])
            sin4 = sinb[:, :].unsqueeze(1).to_broadcast([64, 4, T])
            for g in range(2):
                w = wload(("swa", g)).rearrange("p (k c) -> p k c", c=384)
                for q4 in range(4):
                    pq = nb()
                    for k in range(8):
                        P.mm(pq[0:64, :], w[:, k, 64 * q4:64 * q4 + 64], xn[:, k, :], start=(k == 0), stop=(k == 7))
                    P.copy("act", qraw[:, q4, :], pq[0:64, :])
                pk = nb()
                for k in range(8):
                    P.mm(pk[0:64, :], w[:, k, 256:320], xn[:, k, :], start=(k == 0), stop=(k == 7))
                P.copy("act", kraw, pk[0:64, :])
                for b in range(4):
                    pv = nb()
                    for k in range(8):
                        P.mm(pv[:, 0:64], xn[:, k, 128 * b:128 * b + 128], w[:, k, 320:384], start=(k == 0), stop=(k == 7))
                    P.copy("act", vswa[:, 1 + b, 64 * g:64 * g + 64], pv[:, 0:64])
                P.tt("dve", ra[:], qraw[:], cos4, ALU.mult)
                P.tt("dve", rb[0:32], qraw[32:64], sin4[32:64], ALU.mult)
                P.tt("dve", rb[32:64], qraw[0:32], sin4[0:32], ALU.mult)
                P.tt("dve", qT[0:32], ra[0:32], rb[0:32], ALU.subtract)
                P.tt("dve", qT[32:64], ra[32:64], rb[32:64], ALU.add)
                ra1 = ra[:, 0, :]
                rb1 = rb[:, 0, :]
                P.tt("dve", ra1, kraw, cosb[:], ALU.mult)
                P.tt("dve", rb1[0:32], kraw[32:64], sinb[32:64], ALU.mult)
                P.tt("dve", rb1[32:64], kraw[0:32], sinb[0:32], ALU.mult)
                P.tt("dve", kT[0:32, g, 128:640], ra1[0:32], rb1[0:32], ALU.subtract)
                P.tt("dve", kT[32:64, g, 128:640], ra1[32:64], rb1[32:64], ALU.add)
                for b in range(4):
                    mask = cc("swam0") if (t == 0 and b == 0) else cc("swam")
                    for q4 in range(4):
                        hq = 4 * g + q4
                        sink = cc("sinks", hq, 1)
                        par = swa_it[0] % 2
                        swa_it[0] += 1
                        smx, pexp, pnb, pTt = smx_[par], pexp_[par], pnb_[par], pTt_[par]
                        sc = smallc[:, 8 * par:8 * par + 8]
                        ps_ = nb()
                        P.mm(ps_[:, 0:256], qT[:, q4, 128 * b:128 * b + 128], kT[:, g, 128 * b:128 * b + 256])
                        P.stt("dve", smx, ps_[:, 0:256], 0.125, mask, ALU.mult, ALU.add)
                        P.emit("dve", lambda e, o=sc[:, 0:1], i=smx: e.reduce_max(o, i, AX.X),
                               reads=[smx], writes=[sc[:, 0:1]])
                        P.ts("dve", sc[:, 1:2], sc[:, 0:1], sink, ALU.max, -1.0, ALU.mult)
                        P.memset("dve", sc[:, 2:3], 0.0)
                        P.actf(pexp, smx, AF.Exp, bias=sc[:, 1:2], accum_out=sc[:, 2:3])
                        P.actf(sc[:, 3:4], sink, AF.Exp, bias=sc[:, 1:2])
                        P.tt("dve", sc[:, 4:5], sc[:, 2:3], sc[:, 3:4], ALU.add)
                        P.emit("dve", lambda e, o=sc[:, 5:6], i=sc[:, 4:5]: e.reciprocal(o, i),
                               reads=[sc[:, 4:5]], writes=[sc[:, 5:6]])
                        P.ts("dve", pnb, pexp, sc[:, 5:6], ALU.mult)
                        pt_ = nb()
                        ptb = pt_[:].bitcast(BF16)
                        for j in range(2):
                            P.tr(ptb[:, 128 * j:128 * j + 128], pnb[:, 128 * j:128 * j + 128], identb[:])
                        P.copy("act", pTt.rearrange("p a b -> p (a b)"), ptb[:, 0:256])
                        po = nb()
                        for j in range(2):
                            P.mm(po[0:64, 0:128], vswa[:, b + j, 64 * g:64 * g + 64], pTt[:, j, :], start=(j == 0), stop=(j == 1))
                        P.copy("act", catB[:, hq, 128 * b:128 * b + 128], po[0:64, 0:128])
                P.copy("dve", kT[:, g, 0:128], kT[:, g, 512:640])
            P.copy("dve", vswa[:, 0, :], vswa[:, 4, :])
            for u in range(4):
                w = wload(("ewout", u))
                wa = w[:, 0:1024].rearrange("p (k c) -> p k c", c=256)
                wb_ = w[0:64, 1024:3072].rearrange("p (k c) -> p k c", c=256)
                for o2 in range(2):
                    o = 2 * u + o2
                    py = nb()
                    for hh in range(4):
                        P.mm(py[:], wa[:, hh, 128 * o2:128 * o2 + 128], catA[:, hh, :], start=(hh == 0), stop=False)
                    for hq in range(8):
                        P.mm(py[:], wb_[:, hq, 128 * o2:128 * o2 + 128], catB[:, hq, :], start=False, stop=(hq == 7))
                    evac_y(py[:], o)
            postnorm_res(l, 3, False)

        def odd_mixer(t):
            l = 1
            prenorm(l, 2)
            cat = av(0, 16 * K, BF16, inner=T)
            qraw = av(16 * K, 4 * K, F32, inner=T)
            kraw = av(20 * K, 4 * K, F32, inner=T)
            ta = av(24 * K, 4 * K, F32, inner=T)
            tb = av(28 * K, 4 * K, F32, inner=T)
            rr = av(32 * K, 4 * K, F32, inner=T)
            qr = av(36 * K, 2 * K, BF16, inner=T)
            qin = av(38 * K, 2 * K, BF16, inner=T)
            krb = av(40 * K, 2 * K, BF16, inner=T)
            kinT = av(42 * K, 2 * K, BF16, inner=T)
            ktok = av(44 * K, 2 * K, BF16, inner=256)
            vtok = av(46 * K, 4 * K, BF16, inner=512)
            gs = av(50 * K, 4 * K, BF16, inner=T)
            obuf = av(54 * K, 8 * K, F32, inner=T)
            attb = av(62 * K, 512, BF16, inner=128)
            e2 = av(63 * K, 2 * K, F32)
            cos2 = cosc[:, :].unsqueeze(1).to_broadcast([128, 2, T])
            sin2 = sinc[:, :].unsqueeze(1).to_broadcast([128, 2, T])

            def rope(raw):
                P.tt("dve", ta[:], raw[:], cos2, ALU.mult)
                P.tt("dve", tb[:], raw[:], sin2, ALU.mult)
                P.tt("dve", rr[:, 0, :], ta[:, 0, :], tb[:, 1, :], ALU.subtract)
                P.tt("dve", rr[:, 1, :], ta[:, 1, :], tb[:, 0, :], ALU.add)

            for hh in range(4):
                gq4 = cc("gq", 128 * hh, 128).unsqueeze(1).to_broadcast([128, 4, 128])
                gk4 = cc("gk", 128 * hh, 128).unsqueeze(1).to_broadcast([128, 4, 128])
                w = wload(("oq", hh)).rearrange("p (k c) -> p k c", c=256)
                for hf in range(2):
                    pq = nb()
                    for k in range(8):
                        P.mm(pq[:], w[:, k, 128 * hf:128 * hf + 128], xn[:, k, :], start=(k == 0), stop=(k == 7))
                    P.copy("act", qraw[:, hf, :], pq[:])
                rope(qraw)
                P.copy("act", qr[:], rr[:])
                for hf in range(2):
                    P.tt("dve", qin[:, hf, :].rearrange("p (c t) -> p c t", t=128),
                         rr[:, hf, :].rearrange("p (c t) -> p c t", t=128), gq4, ALU.mult)
                w = wload(("ok", hh)).rearrange("p (k c) -> p k c", c=256)
                for hf in range(2):
                    pk = nb()
                    for k in range(8):
                        P.mm(pk[:], w[:, k, 128 * hf:128 * hf + 128], xn[:, k, :], start=(k == 0), stop=(k == 7))
                    P.copy("act", kraw[:, hf, :], pk[:])
                rope(kraw)
                P.ts("dve", rr[:], rr[:], 1.0 / 16.0, ALU.mult)
                P.copy("act", krb[:], rr[:])
                for hf in range(2):
                    P.tt("dve", kinT[:, hf, :].rearrange("p (c t) -> p c t", t=128),
                         rr[:, hf, :].rearrange("p (c t) -> p c t", t=128), gk4, ALU.mult)
                pt_ = nb()
                ptb = pt_[:].bitcast(BF16)
                for c in range(4):
                    for hf in range(2):
                        P.tr(ptb[:, 256 * c + 128 * hf:256 * c + 128 * hf + 128], kinT[:, hf, 128 * c:128 * c + 128], identb[:])
                P.copy("act", ktok.rearrange("p a b -> p (a b)"), ptb[:])
                w = wload(("ov", hh)).rearrange("p (k c) -> p k c", c=512)
                for c in range(4):
                    pv = nb()
                    for k in range(8):
                        P.mm(pv[:], xn[:, k, 128 * c:128 * c + 128], w[:, k, :], start=(k == 0), stop=(k == 7))
                    P.copy("act", vtok[:, c, :], pv[:])
                w = wload(("og", hh)).rearrange("p (k c) -> p k c", c=512)
                for vc in range(4):
                    pg = nb()
                    for k in range(8):
                        P.mm(pg[:], w[:, k, 128 * vc:128 * vc + 128], xn[:, k, :], start=(k == 0), stop=(k == 7))
                    P.actf(gs[:, vc, :], pg[:], AF.Silu)
                dT = cc("decayT", 128 * hh, 128)
                for c in range(4):
                    cs_ = slice(128 * c, 128 * c + 128)
                    r = c % 2
                    pa = nb()
                    for hf in range(2):
                        P.mm(pa[:, 0:128], krb[:, hf, cs_], qr[:, hf, cs_], start=(hf == 0), stop=(hf == 1))
                    P.tt("dve", attb[:, r, :], pa[:, 0:128], dT, ALU.mult)
                    po = nb()
                    for vc in range(4):
                        vs = slice(128 * vc, 128 * vc + 128)
                        P.mm(po[:, vs], vtok[:, c, vs], attb[:, r, :], start=True, stop=False)
                        for hf in range(2):
                            P.mm(po[:, vs], retSb[:, 2 * hh + hf, vs], qin[:, hf, cs_], start=False, stop=(hf == 1))
                    P.copy("act", obuf[:, :, cs_], po[:].rearrange("p (a b) -> p a b", b=128))
                    for hf in range(2):
                        pu_ = nb()
                        P.mm(pu_[:], ktok[:, c, 128 * hf:128 * hf + 128], vtok[:, c, :])
                        P.stt("dve", retS[:, 2 * hh + hf, :], retS[:, 2 * hh + hf, :], GAMMA128[hh], pu_[:], ALU.mult, ALU.add)
                        P.copy("act", retSb[:, 2 * hh + hf, :], retS[:, 2 * hh + hf, :])
                pn_ = nb()
                for vc in range(4):
                    P.actf(sqb[:, vc % 2, :], obuf[:, vc, :], AF.Square)
                    P.mm(pn_[:], onesb[:], sqb[:, vc % 2, :], start=(vc == 0), stop=(vc == 3))
                rstd_from_ss(pn_[:], e2, 1.0 / 512)
                for vc in range(4):
                    P.stt("dve", obuf[:, vc, :], obuf[:, vc, :], cc("ogr", vc, 1), e2, ALU.mult, ALU.mult)
                    P.tt("dve", cat[:, 4 * hh + vc, :], obuf[:, vc, :], gs[:, vc, :], ALU.mult)
            for u in range(4):
                w = wload(("owout", u)).rearrange("p (k c) -> p k c", c=256)
                for o2 in range(2):
                    o = 2 * u + o2
                    py = nb()
                    for j in range(16):
                        P.mm(py[:], w[:, j, 128 * o2:128 * o2 + 128], cat[:, j, :], start=(j == 0), stop=(j == 15))
                    evac_y(py[:], o)
            postnorm_res(l, 3, False)

        for t in range(NT):
            P.epoch = t // EP_TILES
            load_x(t)
            trig_tables(t)
            stage_fns = [lambda: ffn(0, 1), lambda: even_mixer(t), lambda: ffn(0, 2), lambda: ple(0, t),
                         lambda: ffn(1, 1), lambda: odd_mixer(t), lambda: ffn(1, 2), lambda: ple(1, t)]
            for f in stage_fns[:nstage]:
                f()
            store_out(t)
        stats = P.finalize(sems, blk)
    return nc, stats


_NC_CACHE = {}


def kernel(**inputs):
    NT = inputs["x"].shape[1] // T
    if NT not in _NC_CACHE:
        _NC_CACHE[NT] = build_nc(NT)[0]
    nc = _NC_CACHE[NT]
    wall = pack_weights(inputs)
    cst = pack_consts(inputs)
    x = np.ascontiguousarray(inputs["x"], dtype=np.float32)
    p = np.ascontiguousarray(inputs["p"], dtype=np.float32)
    pos = np.ascontiguousarray(inputs["positions"], dtype=np.int32)
    in_maps = []
    for c in range(8):
        in_maps.append({"x": x[c], "p": np.ascontiguousarray(p[:, c]), "pos": pos[c:c + 1],
                        "wall": wall, "cst": cst})
    res = run_bass_kernel_spmd(nc, in_maps, core_ids=list(range(8)))
    return np.stack([r["out"] for r in res.results], axis=0)
```

```python
from contextlib import ExitStack
import numpy as np
import concourse.bass as bass
import concourse.mybir as mybir
from concourse.bass_utils import run_bass_kernel_spmd

F32 = mybir.dt.float32
BF16 = mybir.dt.bfloat16
I32 = mybir.dt.int32
AF = mybir.ActivationFunctionType
ALU = mybir.AluOpType
AX = mybir.AxisListType

_DSZ = {F32: 4, BF16: 2, I32: 4}
COMPUTE = ("pe", "act", "dve", "pool")
SAME_ENGINE_SYNC = True


def _rng(ap):
    sz = _DSZ[ap.dtype]
    steps = ap.ap
    off = int(ap.offset)
    if str(ap.space) == "DRAM":
        ext = sum((c - 1) * s for s, c in steps)
        return (ap.name, off * sz, (off + ext + 1) * sz)
    if str(ap.space) == "PSUM":
        return (ap.name, 0, 2048)
    pstep = steps[0][0]
    lo = off % pstep if pstep > 0 else off
    ext = sum((c - 1) * s for s, c in steps[1:])
    return (ap.name, lo * sz, (lo + ext + 1) * sz)


class Op:
    __slots__ = ("eng", "fn", "deps", "sem", "cnt", "needs_inc", "inc_idx", "is_dma", "seq", "epoch")

    def __init__(self, eng, fn, is_dma=False):
        self.seq = 0
        self.epoch = 0
        self.eng = eng
        self.fn = fn
        self.deps = set()
        self.sem = None
        self.cnt = 0
        self.needs_inc = False
        self.inc_idx = 0
        self.is_dma = is_dma


class DmaSem:
    def __init__(self, handle):
        self.h = handle
        self.n = 0
        self.last = None


class Prog:
    def __init__(self, nc):
        self.nc = nc
        self.streams = {e: [] for e in ("pe", "act", "dve", "pool", "sp")}
        self.acc = {}
        self.dma_sems = []
        self.epoch = 0

    def _add(self, op):
        op.seq = len(self.streams[op.eng])
        op.epoch = self.epoch
        last = {}
        keep = set()
        for d in op.deps:
            if d.is_dma:
                keep.add(d)
            elif d.eng not in last or d.seq > last[d.eng].seq:
                last[d.eng] = d
        op.deps = keep | set(last.values())
        self.streams[op.eng].append(op)

    def new_dma_sem(self, stack, name):
        s = DmaSem(stack.enter_context(self.nc.semaphore(name)))
        self.dma_sems.append(s)
        return s

    def _track(self, op, reads, writes):
        writes = list(writes) + [ap for ap in reads if str(ap.space) == "PSUM"]
        reads = [ap for ap in reads if str(ap.space) != "PSUM"]
        for ap in reads:
            n, lo, hi = _rng(ap)
            a = self.acc.setdefault(n, ([], []))
            for (l, h, o) in a[0]:
                if l < hi and lo < h:
                    op.deps.add(o)
            a[1].append((lo, hi, op))
        for ap in writes:
            n, lo, hi = _rng(ap)
            a = self.acc.setdefault(n, ([], []))
            for (l, h, o) in a[0]:
                if l < hi and lo < h and o is not op:
                    op.deps.add(o)
            for (l, h, o) in a[1]:
                if l < hi and lo < h and o is not op:
                    op.deps.add(o)
            a[0][:] = [t for t in a[0] if not (lo <= t[0] and t[1] <= hi)]
            a[1][:] = [t for t in a[1] if not (lo <= t[0] and t[1] <= hi)]
            a[0].append((lo, hi, op))

    def emit(self, eng, fn, reads=(), writes=()):
        op = Op(eng, fn)
        self._track(op, reads, writes)
        self._add(op)
        return op

    def dma(self, queue, out, in_, sem):
        op = Op(queue, lambda e: e.dma_start(out=out, in_=in_), is_dma=True)
        if sem.last is not None:
            op.deps.add(sem.last)
        sem.n += 1
        sem.last = op
        op.sem = sem
        op.cnt = sem.n
        self._track(op, [in_], [out])
        self._add(op)
        return op

    def mm(self, out, lhsT, rhs, start=True, stop=True):
        return self.emit("pe", lambda e: e.matmul(out, lhsT, rhs, start=start, stop=stop),
                         reads=[lhsT, rhs], writes=[out])

    def tr(self, out, in_, ident):
        return self.emit("pe", lambda e: e.transpose(out, in_, ident), reads=[in_, ident], writes=[out])

    def actf(self, out, in_, func, bias=None, scale=None, accum_out=None):
        kw = {}
        rd = [in_]
        wr = [out]
        if bias is not None:
            kw["bias"] = bias
            if not isinstance(bias, (int, float)):
                rd.append(bias)
        if scale is not None:
            kw["scale"] = scale
            if not isinstance(scale, (int, float)):
                rd.append(scale)
        if accum_out is not None:
            kw["accum_out"] = accum_out
            wr.append(accum_out)
        return self.emit("act", lambda e: e.activation(out, in_, func, **kw), reads=rd, writes=wr)

    def tt(self, eng, out, in0, in1, op):
        return self.emit(eng, lambda e: e.tensor_tensor(out, in0, in1, op), reads=[in0, in1], writes=[out])

    def ts(self, eng, out, in0, s1, op0, s2=None, op1=None):
        rd = [in0] + [s for s in (s1, s2) if s is not None and not isinstance(s, (int, float))]
        kw = {}
        if op1 is not None:
            kw["op1"] = op1
        return self.emit(eng, lambda e: e.tensor_scalar(out, in0, s1, s2, op0, **kw), reads=rd, writes=[out])

    def stt(self, eng, out, in0, scalar, in1, op0, op1):
        rd = [in0, in1] + ([scalar] if not isinstance(scalar, (int, float)) else [])
        return self.emit(eng, lambda e: e.scalar_tensor_tensor(out, in0, scalar, in1, op0, op1),
                         reads=rd, writes=[out])

    def copy(self, eng, out, in_):
        if eng == "act":
            return self.emit("act", lambda e: e.copy(out, in_), reads=[in_], writes=[out])
        return self.emit(eng, lambda e: e.tensor_copy(out, in_), reads=[in_], writes=[out])

    def memset(self, eng, ap, val):
        return self.emit(eng, lambda e: e.memset(ap, val), writes=[ap])

    def finalize(self, sems, block):
        def needs_sem(op, d):
            if d.is_dma:
                return True
            if d.eng == op.eng and not op.is_dma:
                if d.eng == "pe" or not SAME_ENGINE_SYNC:
                    return False
            return True

        for st in self.streams.values():
            for op in st:
                for d in op.deps:
                    if not d.is_dma and needs_sem(op, d):
                        d.needs_inc = True
        self.max_inc = {}
        for eng in COMPUTE:
            k = {}
            for op in self.streams[eng]:
                if not op.is_dma and op.needs_inc:
                    k[op.epoch] = k.get(op.epoch, 0) + 1
                    op.inc_idx = k[op.epoch]
            self.max_inc[eng] = max(list(k.values()) + [0])
        stats = {}
        nc = self.nc
        engobj = {"pe": nc.tensor, "act": nc.scalar, "dve": nc.vector, "pool": nc.gpsimd, "sp": nc.sync}

        def run(eng):
            e = engobj[eng]
            waited = {}
            nw = 0
            for op in self.streams[eng]:
                need = {}
                for d in op.deps:
                    if not needs_sem(op, d):
                        continue
                    if d.is_dma:
                        key, v, h = ("d", id(d.sem)), d.cnt * 16, d.sem.h
                    else:
                        key, v, h = ("c", d.eng, d.epoch), d.inc_idx, sems[d.eng][d.epoch]
                    if v > need.get(key, (0, None))[0]:
                        need[key] = (v, h)
                for key, (v, h) in need.items():
                    if v > waited.get(key, 0):
                        e.wait_ge(h, v)
                        waited[key] = v
                        nw += 1
                ins = op.fn(e)
                if op.is_dma:
                    ins.then_inc(op.sem.h, 16)
                elif op.needs_inc:
                    ins.then_inc(sems[eng][op.epoch], 1)
            if eng == "sp":
                for s in self.dma_sems:
                    if s.n:
                        e.wait_ge(s.h, s.n * 16)
            stats[eng] = (len(self.streams[eng]), nw)

        block.tensor(lambda _e: run("pe"))
        block.scalar(lambda _e: run("act"))
        block.vector(lambda _e: run("dve"))
        block.gpsimd(lambda _e: run("pool"))
        block.sync(lambda _e: run("sp"))
        return stats


D = 1024
T = 512
DFF = 2816
EPS = 1e-6
NJ = DFF // 128
R_SLOTS = 4
EP_TILES = 2
WCOLS = 4096


class Cols:
    def __init__(self):
        self.n = 0
        self.d = {}

    def add(self, name, w):
        self.d[name] = (self.n, w)
        self.n += w


CST = Cols()
for _n, _w in (("gT", 112), ("lbT", 8), ("ogh", 1), ("ogr", 4), ("sinks", 8), ("invb", 1), ("invc", 1),
               ("ident", 128), ("maskU", 64), ("scanmask", 512), ("swam", 256), ("swam0", 256),
               ("decayT", 512), ("gq", 512), ("gk", 512)):
    CST.add(_n, _w)
NCST = CST.n


def unit_list():
    u = []
    for l in range(2):
        u += [(("ffn1_in", l, i), 4096) for i in range(11)]
        u += [(("ffn1_out", l, i), 2816) for i in range(8)]
        if l == 0:
            u += [(("hg", i), 4096) for i in range(4)]
            u += [(("swa", i), 3072) for i in range(2)]
            u += [(("ewout", i), 3072) for i in range(4)]
        else:
            for hh in range(4):
                u += [(("oq", hh), 2048), (("ok", hh), 2048), (("ov", hh), 4096), (("og", hh), 4096)]
            u += [(("owout", i), 4096) for i in range(4)]
        u += [(("ffn2_in", l, i), 4096) for i in range(11)]
        u += [(("ffn2_out", l, i), 2816) for i in range(8)]
        u += [(("pproj", l), 2048), (("pgate", l, 0), 4096), (("pgate", l, 1), 4096)]
    return u


UNITS = unit_list()
NU = len(UNITS)
UIDX = {k: i for i, (k, _) in enumerate(UNITS)}


def _kmaj(W, cols):
    kc = W.shape[0] // 128
    return np.ascontiguousarray(W.reshape(kc, 128, W.shape[1])[:, :, cols].transpose(1, 0, 2)).reshape(128, -1)


def pack_weights(inp):
    wall = np.zeros((NU, 128, WCOLS), np.float32)
    ar = np.arange
    for (key, n) in UNITS:
        i = UIDX[key]
        k0 = key[0]
        if k0 in ("ffn1_in", "ffn2_in"):
            W = inp[k0[:4] + "_w_in"][key[1]]
            u = key[2]
            cols = np.concatenate([ar(256 * u, 256 * u + 256), DFF + ar(256 * u, 256 * u + 256)])
            blk = _kmaj(W, cols)
        elif k0 in ("ffn1_out", "ffn2_out"):
            W = inp[k0[:4] + "_w_out"][key[1]]
            blk = _kmaj(W, ar(128 * key[2], 128 * key[2] + 128))
        elif k0 == "hg":
            W = inp["even_w_in"][0]
            hh = key[1]
            cols = np.concatenate([g * 512 + hh * 128 + ar(128) for g in range(4)])
            blk = _kmaj(W, cols)
        elif k0 == "swa":
            W = inp["even_w_in"][0]
            g = key[1]
            cols = np.concatenate([2048 + 256 * g + ar(256), 2560 + 64 * g + ar(64), 2688 + 64 * g + ar(64)])
            blk = _kmaj(W, cols)
        elif k0 == "ewout":
            W = inp["even_w_out"][0]
            cols = ar(256 * key[1], 256 * key[1] + 256)
            a = _kmaj(W[0:512], cols)
            b = np.zeros((128, 8, 256), np.float32)
            b[0:64] = W[512:1024].reshape(8, 64, 1024)[:, :, cols].transpose(1, 0, 2)
            blk = np.concatenate([a, b.reshape(128, -1)], axis=1)
        elif k0 in ("oq", "ok", "ov", "og"):
            W = inp["odd_w_in"][0]
            hh = key[1]
            base = {"oq": 0, "ok": 1024, "ov": 2048, "og": 4096}[k0]
            wd = 256 if k0 in ("oq", "ok") else 512
            blk = _kmaj(W, base + hh * wd + ar(wd))
        elif k0 == "owout":
            W = inp["odd_w_out"][0]
            blk = _kmaj(W, ar(256 * key[1], 256 * key[1] + 256))
        elif k0 == "pproj":
            blk = _kmaj(inp["ple_w_proj"][key[1]], ar(1024))
        elif k0 == "pgate":
            blk = _kmaj(inp["ple_w_gate"][key[1]], ar(512 * key[2], 512 * key[2] + 512))
        assert blk.shape == (128, n), (key, blk.shape, n)
        wall[i, :, :n] = blk
    return wall


def pack_consts(inp):
    c = np.zeros((128, NCST), np.float32)

    def put(name, arr):
        o, w = CST.d[name]
        arr = np.asarray(arr, np.float32)
        c[: arr.shape[0], o:o + w] = arr.reshape(arr.shape[0], w)

    put("gT", inp["norm_g"].reshape(2, 7, 8, 128).transpose(3, 0, 1, 2).reshape(128, 112))
    put("lbT", inp["hgrn_lb"].reshape(2, 4, 128).transpose(2, 0, 1).reshape(128, 8))
    put("ogh", inp["hgrn_onorm_g"].reshape(128, 1))
    put("ogr", inp["ret_onorm_g"].reshape(4, 128).T)
    put("sinks", np.broadcast_to(inp["attn_sinks"].reshape(1, 8), (128, 8)))
    inv_b = (np.float32(10000.0) ** (-np.arange(0, 64, 2, dtype=np.float32) / np.float32(64))).astype(np.float32)
    inv_c = (np.float32(10000.0) ** (-np.linspace(0.0, 1.0, 128, dtype=np.float32))).astype(np.float32)
    put("invb", np.concatenate([inv_b, inv_b]).reshape(64, 1))
    put("invc", inv_c.reshape(128, 1))
    put("ident", np.eye(128, dtype=np.float32))
    s = np.arange(64)
    put("maskU", (s[:, None] <= s[None, :]).astype(np.float32))
    sm = np.ones(512, np.float32)
    sm[::64] = 0.0
    put("scanmask", np.broadcast_to(sm, (128, 512)))
    qi = np.arange(128)[:, None]
    kj = np.arange(256)[None, :]
    diff = qi + 128 - kj
    valid = (diff >= 0) & (diff < 128)
    put("swam", np.where(valid, 0.0, -30000.0))
    put("swam0", np.where(valid & (kj >= 128), 0.0, -30000.0))
    hh = np.arange(4, dtype=np.float64)
    lg = np.log1p(-np.exp2(-5.0 - hh))
    idx = np.arange(128, dtype=np.float64)
    dcs = idx[None, :] - idx[:, None]
    dec = np.where(dcs[None] >= 0, np.exp(np.maximum(dcs, 0.0)[None] * lg[:, None, None]), 0.0)
    put("decayT", dec.transpose(1, 0, 2).reshape(128, 512))
    put("gq", np.broadcast_to(np.exp((idx + 1.0)[None, :] * lg[:, None]).reshape(1, 512), (128, 512)))
    put("gk", np.broadcast_to(np.exp((127.0 - idx)[None, :] * lg[:, None]).reshape(1, 512), (128, 512)))
    return c


GAMMA128 = [float(np.exp(128.0 * np.log1p(-np.exp2(-5.0 - h)))) for h in range(4)]


def build_nc(NT, nstage=8):
    S = NT * T
    nc = bass.Bass("TRN2", target_bir_lowering=False)
    x_d = nc.dram_tensor("x", [S, D], F32, kind="ExternalInput").ap()
    p_d = nc.dram_tensor("p", [2, S, 256], F32, kind="ExternalInput").ap()
    pos_d = nc.dram_tensor("pos", [1, S], I32, kind="ExternalInput").ap()
    wall_d = nc.dram_tensor("wall", [NU, 128, WCOLS], F32, kind="ExternalInput").ap()
    cst_d = nc.dram_tensor("cst", [128, NCST], F32, kind="ExternalInput").ap()
    out_d = nc.dram_tensor("out", [S, D], F32, kind="ExternalOutput").ap()
    wbf_d = nc.dram_tensor("wbf", [NU, 128, WCOLS], BF16, kind="Internal").ap()

    with ExitStack() as st:
        P = Prog(nc)
        sb = lambda n, s, d: st.enter_context(nc.sbuf_tensor(n, s, d))
        h = sb("h", [128, 8, T], F32)
        xn = sb("xn", [128, 8, T], BF16)
        ybuf = sb("ybuf", [128, 8, T], F32)
        sqb = sb("sqb", [128, 2, T], BF16)
        rstd = sb("rstd", [128, T], F32)
        wring = sb("wring", [128, R_SLOTS, WCOLS], BF16)
        ptile = sb("ptile", [128, 4, 256], F32)
        pTb = sb("pTb", [128, 2, T], BF16)
        cosb = sb("cosb", [64, T], F32)
        sinb = sb("sinb", [64, T], F32)
        cosc = sb("cosc", [128, T], F32)
        sinc = sb("sinc", [128, T], F32)
        retS = sb("retS", [128, 8, 512], F32)
        retSb = sb("retSb", [128, 8, 512], BF16)
        hgS = sb("hgS", [128, 4, 128], F32)
        hgSb = sb("hgSb", [128, 4, 128], BF16)
        kT = sb("kT", [64, 2, 640], BF16)
        vswa = sb("vswa", [128, 5, 128], BF16)
        csb = sb("csb", [128, NCST], F32)
        gsc = sb("gsc", [128, 112], F32)
        lbs = sb("lbs", [128, 12], F32)
        identb = sb("identb", [128, 128], BF16)
        onesb = sb("onesb", [128, 128], BF16)
        smallc = sb("smallc", [128, 16], F32)
        ARENA = 72 * 1024
        arena = sb("arena", [128, ARENA // 2], BF16)
        pb = [st.enter_context(nc.psum_tensor(f"pb{i}", [128, 512], F32)) for i in range(8)]
        NEP = (NT + EP_TILES - 1) // EP_TILES
        sems = {e: [st.enter_context(nc.semaphore(f"s_{e}{i}")) for i in range(NEP)] for e in COMPUTE}
        wsem_all = [[P.new_dma_sem(st, f"w{j}_{i}") for i in range(R_SLOTS)] for j in range(NEP)]
        csem = [P.new_dma_sem(st, f"c{i}") for i in range(8)]
        xsem = P.new_dma_sem(st, "xs")
        osem = P.new_dma_sem(st, "os")
        psem = P.new_dma_sem(st, "ps")
        qsem = P.new_dma_sem(st, "qs")
        ksem = P.new_dma_sem(st, "ks")
        blk = st.enter_context(nc.Block())

        def av(lo, nbytes, dt, inner=None, parts=128):
            a = arena[0:parts, lo // 2:(lo + nbytes) // 2]
            if dt != BF16:
                a = a.bitcast(dt)
            if inner is not None:
                a = a.rearrange("p (a b) -> p a b", b=inner)
            return a

        K = 1024

        def cc(name, lo=0, n=None, parts=128):
            o, w = CST.d[name]
            n = w - lo if n is None else n
            return csb[0:parts, o + lo:o + lo + n]

        identf = cc("ident")
        bank_ctr = [0]

        def nb():
            b = pb[bank_ctr[0] % 7]
            bank_ctr[0] += 1
            return b

        ss_bank = pb[7]

        wctr = [0]

        def wload(key):
            i = UIDX[key]
            n = UNITS[i][1]
            s = wctr[0] % R_SLOTS
            wctr[0] += 1
            P.dma("sp", wring[:, s, 0:n], wbf_d[i, :, 0:n], wsem_all[P.epoch][s])
            return wring[:, s, 0:n]

        P.dma("sp", csb[:], cst_d, ksem)
        for i, (key, n) in enumerate(UNITS):
            P.dma("pool", wbf_d[i, :, 0:n], wall_d[i, :, 0:n], csem[i % 8])
        P.copy("dve", identb[:], identf)
        P.memset("dve", onesb[:], 1.0)
        P.ts("dve", gsc[:], cc("gT"), 0.5, ALU.mult)
        P.tt("dve", lbs[:, 0:4], cc("lbT", 0, 4), cc("lbT", 4, 4), ALU.subtract)
        P.actf(lbs[:, 4:8], lbs[:, 0:4], AF.Sigmoid)
        P.ts("dve", lbs[:, 8:12], lbs[:, 4:8], -1.0, ALU.mult, 1.0, ALU.add)
        for tns in (retS, retSb, hgS, hgSb, vswa):
            P.memset("dve", tns[:], 0.0)
        P.memset("dve", kT[:], 0.0)

        def gcol(l, n, k, scaled=False):
            c = (l * 7 + n) * 8 + k
            return gsc[:, c:c + 1] if scaled else cc("gT", c, 1)

        def rstd_from_ss(ss_ap, out_ap, inv_n):
            P.actf(out_ap, ss_ap, AF.Ln, bias=EPS, scale=inv_n)
            P.actf(out_ap, out_ap, AF.Exp, scale=-0.5)

        def prenorm(l, n):
            for k in range(8):
                P.actf(sqb[:, k % 2, :], h[:, k, :], AF.Square)
                P.mm(ss_bank[:], onesb[:], sqb[:, k % 2, :], start=(k == 0), stop=(k == 7))
            rstd_from_ss(ss_bank[:], rstd[:], 1.0 / D)
            for k in range(8):
                P.stt("dve", xn[:, k, :], h[:, k, :], gcol(l, n, k), rstd[:], ALU.mult, ALU.mult)

        pending_ss = []

        def flush_ss():
            for o in pending_ss:
                P.mm(ss_bank[:], onesb[:], sqb[:, o % 2, :], start=(o == 0), stop=(o == 7))
            pending_ss.clear()

        def evac_y(py, o):
            flush_ss()
            P.copy("dve", ybuf[:, o, :], py)
            P.actf(sqb[:, o % 2, :], ybuf[:, o, :], AF.Square)
            pending_ss.append(o)

        def postnorm_res(l, n, half):
            flush_ss()
            rstd_from_ss(ss_bank[:], rstd[:], 1.0 / D)
            for k in range(8):
                P.tt("dve" if k % 2 else "pool", ybuf[:, k, :], ybuf[:, k, :], rstd[:], ALU.mult)
                P.stt("dve", h[:, k, :], ybuf[:, k, :], gcol(l, n, k, scaled=half), h[:, k, :], ALU.mult, ALU.add)

        def ffn(l, which):
            n0 = 0 if which == 1 else 4
            hid = av(0, 22 * K, BF16, inner=T)
            sg = av(22 * K, 4 * K, F32, inner=T)
            prenorm(l, n0)
            for u in range(11):
                w = wload((f"ffn{which}_in", l, u)).rearrange("p (k c) -> p k c", c=512)
                for jj in range(2):
                    j = 2 * u + jj
                    pg = nb()
                    pu = nb()
                    for k in range(8):
                        P.mm(pg[:], w[:, k, 128 * jj:128 * jj + 128], xn[:, k, :], start=(k == 0), stop=(k == 7))
                    for k in range(8):
                        P.mm(pu[:], w[:, k, 256 + 128 * jj:256 + 128 * jj + 128], xn[:, k, :], start=(k == 0), stop=(k == 7))
                    P.actf(sg[:, j % 2, :], pg[:], AF.Silu)
                    P.tt("dve", hid[:, j, :], sg[:, j % 2, :], pu[:], ALU.mult)
            for o in range(8):
                w = wload((f"ffn{which}_out", l, o)).rearrange("p (k c) -> p k c", c=128)
                py = nb()
                for j in range(NJ):
                    P.mm(py[:], w[:, j, :], hid[:, j, :], start=(j == 0), stop=(j == NJ - 1))
                evac_y(py[:], o)
            postnorm_res(l, n0 + 1, True)

        def ple(l, t):
            for k in range(8):
                P.copy("act" if k % 2 else "dve", xn[:, k, :], h[:, k, :])
            P.dma("sp", ptile[:], p_d[l, t * T:(t + 1) * T, :].rearrange("(b p) f -> p b f", p=128), psem)
            for c in range(2):
                bk = nb()
                for b in range(4):
                    P.tr(bk[:, 128 * b:128 * b + 128], ptile[:, b, 128 * c:128 * c + 128], identf)
                P.copy("act", pTb[:, c, :], bk[:])
            w = wload(("pproj", l)).rearrange("p (k c) -> p k c", c=1024)
            for o in range(8):
                bk = nb()
                for c in range(2):
                    P.mm(bk[:], w[:, c, 128 * o:128 * o + 128], pTb[:, c, :], start=(c == 0), stop=(c == 1))
                P.copy("dve", ybuf[:, o, :], bk[:])
            sg = av(22 * K, 4 * K, F32, inner=T)
            for u in range(2):
                w = wload(("pgate", l, u)).rearrange("p (k c) -> p k c", c=512)
                for o4 in range(4):
                    o = 4 * u + o4
                    bk = nb()
                    for k in range(8):
                        P.mm(bk[:], w[:, k, 128 * o4:128 * o4 + 128], xn[:, k, :], start=(k == 0), stop=(k == 7))
                    P.actf(sg[:, o % 2, :], bk[:], AF.Sigmoid)
                    P.tt("dve", ybuf[:, o, :], ybuf[:, o, :], sg[:, o % 2, :], ALU.mult)
                    flush_ss()
                    P.actf(sqb[:, o % 2, :], ybuf[:, o, :], AF.Square)
                    pending_ss.append(o)
            postnorm_res(l, 6, False)

        xio = av(0, 16 * K, F32, inner=D)

        def load_x(t):
            P.dma("sp", xio, x_d[t * T:(t + 1) * T, :].rearrange("(b p) f -> p b f", p=128), xsem)
            for k in range(8):
                bk = nb()
                for b in range(4):
                    P.tr(bk[:, 128 * b:128 * b + 128], xio[:, b, 128 * k:128 * k + 128], identf)
                P.copy("act" if k % 2 else "dve", h[:, k, :], bk[:])

        def store_out(t):
            for b in range(4):
                for hf in range(2):
                    bk = nb()
                    for kk in range(4):
                        k = 4 * hf + kk
                        P.tr(bk[:, 128 * kk:128 * kk + 128], h[:, k, 128 * b:128 * b + 128], identf)
                    P.copy("act" if hf else "dve", xio[:, b, 512 * hf:512 * hf + 512], bk[:])
            P.dma("sp", out_d[t * T:(t + 1) * T, :].rearrange("(b p) f -> p b f", p=128), xio, osem)

        TWO_PI = float(2 * np.pi)
        MAGIC = 12582912.0
        C1 = 6.28125
        C2 = float(2 * np.pi - 6.28125)
        PI_LO = 3.1415925

        def trig_tables(t):
            posi = av(16 * K, 2 * K, I32)
            posf = av(18 * K, 2 * K, F32)
            ang = av(20 * K, 2 * K, F32)
            nn = av(22 * K, 2 * K, F32)
            r2 = av(24 * K, 2 * K, F32)
            P.dma("sp", posi, pos_d[:, t * T:(t + 1) * T].partition_broadcast(128), qsem)
            P.copy("dve", posf, posi)
            for (inv, parts, cs, sn) in ((cc("invb", parts=64), 64, cosb, sinb), (cc("invc"), 128, cosc, sinc)):
                a = ang[0:parts]
                n_ = nn[0:parts]
                r = r2[0:parts]
                P.ts("dve", a, posf[0:parts], inv, ALU.mult)
                P.ts("dve", n_, a, float(1.0 / TWO_PI), ALU.mult, MAGIC, ALU.add)
                P.ts("dve", n_, n_, -MAGIC, ALU.add)
                P.stt("dve", a, n_, -C1, a, ALU.mult, ALU.add)
                P.stt("dve", a, n_, -C2, a, ALU.mult, ALU.add)
                P.ts("dve", a, a, -PI_LO, ALU.max, PI_LO, ALU.min)
                P.actf(sn[:], a, AF.Sin)
                P.ts("dve", r, a, float(np.pi / 2), ALU.add)
                P.ts("dve", n_, r, PI_LO, ALU.is_gt, TWO_PI, ALU.mult)
                P.tt("dve", r, r, n_, ALU.subtract)
                P.ts("dve", r, r, -PI_LO, ALU.max, PI_LO, ALU.min)
                P.actf(cs[:], r, AF.Sin)

        def even_mixer(t):
            l = 0
            prenorm(l, 2)
            catA = av(0, 4 * K, BF16, inner=T)
            catB = av(4 * K, 8 * K, BF16, inner=T, parts=64)
            qf = av(12 * K, 2 * K, F32)
            fb = av(14 * K, 2 * K, F32)
            kk_ = av(16 * K, 2 * K, F32)
            bb = av(18 * K, 2 * K, F32)
            e1 = av(20 * K, 2 * K, F32)
            e2 = av(22 * K, 2 * K, F32)
            qs = av(24 * K, 1 * K, BF16)
            ks = av(25 * K, 1 * K, BF16)
            kdT = av(26 * K, 1 * K, BF16)
            vtok = av(27 * K, 2 * K, BF16, inner=128, parts=64)
            gs = av(29 * K, 2 * K, F32)
            obuf = av(31 * K, 2 * K, F32)
            decb = av(33 * K, 32, F32)
            kdtok = av(33 * K + 512, 512, BF16, inner=128, parts=64)
            attb = av(34 * K, 256, BF16, inner=64, parts=64)
            lb = lbs[:, 4:8]
            omlb = lbs[:, 8:12]
            maskU = cc("maskU", parts=64)
            for hh in range(4):
                w = wload(("hg", hh)).rearrange("p (k c) -> p k c", c=512)
                pq = nb()
                for k in range(8):
                    P.mm(pq[:], w[:, k, 0:128], xn[:, k, :], start=(k == 0), stop=(k == 7))
                P.actf(qf, pq[:], AF.Silu)
                pf = nb()
                for k in range(8):
                    P.mm(pf[:], w[:, k, 128:256], xn[:, k, :], start=(k == 0), stop=(k == 7))
                P.actf(fb, pf[:], AF.Sigmoid)
                P.ts("dve", fb, fb, omlb[:, hh:hh + 1], ALU.mult, lb[:, hh:hh + 1], ALU.add)
                P.ts("dve", kk_, fb, -1.0, ALU.mult, 1.0, ALU.add)
                P.actf(e1, fb, AF.Ln)
                P.emit("dve", lambda e, o=bb, m=cc("scanmask"), d=e1: e.tensor_tensor_scan(o, m, d, 0.0, ALU.mult, ALU.add),
                       reads=[cc("scanmask"), e1], writes=[bb])
                b3 = bb.rearrange("p (c t) -> p c t", t=64)
                P.actf(e1, bb, AF.Exp)
                P.tt("dve", qs, qf, e1, ALU.mult)
                P.actf(e2, bb, AF.Exp, scale=-1.0)
                P.tt("dve", ks, kk_, e2, ALU.mult)
                P.actf(decb, b3[:, :, 63], AF.Exp)
                e13 = e1.rearrange("p (c t) -> p c t", t=64)
                P.tt("dve", e13, b3[:, :, 63:64].to_broadcast([128, 8, 64]), b3, ALU.subtract)
                P.actf(e1, e1, AF.Exp)
                P.tt("dve", kdT, kk_, e1, ALU.mult)
                for c in range(8):
                    pv = nb()
                    for k in range(8):
                        P.mm(pv[0:64, 0:128], xn[:, k, 64 * c:64 * c + 64], w[:, k, 256:384], start=(k == 0), stop=(k == 7))
                    P.copy("act", vtok[:, c, :], pv[0:64, 0:128])
                pgt = nb()
                for k in range(8):
                    P.mm(pgt[:], w[:, k, 384:512], xn[:, k, :], start=(k == 0), stop=(k == 7))
                P.actf(gs, pgt[:], AF.Silu)
                for c in range(8):
                    cs_ = slice(64 * c, 64 * c + 64)
                    r = c % 2
                    pt_ = nb()
                    ptb = pt_[:].bitcast(BF16)
                    P.tr(ptb[0:64, 0:128], kdT[:, cs_], identb[:])
                    P.copy("act", kdtok[:, r, :], ptb[0:64, 0:128])
                    pa = nb()
                    P.mm(pa[0:64, 0:64], ks[:, cs_], qs[:, cs_])
                    P.tt("dve", attb[:, r, :], pa[0:64, 0:64], maskU, ALU.mult)
                    po = nb()
                    P.mm(po[:, 0:64], vtok[:, c, :], attb[:, r, :], start=True, stop=False)
                    P.mm(po[:, 0:64], hgSb[:, hh, :], qs[:, cs_], start=False, stop=True)
                    P.copy("act", obuf[:, cs_], po[:, 0:64])
                    pu_ = nb()
                    P.mm(pu_[:, 0:128], kdtok[:, r, :], vtok[:, c, :])
                    P.stt("dve", hgS[:, hh, :], hgS[:, hh, :], decb[:, c:c + 1], pu_[:, 0:128], ALU.mult, ALU.add)
                    P.copy("act", hgSb[:, hh, :], hgS[:, hh, :])
                P.actf(sqb[:, 0, :], obuf, AF.Square)
                pn_ = nb()
                P.mm(pn_[:], onesb[:], sqb[:, 0, :])
                rstd_from_ss(pn_[:], e2, 1.0 / 128)
                P.stt("dve", obuf, obuf, cc("ogh"), e2, ALU.mult, ALU.mult)
                P.tt("dve", catA[:, hh, :], obuf, gs, ALU.mult)
            qraw = av(35 * K, 8 * K, F32, inner=T, parts=64)
            kraw = av(43 * K, 2 * K, F32, parts=64)
            ra = av(45 * K, 8 * K, F32, inner=T, parts=64)
            rb = av(53 * K, 8 * K, F32, inner=T, parts=64)
            qT = av(61 * K, 4 * K, BF16, inner=T, parts=64)
            smx_ = [av(65 * K, 1 * K, F32), av(68 * K, 1 * K, F32)]
            pexp_ = [av(66 * K, 1 * K, F32), av(69 * K, 1 * K, F32)]
            pnb_ = [av(67 * K, 512, BF16), av(70 * K, 512, BF16)]
            pTt_ = [av(67 * K + 512, 512, BF16, inner=128), av(70 * K + 512, 512, BF16, inner=128)]
            swa_it = [0]
            cos4 = cosb[:, :].unsqueeze(1).to_broadcast([64, 4, T])
            sin4 = sinb[:, :].unsqueeze(1).to_broadcast([64, 4, T])
            for g in range(2):
                w = wload(("swa", g)).rearrange("p (k c) -> p k c", c=384)
                for q4 in range(4):
                    pq = nb()
                    for k in range(8):
                        P.mm(pq[0:64, :], w[:, k, 64 * q4:64 * q4 + 64], xn[:, k, :], start=(k == 0), stop=(k == 7))
                    P.copy("act", qraw[:, q4, :], pq[0:64, :])
                pk = nb()
                for k in range(8):
                    P.mm(pk[0:64, :], w[:, k, 256:320], xn[:, k, :], start=(k == 0), stop=(k == 7))
                P.copy("act", kraw, pk[0:64, :])
                for b in range(4):
                    pv = nb()
                    for k in range(8):
                        P.mm(pv[:, 0:64], xn[:, k, 128 * b:128 * b + 128], w[:, k, 320:384], start=(k == 0), stop=(k == 7))
                    P.copy("act", vswa[:, 1 + b, 64 * g:64 * g + 64], pv[:, 0:64])
                P.tt("dve", ra[:], qraw[:], cos4, ALU.mult)
                P.tt("dve", rb[0:32], qraw[32:64], sin4[32:64], ALU.mult)
                P.tt("dve", rb[32:64], qraw[0:32], sin4[0:32], ALU.mult)
                P.tt("dve", qT[0:32], ra[0:32], rb[0:32], ALU.subtract)
                P.tt("dve", qT[32:64], ra[32:64], rb[32:64], ALU.add)
                ra1 = ra[:, 0, :]
                rb1 = rb[:, 0, :]
                P.tt("dve", ra1, kraw, cosb[:], ALU.mult)
                P.tt("dve", rb1[0:32], kraw[32:64], sinb[32:64], ALU.mult)
                P.tt("dve", rb1[32:64], kraw[0:32], sinb[0:32], ALU.mult)
                P.tt("dve", kT[0:32, g, 128:640], ra1[0:32], rb1[0:32], ALU.subtract)
                P.tt("dve", kT[32:64, g, 128:640], ra1[32:64], rb1[32:64], ALU.add)
                for b in range(4):
                    mask = cc("swam0") if (t == 0 and b == 0) else cc("swam")
                    for q4 in range(4):
                        hq = 4 * g + q4
                        sink = cc("sinks", hq, 1)
                        par = swa_it[0] % 2
                        swa_it[0] += 1
                        smx, pexp, pnb, pTt = smx_[par], pexp_[par], pnb_[par], pTt_[par]
                        sc = smallc[:, 8 * par:8 * par + 8]
                        ps_ = nb()
                        P.mm(ps_[:, 0:256], qT[:, q4, 128 * b:128 * b + 128], kT[:, g, 128 * b:128 * b + 256])
                        P.stt("dve", smx, ps_[:, 0:256], 0.125, mask, ALU.mult, ALU.add)
                        P.emit("dve", lambda e, o=sc[:, 0:1], i=smx: e.reduce_max(o, i, AX.X),
                               reads=[smx], writes=[sc[:, 0:1]])
                        P.ts("dve", sc[:, 1:2], sc[:, 0:1], sink, ALU.max, -1.0, ALU.mult)
                        P.memset("dve", sc[:, 2:3], 0.0)
                        P.actf(pexp, smx, AF.Exp, bias=sc[:, 1:2], accum_out=sc[:, 2:3])
                        P.actf(sc[:, 3:4], sink, AF.Exp, bias=sc[:, 1:2])
                        P.tt("dve", sc[:, 4:5], sc[:, 2:3], sc[:, 3:4], ALU.add)
                        P.emit("dve", lambda e, o=sc[:, 5:6], i=sc[:, 4:5]: e.reciprocal(o, i),
                               reads=[sc[:, 4:5]], writes=[sc[:, 5:6]])
                        P.ts("dve", pnb, pexp, sc[:, 5:6], ALU.mult)
                        pt_ = nb()
                        ptb = pt_[:].bitcast(BF16)
                        for j in range(2):
                            P.tr(ptb[:, 128 * j:128 * j + 128], pnb[:, 128 * j:128 * j + 128], identb[:])
                        P.copy("act", pTt.rearrange("p a b -> p (a b)"), ptb[:, 0:256])
                        po = nb()
                        for j in range(2):
                            P.mm(po[0:64, 0:128], vswa[:, b + j, 64 * g:64 * g + 64], pTt[:, j, :], start=(j == 0), stop=(j == 1))
                        P.copy("act", catB[:, hq, 128 * b:128 * b + 128], po[0:64, 0:128])
                P.copy("dve", kT[:, g, 0:128], kT[:, g, 512:640])
            P.copy("dve", vswa[:, 0, :], vswa[:, 4, :])
            for u in range(4):
                w = wload(("ewout", u))
                wa = w[:, 0:1024].rearrange("p (k c) -> p k c", c=256)
                wb_ = w[0:64, 1024:3072].rearrange("p (k c) -> p k c", c=256)
                for o2 in range(2):
                    o = 2 * u + o2
                    py = nb()
                    for hh in range(4):
                        P.mm(py[:], wa[:, hh, 128 * o2:128 * o2 + 128], catA[:, hh, :], start=(hh == 0), stop=False)
                    for hq in range(8):
                        P.mm(py[:], wb_[:, hq, 128 * o2:128 * o2 + 128], catB[:, hq, :], start=False, stop=(hq == 7))
                    evac_y(py[:], o)
            postnorm_res(l, 3, False)

        def odd_mixer(t):
            l = 1
            prenorm(l, 2)
            cat = av(0, 16 * K, BF16, inner=T)
            qraw = av(16 * K, 4 * K, F32, inner=T)
            kraw = av(20 * K, 4 * K, F32, inner=T)
            ta = av(24 * K, 4 * K, F32, inner=T)
            tb = av(28 * K, 4 * K, F32, inner=T)
            rr = av(32 * K, 4 * K, F32, inner=T)
            qr = av(36 * K, 2 * K, BF16, inner=T)
            qin = av(38 * K, 2 * K, BF16, inner=T)
            krb = av(40 * K, 2 * K, BF16, inner=T)
            kinT = av(42 * K, 2 * K, BF16, inner=T)
            ktok = av(44 * K, 2 * K, BF16, inner=256)
            vtok = av(46 * K, 4 * K, BF16, inner=512)
            gs = av(50 * K, 4 * K, BF16, inner=T)
            obuf = av(54 * K, 8 * K, F32, inner=T)
            attb = av(62 * K, 512, BF16, inner=128)
            e2 = av(63 * K, 2 * K, F32)
            cos2 = cosc[:, :].unsqueeze(1).to_broadcast([128, 2, T])
            sin2 = sinc[:, :].unsqueeze(1).to_broadcast([128, 2, T])

            def rope(raw):
                P.tt("dve", ta[:], raw[:], cos2, ALU.mult)
                P.tt("dve", tb[:], raw[:], sin2, ALU.mult)
                P.tt("dve", rr[:, 0, :], ta[:, 0, :], tb[:, 1, :], ALU.subtract)
                P.tt("dve", rr[:, 1, :], ta[:, 1, :], tb[:, 0, :], ALU.add)

            for hh in range(4):
                gq4 = cc("gq", 128 * hh, 128).unsqueeze(1).to_broadcast([128, 4, 128])
                gk4 = cc("gk", 128 * hh, 128).unsqueeze(1).to_broadcast([128, 4, 128])
                w = wload(("oq", hh)).rearrange("p (k c) -> p k c", c=256)
                for hf in range(2):
                    pq = nb()
                    for k in range(8):
                        P.mm(pq[:], w[:, k, 128 * hf:128 * hf + 128], xn[:, k, :], start=(k == 0), stop=(k == 7))
                    P.copy("act", qraw[:, hf, :], pq[:])
                rope(qraw)
                P.copy("act", qr[:], rr[:])
                for hf in range(2):
                    P.tt("dve", qin[:, hf, :].rearrange("p (c t) -> p c t", t=128),
                         rr[:, hf, :].rearrange("p (c t) -> p c t", t=128), gq4, ALU.mult)
                w = wload(("ok", hh)).rearrange("p (k c) -> p k c", c=256)
                for hf in range(2):
                    pk = nb()
                    for k in range(8):
                        P.mm(pk[:], w[:, k, 128 * hf:128 * hf + 128], xn[:, k, :], start=(k == 0), stop=(k == 7))
                    P.copy("act", kraw[:, hf, :], pk[:])
                rope(kraw)
                P.ts("dve", rr[:], rr[:], 1.0 / 16.0, ALU.mult)
                P.copy("act", krb[:], rr[:])
                for hf in range(2):
                    P.tt("dve", kinT[:, hf, :].rearrange("p (c t) -> p c t", t=128),
                         rr[:, hf, :].rearrange("p (c t) -> p c t", t=128), gk4, ALU.mult)
                pt_ = nb()
                ptb = pt_[:].bitcast(BF16)
                for c in range(4):
                    for hf in range(2):
                        P.tr(ptb[:, 256 * c + 128 * hf:256 * c + 128 * hf + 128], kinT[:, hf, 128 * c:128 * c + 128], identb[:])
                P.copy("act", ktok.rearrange("p a b -> p (a b)"), ptb[:])
                w = wload(("ov", hh)).rearrange("p (k c) -> p k c", c=512)
                for c in range(4):
                    pv = nb()
                    for k in range(8):
                        P.mm(pv[:], xn[:, k, 128 * c:128 * c + 128], w[:, k, :], start=(k == 0), stop=(k == 7))
                    P.copy("act", vtok[:, c, :], pv[:])
                w = wload(("og", hh)).rearrange("p (k c) -> p k c", c=512)
                for vc in range(4):
                    pg = nb()
                    for k in range(8):
                        P.mm(pg[:], w[:, k, 128 * vc:128 * vc + 128], xn[:, k, :], start=(k == 0), stop=(k == 7))
                    P.actf(gs[:, vc, :], pg[:], AF.Silu)
                dT = cc("decayT", 128 * hh, 128)
                for c in range(4):
                    cs_ = slice(128 * c, 128 * c + 128)
                    r = c % 2
                    pa = nb()
                    for hf in range(2):
                        P.mm(pa[:, 0:128], krb[:, hf, cs_], qr[:, hf, cs_], start=(hf == 0), stop=(hf == 1))
                    P.tt("dve", attb[:, r, :], pa[:, 0:128], dT, ALU.mult)
                    po = nb()
                    for vc in range(4):
                        vs = slice(128 * vc, 128 * vc + 128)
                        P.mm(po[:, vs], vtok[:, c, vs], attb[:, r, :], start=True, stop=False)
                        for hf in range(2):
                            P.mm(po[:, vs], retSb[:, 2 * hh + hf, vs], qin[:, hf, cs_], start=False, stop=(hf == 1))
                    P.copy("act", obuf[:, :, cs_], po[:].rearrange("p (a b) -> p a b", b=128))
                    for hf in range(2):
                        pu_ = nb()
                        P.mm(pu_[:], ktok[:, c, 128 * hf:128 * hf + 128], vtok[:, c, :])
                        P.stt("dve", retS[:, 2 * hh + hf, :], retS[:, 2 * hh + hf, :], GAMMA128[hh], pu_[:], ALU.mult, ALU.add)
                        P.copy("act", retSb[:, 2 * hh + hf, :], retS[:, 2 * hh + hf, :])
                pn_ = nb()
                for vc in range(4):
                    P.actf(sqb[:, vc % 2, :], obuf[:, vc, :], AF.Square)
                    P.mm(pn_[:], onesb[:], sqb[:, vc % 2, :], start=(vc == 0), stop=(vc == 3))
                rstd_from_ss(pn_[:], e2, 1.0 / 512)
                for vc in range(4):
                    P.stt("dve", obuf[:, vc, :], obuf[:, vc, :], cc("ogr", vc, 1), e2, ALU.mult, ALU.mult)
                    P.tt("dve", cat[:, 4 * hh + vc, :], obuf[:, vc, :], gs[:, vc, :], ALU.mult)
            for u in range(4):
                w = wload(("owout", u)).rearrange("p (k c) -> p k c", c=256)
                for o2 in range(2):
                    o = 2 * u + o2
                    py = nb()
                    for j in range(16):
                        P.mm(py[:], w[:, j, 128 * o2:128 * o2 + 128], cat[:, j, :], start=(j == 0), stop=(j == 15))
                    evac_y(py[:], o)
            postnorm_res(l, 3, False)

        for t in range(NT):
            P.epoch = t // EP_TILES
            load_x(t)
            trig_tables(t)
            stage_fns = [lambda: ffn(0, 1), lambda: even_mixer(t), lambda: ffn(0, 2), lambda: ple(0, t),
                         lambda: ffn(1, 1), lambda: odd_mixer(t), lambda: ffn(1, 2), lambda: ple(1, t)]
            for f in stage_fns[:nstage]:
                f()
            store_out(t)
        stats = P.finalize(sems, blk)
    return nc, stats


_NC_CACHE = {}


def kernel(**inputs):
    NT = inputs["x"].shape[1] // T
    if NT not in _NC_CACHE:
        _NC_CACHE[NT] = build_nc(NT)[0]
    nc = _NC_CACHE[NT]
    wall = pack_weights(inputs)
    cst = pack_consts(inputs)
    x = np.ascontiguousarray(inputs["x"], dtype=np.float32)
    p = np.ascontiguousarray(inputs["p"], dtype=np.float32)
    pos = np.ascontiguousarray(inputs["positions"], dtype=np.int32)
    in_maps = []
    for c in range(8):
        in_maps.append({"x": x[c], "p": np.ascontiguousarray(p[:, c]), "pos": pos[c:c + 1],
                        "wall": wall, "cst": cst})
    res = run_bass_kernel_spmd(nc, in_maps, core_ids=list(range(8)))
    return np.stack([r["out"] for r in res.results], axis=0)
```

```python
from contextlib import ExitStack
import numpy as np
import concourse.bass as bass
import concourse.mybir as mybir
from concourse.bass_utils import run_bass_kernel_spmd

F32 = mybir.dt.float32
BF16 = mybir.dt.bfloat16
I32 = mybir.dt.int32
AF = mybir.ActivationFunctionType
ALU = mybir.AluOpType
AX = mybir.AxisListType

_DSZ = {F32: 4, BF16: 2, I32: 4}
COMPUTE = ("pe", "act", "dve", "pool")
SAME_ENGINE_SYNC = True


def _rng(ap):
    sz = _DSZ[ap.dtype]
    steps = ap.ap
    off = int(ap.offset)
    if str(ap.space) == "DRAM":
        ext = sum((c - 1) * s for s, c in steps)
        return (ap.name, off * sz, (off + ext + 1) * sz)
    if str(ap.space) == "PSUM":
        return (ap.name, 0, 2048)
    pstep = steps[0][0]
    lo = off % pstep if pstep > 0 else off
    ext = sum((c - 1) * s for s, c in steps[1:])
    return (ap.name, lo * sz, (lo + ext + 1) * sz)


class Op:
    __slots__ = ("eng", "fn", "deps", "sem", "cnt", "needs_inc", "inc_idx", "is_dma", "seq", "epoch")

    def __init__(self, eng, fn, is_dma=False):
        self.seq = 0
        self.epoch = 0
        self.eng = eng
        self.fn = fn
        self.deps = set()
        self.sem = None
        self.cnt = 0
        self.needs_inc = False
        self.inc_idx = 0
        self.is_dma = is_dma


class DmaSem:
    def __init__(self, handle):
        self.h = handle
        self.n = 0
        self.last = None


class Prog:
    def __init__(self, nc):
        self.nc = nc
        self.streams = {e: [] for e in ("pe", "act", "dve", "pool", "sp")}
        self.acc = {}
        self.dma_sems = []
        self.epoch = 0

    def _add(self, op):
        op.seq = len(self.streams[op.eng])
        op.epoch = self.epoch
        last = {}
        keep = set()
        for d in op.deps:
            if d.is_dma:
                keep.add(d)
            elif d.eng not in last or d.seq > last[d.eng].seq:
                last[d.eng] = d
        op.deps = keep | set(last.values())
        self.streams[op.eng].append(op)

    def new_dma_sem(self, stack, name):
        s = DmaSem(stack.enter_context(self.nc.semaphore(name)))
        self.dma_sems.append(s)
        return s

    def _track(self, op, reads, writes):
        writes = list(writes) + [ap for ap in reads if str(ap.space) == "PSUM"]
        reads = [ap for ap in reads if str(ap.space) != "PSUM"]
        for ap in reads:
            n, lo, hi = _rng(ap)
            a = self.acc.setdefault(n, ([], []))
            for (l, h, o) in a[0]:
                if l < hi and lo < h:
                    op.deps.add(o)
            a[1].append((lo, hi, op))
        for ap in writes:
            n, lo, hi = _rng(ap)
            a = self.acc.setdefault(n, ([], []))
            for (l, h, o) in a[0]:
                if l < hi and lo < h and o is not op:
                    op.deps.add(o)
            for (l, h, o) in a[1]:
                if l < hi and lo < h and o is not op:
                    op.deps.add(o)
            a[0][:] = [t for t in a[0] if not (lo <= t[0] and t[1] <= hi)]
            a[1][:] = [t for t in a[1] if not (lo <= t[0] and t[1] <= hi)]
            a[0].append((lo, hi, op))

    def emit(self, eng, fn, reads=(), writes=()):
        op = Op(eng, fn)
        self._track(op, reads, writes)
        self._add(op)
        return op

    def dma(self, queue, out, in_, sem):
        op = Op(queue, lambda e: e.dma_start(out=out, in_=in_), is_dma=True)
        if sem.last is not None:
            op.deps.add(sem.last)
        sem.n += 1
        sem.last = op
        op.sem = sem
        op.cnt = sem.n
        self._track(op, [in_], [out])
        self._add(op)
        return op

    def mm(self, out, lhsT, rhs, start=True, stop=True):
        return self.emit("pe", lambda e: e.matmul(out, lhsT, rhs, start=start, stop=stop),
                         reads=[lhsT, rhs], writes=[out])

    def tr(self, out, in_, ident):
        return self.emit("pe", lambda e: e.transpose(out, in_, ident), reads=[in_, ident], writes=[out])

    def actf(self, out, in_, func, bias=None, scale=None, accum_out=None):
        kw = {}
        rd = [in_]
        wr = [out]
        if bias is not None:
            kw["bias"] = bias
            if not isinstance(bias, (int, float)):
                rd.append(bias)
        if scale is not None:
            kw["scale"] = scale
            if not isinstance(scale, (int, float)):
                rd.append(scale)
        if accum_out is not None:
            kw["accum_out"] = accum_out
            wr.append(accum_out)
        return self.emit("act", lambda e: e.activation(out, in_, func, **kw), reads=rd, writes=wr)

    def tt(self, eng, out, in0, in1, op):
        return self.emit(eng, lambda e: e.tensor_tensor(out, in0, in1, op), reads=[in0, in1], writes=[out])

    def ts(self, eng, out, in0, s1, op0, s2=None, op1=None):
        rd = [in0] + [s for s in (s1, s2) if s is not None and not isinstance(s, (int, float))]
        kw = {}
        if op1 is not None:
            kw["op1"] = op1
        return self.emit(eng, lambda e: e.tensor_scalar(out, in0, s1, s2, op0, **kw), reads=rd, writes=[out])

    def stt(self, eng, out, in0, scalar, in1, op0, op1):
        rd = [in0, in1] + ([scalar] if not isinstance(scalar, (int, float)) else [])
        return self.emit(eng, lambda e: e.scalar_tensor_tensor(out, in0, scalar, in1, op0, op1),
                         reads=rd, writes=[out])

    def copy(self, eng, out, in_):
        if eng == "act":
            return self.emit("act", lambda e: e.copy(out, in_), reads=[in_], writes=[out])
        return self.emit(eng, lambda e: e.tensor_copy(out, in_), reads=[in_], writes=[out])

    def memset(self, eng, ap, val):
        return self.emit(eng, lambda e: e.memset(ap, val), writes=[ap])

    def finalize(self, sems, block):
        def needs_sem(op, d):
            if d.is_dma:
                return True
            if d.eng == op.eng and not op.is_dma:
                if d.eng == "pe" or not SAME_ENGINE_SYNC:
                    return False
            return True

        for st in self.streams.values():
            for op in st:
                for d in op.deps:
                    if not d.is_dma and needs_sem(op, d):
                        d.needs_inc = True
        self.max_inc = {}
        for eng in COMPUTE:
            k = {}
            for op in self.streams[eng]:
                if not op.is_dma and op.needs_inc:
                    k[op.epoch] = k.get(op.epoch, 0) + 1
                    op.inc_idx = k[op.epoch]
            self.max_inc[eng] = max(list(k.values()) + [0])
        stats = {}
        nc = self.nc
        engobj = {"pe": nc.tensor, "act": nc.scalar, "dve": nc.vector, "pool": nc.gpsimd, "sp": nc.sync}

        def run(eng):
            e = engobj[eng]
            waited = {}
            nw = 0
            for op in self.streams[eng]:
                need = {}
                for d in op.deps:
                    if not needs_sem(op, d):
                        continue
                    if d.is_dma:
                        key, v, h = ("d", id(d.sem)), d.cnt * 16, d.sem.h
                    else:
                        key, v, h = ("c", d.eng, d.epoch), d.inc_idx, sems[d.eng][d.epoch]
                    if v > need.get(key, (0, None))[0]:
                        need[key] = (v, h)
                for key, (v, h) in need.items():
                    if v > waited.get(key, 0):
                        e.wait_ge(h, v)
                        waited[key] = v
                        nw += 1
                ins = op.fn(e)
                if op.is_dma:
                    ins.then_inc(op.sem.h, 16)
                elif op.needs_inc:
                    ins.then_inc(sems[eng][op.epoch], 1)
            if eng == "sp":
                for s in self.dma_sems:
                    if s.n:
                        e.wait_ge(s.h, s.n * 16)
            stats[eng] = (len(self.streams[eng]), nw)

        block.tensor(lambda _e: run("pe"))
        block.scalar(lambda _e: run("act"))
        block.vector(lambda _e: run("dve"))
        block.gpsimd(lambda _e: run("pool"))
        block.sync(lambda _e: run("sp"))
        return stats


D = 1024
T = 512
DFF = 2816
EPS = 1e-6
NJ = DFF // 128
R_SLOTS = 4
EP_TILES = 2
WCOLS = 4096


class Cols:
    def __init__(self):
        self.n = 0
        self.d = {}

    def add(self, name, w):
        self.d[name] = (self.n, w)
        self.n += w


CST = Cols()
for _n, _w in (("gT", 112), ("lbT", 8), ("ogh", 1), ("ogr", 4), ("sinks", 8), ("invb", 1), ("invc", 1),
               ("ident", 128), ("maskU", 64), ("scanmask", 512), ("swam", 256), ("swam0", 256),
               ("decayT", 512), ("gq", 512), ("gk", 512)):
    CST.add(_n, _w)
NCST = CST.n


def unit_list():
    u = []
    for l in range(2):
        u += [(("ffn1_in", l, i), 4096) for i in range(11)]
        u += [(("ffn1_out", l, i), 2816) for i in range(8)]
        if l == 0:
            u += [(("hg", i), 4096) for i in range(4)]
            u += [(("swa", i), 3072) for i in range(2)]
            u += [(("ewout", i), 3072) for i in range(4)]
        else:
            for hh in range(4):
                u += [(("oq", hh), 2048), (("ok", hh), 2048), (("ov", hh), 4096), (("og", hh), 4096)]
            u += [(("owout", i), 4096) for i in range(4)]
        u += [(("ffn2_in", l, i), 4096) for i in range(11)]
        u += [(("ffn2_out", l, i), 2816) for i in range(8)]
        u += [(("pproj", l), 2048), (("pgate", l, 0), 4096), (("pgate", l, 1), 4096)]
    return u


UNITS = unit_list()
NU = len(UNITS)
UIDX = {k: i for i, (k, _) in enumerate(UNITS)}


def _kmaj(W, cols):
    kc = W.shape[0] // 128
    return np.ascontiguousarray(W.reshape(kc, 128, W.shape[1])[:, :, cols].transpose(1, 0, 2)).reshape(128, -1)


def pack_weights(inp):
    wall = np.zeros((NU, 128, WCOLS), np.float32)
    ar = np.arange
    for (key, n) in UNITS:
        i = UIDX[key]
        k0 = key[0]
        if k0 in ("ffn1_in", "ffn2_in"):
            W = inp[k0[:4] + "_w_in"][key[1]]
            u = key[2]
            cols = np.concatenate([ar(256 * u, 256 * u + 256), DFF + ar(256 * u, 256 * u + 256)])
            blk = _kmaj(W, cols)
        elif k0 in ("ffn1_out", "ffn2_out"):
            W = inp[k0[:4] + "_w_out"][key[1]]
            blk = _kmaj(W, ar(128 * key[2], 128 * key[2] + 128))
        elif k0 == "hg":
            W = inp["even_w_in"][0]
            hh = key[1]
            cols = np.concatenate([g * 512 + hh * 128 + ar(128) for g in range(4)])
            blk = _kmaj(W, cols)
        elif k0 == "swa":
            W = inp["even_w_in"][0]
            g = key[1]
            cols = np.concatenate([2048 + 256 * g + ar(256), 2560 + 64 * g + ar(64), 2688 + 64 * g + ar(64)])
            blk = _kmaj(W, cols)
        elif k0 == "ewout":
            W = inp["even_w_out"][0]
            cols = ar(256 * key[1], 256 * key[1] + 256)
            a = _kmaj(W[0:512], cols)
            b = np.zeros((128, 8, 256), np.float32)
            b[0:64] = W[512:1024].reshape(8, 64, 1024)[:, :, cols].transpose(1, 0, 2)
            blk = np.concatenate([a, b.reshape(128, -1)], axis=1)
        elif k0 in ("oq", "ok", "ov", "og"):
            W = inp["odd_w_in"][0]
            hh = key[1]
            base = {"oq": 0, "ok": 1024, "ov": 2048, "og": 4096}[k0]
            wd = 256 if k0 in ("oq", "ok") else 512
            blk = _kmaj(W, base + hh * wd + ar(wd))
        elif k0 == "owout":
            W = inp["odd_w_out"][0]
            blk = _kmaj(W, ar(256 * key[1], 256 * key[1] + 256))
        elif k0 == "pproj":
            blk = _kmaj(inp["ple_w_proj"][key[1]], ar(1024))
        elif k0 == "pgate":
            blk = _kmaj(inp["ple_w_gate"][key[1]], ar(512 * key[2], 512 * key[2] + 512))
        assert blk.shape == (128, n), (key, blk.shape, n)
        wall[i, :, :n] = blk
    return wall


def pack_consts(inp):
    c = np.zeros((128, NCST), np.float32)

    def put(name, arr):
        o, w = CST.d[name]
        arr = np.asarray(arr, np.float32)
        c[: arr.shape[0], o:o + w] = arr.reshape(arr.shape[0], w)

    put("gT", inp["norm_g"].reshape(2, 7, 8, 128).transpose(3, 0, 1, 2).reshape(128, 112))
    put("lbT", inp["hgrn_lb"].reshape(2, 4, 128).transpose(2, 0, 1).reshape(128, 8))
    put("ogh", inp["hgrn_onorm_g"].reshape(128, 1))
    put("ogr", inp["ret_onorm_g"].reshape(4, 128).T)
    put("sinks", np.broadcast_to(inp["attn_sinks"].reshape(1, 8), (128, 8)))
    inv_b = (np.float32(10000.0) ** (-np.arange(0, 64, 2, dtype=np.float32) / np.float32(64))).astype(np.float32)
    inv_c = (np.float32(10000.0) ** (-np.linspace(0.0, 1.0, 128, dtype=np.float32))).astype(np.float32)
    put("invb", np.concatenate([inv_b, inv_b]).reshape(64, 1))
    put("invc", inv_c.reshape(128, 1))
    put("ident", np.eye(128, dtype=np.float32))
    s = np.arange(64)
    put("maskU", (s[:, None] <= s[None, :]).astype(np.float32))
    sm = np.ones(512, np.float32)
    sm[::64] = 0.0
    put("scanmask", np.broadcast_to(sm, (128, 512)))
    qi = np.arange(128)[:, None]
    kj = np.arange(256)[None, :]
    diff = qi + 128 - kj
    valid = (diff >= 0) & (diff < 128)
    put("swam", np.where(valid, 0.0, -30000.0))
    put("swam0", np.where(valid & (kj >= 128), 0.0, -30000.0))
    hh = np.arange(4, dtype=np.float64)
    lg = np.log1p(-np.exp2(-5.0 - hh))
    idx = np.arange(128, dtype=np.float64)
    dcs = idx[None, :] - idx[:, None]
    dec = np.where(dcs[None] >= 0, np.exp(np.maximum(dcs, 0.0)[None] * lg[:, None, None]), 0.0)
    put("decayT", dec.transpose(1, 0, 2).reshape(128, 512))
    put("gq", np.broadcast_to(np.exp((idx + 1.0)[None, :] * lg[:, None]).reshape(1, 512), (128, 512)))
    put("gk", np.broadcast_to(np.exp((127.0 - idx)[None, :] * lg[:, None]).reshape(1, 512), (128, 512)))
    return c


GAMMA128 = [float(np.exp(128.0 * np.log1p(-np.exp2(-5.0 - h)))) for h in range(4)]


def build_nc(NT, nstage=8):
    S = NT * T
    nc = bass.Bass("TRN2", target_bir_lowering=False)
    x_d = nc.dram_tensor("x", [S, D], F32, kind="ExternalInput").ap()
    p_d = nc.dram_tensor("p", [2, S, 256], F32, kind="ExternalInput").ap()
    pos_d = nc.dram_tensor("pos", [1, S], I32, kind="ExternalInput").ap()
    wall_d = nc.dram_tensor("wall", [NU, 128, WCOLS], F32, kind="ExternalInput").ap()
    cst_d = nc.dram_tensor("cst", [128, NCST], F32, kind="ExternalInput").ap()
    out_d = nc.dram_tensor("out", [S, D], F32, kind="ExternalOutput").ap()
    wbf_d = nc.dram_tensor("wbf", [NU, 128, WCOLS], BF16, kind="Internal").ap()

    with ExitStack() as st:
        P = Prog(nc)
        sb = lambda n, s, d: st.enter_context(nc.sbuf_tensor(n, s, d))
        h = sb("h", [128, 8, T], F32)
        xn = sb("xn", [128, 8, T], BF16)
        ybuf = sb("ybuf", [128, 8, T], F32)
        sqb = sb("sqb", [128, 2, T], BF16)
        rstd = sb("rstd", [128, T], F32)
        wring = sb("wring", [128, R_SLOTS, WCOLS], BF16)
        ptile = sb("ptile", [128, 4, 256], F32)
        pTb = sb("pTb", [128, 2, T], BF16)
        cosb = sb("cosb", [64, T], F32)
        sinb = sb("sinb", [64, T], F32)
        cosc = sb("cosc", [128, T], F32)
        sinc = sb("sinc", [128, T], F32)
        retS = sb("retS", [128, 8, 512], F32)
        retSb = sb("retSb", [128, 8, 512], BF16)
        hgS = sb("hgS", [128, 4, 128], F32)
        hgSb = sb("hgSb", [128, 4, 128], BF16)
        kT = sb("kT", [64, 2, 640], BF16)
        vswa = sb("vswa", [128, 5, 128], BF16)
        csb = sb("csb", [128, NCST], F32)
        gsc = sb("gsc", [128, 112], F32)
        lbs = sb("lbs", [128, 12], F32)
        identb = sb("identb", [128, 128], BF16)
        onesb = sb("onesb", [128, 128], BF16)
        smallc = sb("smallc", [128, 16], F32)
        ARENA = 72 * 1024
        arena = sb("arena", [128, ARENA // 2], BF16)
        pb = [st.enter_context(nc.psum_tensor(f"pb{i}", [128, 512], F32)) for i in range(8)]
        NEP = (NT + EP_TILES - 1) // EP_TILES
        sems = {e: [st.enter_context(nc.semaphore(f"s_{e}{i}")) for i in range(NEP)] for e in COMPUTE}
        wsem_all = [[P.new_dma_sem(st, f"w{j}_{i}") for i in range(R_SLOTS)] for j in range(NEP)]
        csem = [P.new_dma_sem(st, f"c{i}") for i in range(8)]
        xsem = P.new_dma_sem(st, "xs")
        osem = P.new_dma_sem(st, "os")
        psem = P.new_dma_sem(st, "ps")
        qsem = P.new_dma_sem(st, "qs")
        ksem = P.new_dma_sem(st, "ks")
        blk = st.enter_context(nc.Block())

        def av(lo, nbytes, dt, inner=None, parts=128):
            a = arena[0:parts, lo // 2:(lo + nbytes) // 2]
            if dt != BF16:
                a = a.bitcast(dt)
            if inner is not None:
                a = a.rearrange("p (a b) -> p a b", b=inner)
            return a

        K = 1024

        def cc(name, lo=0, n=None, parts=128):
            o, w = CST.d[name]
            n = w - lo if n is None else n
            return csb[0:parts, o + lo:o + lo + n]

        identf = cc("ident")
        bank_ctr = [0]

        def nb():
            b = pb[bank_ctr[0] % 7]
            bank_ctr[0] += 1
            return b

        ss_bank = pb[7]

        wctr = [0]

        def wload(key):
            i = UIDX[key]
            n = UNITS[i][1]
            s = wctr[0] % R_SLOTS
            wctr[0] += 1
            P.dma("sp", wring[:, s, 0:n], wbf_d[i, :, 0:n], wsem_all[P.epoch][s])
            return wring[:, s, 0:n]

        P.dma("sp", csb[:], cst_d, ksem)
        for i, (key, n) in enumerate(UNITS):
            P.dma("pool", wbf_d[i, :, 0:n], wall_d[i, :, 0:n], csem[i % 8])
        P.copy("dve", identb[:], identf)
        P.memset("dve", onesb[:], 1.0)
        P.ts("dve", gsc[:], cc("gT"), 0.5, ALU.mult)
        P.tt("dve", lbs[:, 0:4], cc("lbT", 0, 4), cc("lbT", 4, 4), ALU.subtract)
        P.actf(lbs[:, 4:8], lbs[:, 0:4], AF.Sigmoid)
        P.ts("dve", lbs[:, 8:12], lbs[:, 4:8], -1.0, ALU.mult, 1.0, ALU.add)
        for tns in (retS, retSb, hgS, hgSb, vswa):
            P.memset("dve", tns[:], 0.0)
        P.memset("dve", kT[:], 0.0)

        def gcol(l, n, k, scaled=False):
            c = (l * 7 + n) * 8 + k
            return gsc[:, c:c + 1] if scaled else cc("gT", c, 1)

        def rstd_from_ss(ss_ap, out_ap, inv_n):
            P.actf(out_ap, ss_ap, AF.Ln, bias=EPS, scale=inv_n)
            P.actf(out_ap, out_ap, AF.Exp, scale=-0.5)

        h_ss_ready = [False]

        def prenorm(l, n):
            if not h_ss_ready[0]:
                for k in range(8):
                    P.actf(sqb[:, k % 2, :], h[:, k, :], AF.Square)
                    P.mm(ss_bank[:], onesb[:], sqb[:, k % 2, :], start=(k == 0), stop=(k == 7))
            h_ss_ready[0] = False
            rstd_from_ss(ss_bank[:], rstd[:], 1.0 / D)
            for k in range(8):
                P.stt("dve", xn[:, k, :], h[:, k, :], gcol(l, n, k), rstd[:], ALU.mult, ALU.mult)

        pending_ss = []

        def flush_ss():
            for o in pending_ss:
                P.mm(ss_bank[:], onesb[:], sqb[:, o % 2, :], start=(o == 0), stop=(o == 7))
            pending_ss.clear()

        def evac_y(py, o):
            flush_ss()
            P.copy("dve", ybuf[:, o, :], py)
            P.actf(sqb[:, o % 2, :], ybuf[:, o, :], AF.Square)
            pending_ss.append(o)

        def postnorm_res(l, n, half, next_norm=True):
            flush_ss()
            rstd_from_ss(ss_bank[:], rstd[:], 1.0 / D)
            for k in range(8):
                P.tt("dve" if k % 2 else "pool", ybuf[:, k, :], ybuf[:, k, :], rstd[:], ALU.mult)
                P.stt("dve", h[:, k, :], ybuf[:, k, :], gcol(l, n, k, scaled=half), h[:, k, :], ALU.mult, ALU.add)
                if next_norm:
                    P.actf(sqb[:, k % 2, :], h[:, k, :], AF.Square)
                    P.mm(ss_bank[:], onesb[:], sqb[:, k % 2, :], start=(k == 0), stop=(k == 7))
            h_ss_ready[0] = next_norm

        def ffn(l, which):
            n0 = 0 if which == 1 else 4
            hid = av(0, 22 * K, BF16, inner=T)
            sg = av(22 * K, 4 * K, F32, inner=T)
            prenorm(l, n0)
            for u in range(11):
                w = wload((f"ffn{which}_in", l, u)).rearrange("p (k c) -> p k c", c=512)
                for jj in range(2):
                    j = 2 * u + jj
                    pg = nb()
                    pu = nb()
                    for k in range(8):
                        P.mm(pg[:], w[:, k, 128 * jj:128 * jj + 128], xn[:, k, :], start=(k == 0), stop=(k == 7))
                    for k in range(8):
                        P.mm(pu[:], w[:, k, 256 + 128 * jj:256 + 128 * jj + 128], xn[:, k, :], start=(k == 0), stop=(k == 7))
                    P.actf(sg[:, j % 2, :], pg[:], AF.Silu)
                    P.tt("dve", hid[:, j, :], sg[:, j % 2, :], pu[:], ALU.mult)
            for o in range(8):
                w = wload((f"ffn{which}_out", l, o)).rearrange("p (k c) -> p k c", c=128)
                py = nb()
                for j in range(NJ):
                    P.mm(py[:], w[:, j, :], hid[:, j, :], start=(j == 0), stop=(j == NJ - 1))
                evac_y(py[:], o)
            postnorm_res(l, n0 + 1, True, next_norm=(which == 1))

        def ple(l, t):
            for k in range(8):
                P.copy("act" if k % 2 else "dve", xn[:, k, :], h[:, k, :])
            P.dma("sp", ptile[:], p_d[l, t * T:(t + 1) * T, :].rearrange("(b p) f -> p b f", p=128), psem)
            for c in range(2):
                bk = nb()
                for b in range(4):
                    P.tr(bk[:, 128 * b:128 * b + 128], ptile[:, b, 128 * c:128 * c + 128], identf)
                P.copy("act", pTb[:, c, :], bk[:])
            w = wload(("pproj", l)).rearrange("p (k c) -> p k c", c=1024)
            for o in range(8):
                bk = nb()
                for c in range(2):
                    P.mm(bk[:], w[:, c, 128 * o:128 * o + 128], pTb[:, c, :], start=(c == 0), stop=(c == 1))
                P.copy("dve", ybuf[:, o, :], bk[:])
            sg = av(22 * K, 4 * K, F32, inner=T)
            for u in range(2):
                w = wload(("pgate", l, u)).rearrange("p (k c) -> p k c", c=512)
                for o4 in range(4):
                    o = 4 * u + o4
                    bk = nb()
                    for k in range(8):
                        P.mm(bk[:], w[:, k, 128 * o4:128 * o4 + 128], xn[:, k, :], start=(k == 0), stop=(k == 7))
                    P.actf(sg[:, o % 2, :], bk[:], AF.Sigmoid)
                    P.tt("dve", ybuf[:, o, :], ybuf[:, o, :], sg[:, o % 2, :], ALU.mult)
                    flush_ss()
                    P.actf(sqb[:, o % 2, :], ybuf[:, o, :], AF.Square)
                    pending_ss.append(o)
            postnorm_res(l, 6, False, next_norm=(l == 0))

        xio = av(0, 16 * K, F32, inner=D)

        def load_x(t):
            P.dma("sp", xio, x_d[t * T:(t + 1) * T, :].rearrange("(b p) f -> p b f", p=128), xsem)
            for k in range(8):
                bk = nb()
                for b in range(4):
                    P.tr(bk[:, 128 * b:128 * b + 128], xio[:, b, 128 * k:128 * k + 128], identf)
                P.copy("act" if k % 2 else "dve", h[:, k, :], bk[:])

        def store_out(t):
            for b in range(4):
                for hf in range(2):
                    bk = nb()
                    for kk in range(4):
                        k = 4 * hf + kk
                        P.tr(bk[:, 128 * kk:128 * kk + 128], h[:, k, 128 * b:128 * b + 128], identf)
                    P.copy("act" if hf else "dve", xio[:, b, 512 * hf:512 * hf + 512], bk[:])
            P.dma("sp", out_d[t * T:(t + 1) * T, :].rearrange("(b p) f -> p b f", p=128), xio, osem)

        TWO_PI = float(2 * np.pi)
        MAGIC = 12582912.0
        C1 = 6.28125
        C2 = float(2 * np.pi - 6.28125)
        PI_LO = 3.1415925

        def trig_tables(t):
            posi = av(16 * K, 2 * K, I32)
            posf = av(18 * K, 2 * K, F32)
            ang = av(20 * K, 2 * K, F32)
            nn = av(22 * K, 2 * K, F32)
            r2 = av(24 * K, 2 * K, F32)
            P.dma("sp", posi, pos_d[:, t * T:(t + 1) * T].partition_broadcast(128), qsem)
            P.copy("dve", posf, posi)
            for (inv, parts, cs, sn) in ((cc("invb", parts=64), 64, cosb, sinb), (cc("invc"), 128, cosc, sinc)):
                a = ang[0:parts]
                n_ = nn[0:parts]
                r = r2[0:parts]
                P.ts("dve", a, posf[0:parts], inv, ALU.mult)
                P.ts("dve", n_, a, float(1.0 / TWO_PI), ALU.mult, MAGIC, ALU.add)
                P.ts("dve", n_, n_, -MAGIC, ALU.add)
                P.stt("dve", a, n_, -C1, a, ALU.mult, ALU.add)
                P.stt("dve", a, n_, -C2, a, ALU.mult, ALU.add)
                P.ts("dve", a, a, -PI_LO, ALU.max, PI_LO, ALU.min)
                P.actf(sn[:], a, AF.Sin)
                P.ts("dve", r, a, float(np.pi / 2), ALU.add)
                P.ts("dve", n_, r, PI_LO, ALU.is_gt, TWO_PI, ALU.mult)
                P.tt("dve", r, r, n_, ALU.subtract)
                P.ts("dve", r, r, -PI_LO, ALU.max, PI_LO, ALU.min)
                P.actf(cs[:], r, AF.Sin)

        def even_mixer(t):
            l = 0
            prenorm(l, 2)
            catA = av(0, 4 * K, BF16, inner=T)
            catB = av(4 * K, 8 * K, BF16, inner=T, parts=64)
            qf = av(12 * K, 2 * K, F32)
            fb = av(14 * K, 2 * K, F32)
            kk_ = av(16 * K, 2 * K, F32)
            bb = av(18 * K, 2 * K, F32)
            e1 = av(20 * K, 2 * K, F32)
            e2 = av(22 * K, 2 * K, F32)
            qs = av(24 * K, 1 * K, BF16)
            ks = av(25 * K, 1 * K, BF16)
            kdT = av(26 * K, 1 * K, BF16)
            vtok = av(27 * K, 2 * K, BF16, inner=128, parts=64)
            gs = av(29 * K, 2 * K, F32)
            obuf = av(31 * K, 2 * K, F32)
            decb = av(33 * K, 32, F32)
            kdtok = av(33 * K + 512, 512, BF16, inner=128, parts=64)
            attb = av(34 * K, 256, BF16, inner=64, parts=64)
            lb = lbs[:, 4:8]
            omlb = lbs[:, 8:12]
            maskU = cc("maskU", parts=64)

            qraw = av(35 * K, 8 * K, F32, inner=T, parts=64)
            kraw = av(43 * K, 2 * K, F32, parts=64)
            ra = av(45 * K, 8 * K, F32, inner=T, parts=64)
            rb = av(53 * K, 8 * K, F32, inner=T, parts=64)
            qT = av(61 * K, 4 * K, BF16, inner=T, parts=64)
            smx_ = [av(65 * K, 1 * K, F32), av(68 * K, 1 * K, F32)]
            pexp_ = [av(66 * K, 1 * K, F32), av(69 * K, 1 * K, F32)]
            pnb_ = [av(67 * K, 512, BF16), av(70 * K, 512, BF16)]
            pTt_ = [av(67 * K + 512, 512, BF16, inner=128), av(70 * K + 512, 512, BF16, inner=128)]
            swa_it = [0]
            cos4 = cosb[:, :].unsqueeze(1).to_broadcast([64, 4, T])
            sin4 = sinb[:, :].unsqueeze(1).to_broadcast([64, 4, T])

            def hg_head(hh):
                w = wload(("hg", hh)).rearrange("p (k c) -> p k c", c=512)
                pq = nb()
                for k in range(8):
                    P.mm(pq[:], w[:, k, 0:128], xn[:, k, :], start=(k == 0), stop=(k == 7))
                P.actf(qf, pq[:], AF.Silu)
                pf = nb()
                for k in range(8):
                    P.mm(pf[:], w[:, k, 128:256], xn[:, k, :], start=(k == 0), stop=(k == 7))
                P.actf(fb, pf[:], AF.Sigmoid)
                P.ts("dve", fb, fb, omlb[:, hh:hh + 1], ALU.mult, lb[:, hh:hh + 1], ALU.add)
                P.ts("dve", kk_, fb, -1.0, ALU.mult, 1.0, ALU.add)
                P.actf(e1, fb, AF.Ln)
                P.emit("dve", lambda e, o=bb, m=cc("scanmask"), d=e1: e.tensor_tensor_scan(o, m, d, 0.0, ALU.mult, ALU.add),
                       reads=[cc("scanmask"), e1], writes=[bb])
                b3 = bb.rearrange("p (c t) -> p c t", t=64)
                P.actf(e1, bb, AF.Exp)
                P.tt("dve", qs, qf, e1, ALU.mult)
                P.actf(e2, bb, AF.Exp, scale=-1.0)
                P.tt("dve", ks, kk_, e2, ALU.mult)
                P.actf(decb, b3[:, :, 63], AF.Exp)
                e13 = e1.rearrange("p (c t) -> p c t", t=64)
                P.tt("dve", e13, b3[:, :, 63:64].to_broadcast([128, 8, 64]), b3, ALU.subtract)
                P.actf(e1, e1, AF.Exp)
                P.tt("dve", kdT, kk_, e1, ALU.mult)
                for c in range(8):
                    pv = nb()
                    for k in range(8):
                        P.mm(pv[0:64, 0:128], xn[:, k, 64 * c:64 * c + 64], w[:, k, 256:384], start=(k == 0), stop=(k == 7))
                    P.copy("act", vtok[:, c, :], pv[0:64, 0:128])
                pgt = nb()
                for k in range(8):
                    P.mm(pgt[:], w[:, k, 384:512], xn[:, k, :], start=(k == 0), stop=(k == 7))
                P.actf(gs, pgt[:], AF.Silu)
                for c in range(8):
                    cs_ = slice(64 * c, 64 * c + 64)
                    r = c % 2
                    pt_ = nb()
                    ptb = pt_[:].bitcast(BF16)
                    P.tr(ptb[0:64, 0:128], kdT[:, cs_], identb[:])
                    P.copy("act", kdtok[:, r, :], ptb[0:64, 0:128])
                    pa = nb()
                    P.mm(pa[0:64, 0:64], ks[:, cs_], qs[:, cs_])
                    P.tt("dve", attb[:, r, :], pa[0:64, 0:64], maskU, ALU.mult)
                    po = nb()
                    P.mm(po[:, 0:64], vtok[:, c, :], attb[:, r, :], start=True, stop=False)
                    P.mm(po[:, 0:64], hgSb[:, hh, :], qs[:, cs_], start=False, stop=True)
                    P.copy("act", obuf[:, cs_], po[:, 0:64])
                    pu_ = nb()
                    P.mm(pu_[:, 0:128], kdtok[:, r, :], vtok[:, c, :])
                    P.stt("dve", hgS[:, hh, :], hgS[:, hh, :], decb[:, c:c + 1], pu_[:, 0:128], ALU.mult, ALU.add)
                    P.copy("act", hgSb[:, hh, :], hgS[:, hh, :])
                P.actf(sqb[:, 0, :], obuf, AF.Square)
                pn_ = nb()
                P.mm(pn_[:], onesb[:], sqb[:, 0, :])
                rstd_from_ss(pn_[:], e2, 1.0 / 128)
                P.stt("dve", obuf, obuf, cc("ogh"), e2, ALU.mult, ALU.mult)
                P.tt("dve", catA[:, hh, :], obuf, gs, ALU.mult)
            def swa_proj(g):
                w = wload(("swa", g)).rearrange("p (k c) -> p k c", c=384)
                for q4 in range(4):
                    pq = nb()
                    for k in range(8):
                        P.mm(pq[0:64, :], w[:, k, 64 * q4:64 * q4 + 64], xn[:, k, :], start=(k == 0), stop=(k == 7))
                    P.copy("act", qraw[:, q4, :], pq[0:64, :])
                pk = nb()
                for k in range(8):
                    P.mm(pk[0:64, :], w[:, k, 256:320], xn[:, k, :], start=(k == 0), stop=(k == 7))
                P.copy("act", kraw, pk[0:64, :])
                for b in range(4):
                    pv = nb()
                    for k in range(8):
                        P.mm(pv[:, 0:64], xn[:, k, 128 * b:128 * b + 128], w[:, k, 320:384], start=(k == 0), stop=(k == 7))
                    P.copy("act", vswa[:, 1 + b, 64 * g:64 * g + 64], pv[:, 0:64])
                P.tt("dve", ra[:], qraw[:], cos4, ALU.mult)
                P.tt("dve", rb[0:32], qraw[32:64], sin4[32:64], ALU.mult)
                P.tt("dve", rb[32:64], qraw[0:32], sin4[0:32], ALU.mult)
                P.tt("dve", qT[0:32], ra[0:32], rb[0:32], ALU.subtract)
                P.tt("dve", qT[32:64], ra[32:64], rb[32:64], ALU.add)
                ra1 = ra[:, 0, :]
                rb1 = rb[:, 0, :]
                P.tt("dve", ra1, kraw, cosb[:], ALU.mult)
                P.tt("dve", rb1[0:32], kraw[32:64], sinb[32:64], ALU.mult)
                P.tt("dve", rb1[32:64], kraw[0:32], sinb[0:32], ALU.mult)
                P.tt("dve", kT[0:32, g, 128:640], ra1[0:32], rb1[0:32], ALU.subtract)
                P.tt("dve", kT[32:64, g, 128:640], ra1[32:64], rb1[32:64], ALU.add)

            def swa_attn(g):
                for b in range(4):
                    mask = cc("swam0") if (t == 0 and b == 0) else cc("swam")
                    for q4 in range(4):
                        hq = 4 * g + q4
                        sink = cc("sinks", hq, 1)
                        par = swa_it[0] % 2
                        swa_it[0] += 1
                        smx, pexp, pnb, pTt = smx_[par], pexp_[par], pnb_[par], pTt_[par]
                        sc = smallc[:, 8 * par:8 * par + 8]
                        ps_ = nb()
                        P.mm(ps_[:, 0:256], qT[:, q4, 128 * b:128 * b + 128], kT[:, g, 128 * b:128 * b + 256])
                        P.stt("dve", smx, ps_[:, 0:256], 0.125, mask, ALU.mult, ALU.add)
                        P.emit("dve", lambda e, o=sc[:, 0:1], i=smx: e.reduce_max(o, i, AX.X),
                               reads=[smx], writes=[sc[:, 0:1]])
                        P.ts("dve", sc[:, 1:2], sc[:, 0:1], sink, ALU.max, -1.0, ALU.mult)
                        P.memset("dve", sc[:, 2:3], 0.0)
                        P.actf(pexp, smx, AF.Exp, bias=sc[:, 1:2], accum_out=sc[:, 2:3])
                        P.actf(sc[:, 3:4], sink, AF.Exp, bias=sc[:, 1:2])
                        P.tt("dve", sc[:, 4:5], sc[:, 2:3], sc[:, 3:4], ALU.add)
                        P.emit("dve", lambda e, o=sc[:, 5:6], i=sc[:, 4:5]: e.reciprocal(o, i),
                               reads=[sc[:, 4:5]], writes=[sc[:, 5:6]])
                        P.ts("dve", pnb, pexp, sc[:, 5:6], ALU.mult)
                        pt_ = nb()
                        ptb = pt_[:].bitcast(BF16)
                        for j in range(2):
                            P.tr(ptb[:, 128 * j:128 * j + 128], pnb[:, 128 * j:128 * j + 128], identb[:])
                        P.copy("act", pTt.rearrange("p a b -> p (a b)"), ptb[:, 0:256])
                        po = nb()
                        for j in range(2):
                            P.mm(po[0:64, 0:128], vswa[:, b + j, 64 * g:64 * g + 64], pTt[:, j, :], start=(j == 0), stop=(j == 1))
                        P.copy("act", catB[:, hq, 128 * b:128 * b + 128], po[0:64, 0:128])
                P.copy("dve", kT[:, g, 0:128], kT[:, g, 512:640])

            swa_proj(0)
            hg_head(0)
            hg_head(1)
            swa_attn(0)
            swa_proj(1)
            hg_head(2)
            hg_head(3)
            swa_attn(1)
            P.copy("dve", vswa[:, 0, :], vswa[:, 4, :])
            for u in range(4):
                w = wload(("ewout", u))
                wa = w[:, 0:1024].rearrange("p (k c) -> p k c", c=256)
                wb_ = w[0:64, 1024:3072].rearrange("p (k c) -> p k c", c=256)
                for o2 in range(2):
                    o = 2 * u + o2
                    py = nb()
                    for hh in range(4):
                        P.mm(py[:], wa[:, hh, 128 * o2:128 * o2 + 128], catA[:, hh, :], start=(hh == 0), stop=False)
                    for hq in range(8):
                        P.mm(py[:], wb_[:, hq, 128 * o2:128 * o2 + 128], catB[:, hq, :], start=False, stop=(hq == 7))
                    evac_y(py[:], o)
            postnorm_res(l, 3, False)

        def odd_mixer(t):
            l = 1
            prenorm(l, 2)
            cat = av(0, 16 * K, BF16, inner=T)
            qraw = av(16 * K, 4 * K, F32, inner=T)
            kraw = av(20 * K, 4 * K, F32, inner=T)
            ta = av(24 * K, 4 * K, F32, inner=T)
            tb = av(28 * K, 4 * K, F32, inner=T)
            rr = av(32 * K, 4 * K, F32, inner=T)
            qr = av(36 * K, 2 * K, BF16, inner=T)
            qin = av(38 * K, 2 * K, BF16, inner=T)
            krb = av(40 * K, 2 * K, BF16, inner=T)
            kinT = av(42 * K, 2 * K, BF16, inner=T)
            ktok = av(44 * K, 2 * K, BF16, inner=256)
            vtok = av(46 * K, 4 * K, BF16, inner=512)
            gs = av(50 * K, 4 * K, BF16, inner=T)
            obuf = av(54 * K, 8 * K, F32, inner=T)
            attb = av(62 * K, 512, BF16, inner=128)
            e2 = av(63 * K, 2 * K, F32)
            cos2 = cosc[:, :].unsqueeze(1).to_broadcast([128, 2, T])
            sin2 = sinc[:, :].unsqueeze(1).to_broadcast([128, 2, T])

            def rope(raw):
                P.tt("dve", ta[:], raw[:], cos2, ALU.mult)
                P.tt("dve", tb[:], raw[:], sin2, ALU.mult)
                P.tt("dve", rr[:, 0, :], ta[:, 0, :], tb[:, 1, :], ALU.subtract)
                P.tt("dve", rr[:, 1, :], ta[:, 1, :], tb[:, 0, :], ALU.add)

            for hh in range(4):
                gq4 = cc("gq", 128 * hh, 128).unsqueeze(1).to_broadcast([128, 4, 128])
                gk4 = cc("gk", 128 * hh, 128).unsqueeze(1).to_broadcast([128, 4, 128])
                w = wload(("oq", hh)).rearrange("p (k c) -> p k c", c=256)
                for hf in range(2):
                    pq = nb()
                    for k in range(8):
                        P.mm(pq[:], w[:, k, 128 * hf:128 * hf + 128], xn[:, k, :], start=(k == 0), stop=(k == 7))
                    P.copy("act", qraw[:, hf, :], pq[:])
                w = wload(("ok", hh)).rearrange("p (k c) -> p k c", c=256)
                for hf in range(2):
                    pk = nb()
                    for k in range(8):
                        P.mm(pk[:], w[:, k, 128 * hf:128 * hf + 128], xn[:, k, :], start=(k == 0), stop=(k == 7))
                    P.copy("act", kraw[:, hf, :], pk[:])
                w = wload(("ov", hh)).rearrange("p (k c) -> p k c", c=512)
                for c in range(4):
                    pv = nb()
                    for k in range(8):
                        P.mm(pv[:], xn[:, k, 128 * c:128 * c + 128], w[:, k, :], start=(k == 0), stop=(k == 7))
                    P.copy("act", vtok[:, c, :], pv[:])
                w = wload(("og", hh)).rearrange("p (k c) -> p k c", c=512)
                for vc in range(4):
                    pg = nb()
                    for k in range(8):
                        P.mm(pg[:], w[:, k, 128 * vc:128 * vc + 128], xn[:, k, :], start=(k == 0), stop=(k == 7))
                    P.actf(gs[:, vc, :], pg[:], AF.Silu)
                rope(qraw)
                P.copy("act", qr[:], rr[:])
                for hf in range(2):
                    P.tt("dve", qin[:, hf, :].rearrange("p (c t) -> p c t", t=128),
                         rr[:, hf, :].rearrange("p (c t) -> p c t", t=128), gq4, ALU.mult)
                rope(kraw)
                P.ts("dve", rr[:], rr[:], 1.0 / 16.0, ALU.mult)
                P.copy("act", krb[:], rr[:])
                for hf in range(2):
                    P.tt("dve", kinT[:, hf, :].rearrange("p (c t) -> p c t", t=128),
                         rr[:, hf, :].rearrange("p (c t) -> p c t", t=128), gk4, ALU.mult)
                pt_ = nb()
                ptb = pt_[:].bitcast(BF16)
                for c in range(4):
                    for hf in range(2):
                        P.tr(ptb[:, 256 * c + 128 * hf:256 * c + 128 * hf + 128], kinT[:, hf, 128 * c:128 * c + 128], identb[:])
                P.copy("act", ktok.rearrange("p a b -> p (a b)"), ptb[:])
                dT = cc("decayT", 128 * hh, 128)
                for c in range(4):
                    cs_ = slice(128 * c, 128 * c + 128)
                    r = c % 2
                    pa = nb()
                    for hf in range(2):
                        P.mm(pa[:, 0:128], krb[:, hf, cs_], qr[:, hf, cs_], start=(hf == 0), stop=(hf == 1))
                    P.tt("dve", attb[:, r, :], pa[:, 0:128], dT, ALU.mult)
                    po = nb()
                    for vc in range(4):
                        vs = slice(128 * vc, 128 * vc + 128)
                        P.mm(po[:, vs], vtok[:, c, vs], attb[:, r, :], start=True, stop=False)
                        for hf in range(2):
                            P.mm(po[:, vs], retSb[:, 2 * hh + hf, vs], qin[:, hf, cs_], start=False, stop=(hf == 1))
                    P.copy("act", obuf[:, :, cs_], po[:].rearrange("p (a b) -> p a b", b=128))
                    for hf in range(2):
                        pu_ = nb()
                        P.mm(pu_[:], ktok[:, c, 128 * hf:128 * hf + 128], vtok[:, c, :])
                        P.stt("dve", retS[:, 2 * hh + hf, :], retS[:, 2 * hh + hf, :], GAMMA128[hh], pu_[:], ALU.mult, ALU.add)
                        P.copy("act", retSb[:, 2 * hh + hf, :], retS[:, 2 * hh + hf, :])
                pn_ = nb()
                for vc in range(4):
                    P.actf(sqb[:, vc % 2, :], obuf[:, vc, :], AF.Square)
                    P.mm(pn_[:], onesb[:], sqb[:, vc % 2, :], start=(vc == 0), stop=(vc == 3))
                rstd_from_ss(pn_[:], e2, 1.0 / 512)
                for vc in range(4):
                    P.stt("dve", obuf[:, vc, :], obuf[:, vc, :], cc("ogr", vc, 1), e2, ALU.mult, ALU.mult)
                    P.tt("dve", cat[:, 4 * hh + vc, :], obuf[:, vc, :], gs[:, vc, :], ALU.mult)
            for u in range(4):
                w = wload(("owout", u)).rearrange("p (k c) -> p k c", c=256)
                for o2 in range(2):
                    o = 2 * u + o2
                    py = nb()
                    for j in range(16):
                        P.mm(py[:], w[:, j, 128 * o2:128 * o2 + 128], cat[:, j, :], start=(j == 0), stop=(j == 15))
                    evac_y(py[:], o)
            postnorm_res(l, 3, False)

        for t in range(NT):
            P.epoch = t // EP_TILES
            load_x(t)
            trig_tables(t)
            stage_fns = [lambda: ffn(0, 1), lambda: even_mixer(t), lambda: ffn(0, 2), lambda: ple(0, t),
                         lambda: ffn(1, 1), lambda: odd_mixer(t), lambda: ffn(1, 2), lambda: ple(1, t)]
            for f in stage_fns[:nstage]:
                f()
            store_out(t)
        stats = P.finalize(sems, blk)
    return nc, stats


_NC_CACHE = {}


def kernel(**inputs):
    NT = inputs["x"].shape[1] // T
    if NT not in _NC_CACHE:
        _NC_CACHE[NT] = build_nc(NT)[0]
    nc = _NC_CACHE[NT]
    wall = pack_weights(inputs)
    cst = pack_consts(inputs)
    x = np.ascontiguousarray(inputs["x"], dtype=np.float32)
    p = np.ascontiguousarray(inputs["p"], dtype=np.float32)
    pos = np.ascontiguousarray(inputs["positions"], dtype=np.int32)
    in_maps = []
    for c in range(8):
        in_maps.append({"x": x[c], "p": np.ascontiguousarray(p[:, c]), "pos": pos[c:c + 1],
                        "wall": wall, "cst": cst})
    res = run_bass_kernel_spmd(nc, in_maps, core_ids=list(range(8)))
    return np.stack([r["out"] for r in res.results], axis=0)
```

```python
from contextlib import ExitStack
import numpy as np
import concourse.bass as bass
import concourse.mybir as mybir
from concourse.bass_utils import run_bass_kernel_spmd

F32 = mybir.dt.float32
BF16 = mybir.dt.bfloat16
I32 = mybir.dt.int32
AF = mybir.ActivationFunctionType
ALU = mybir.AluOpType
AX = mybir.AxisListType

_DSZ = {F32: 4, BF16: 2, I32: 4}
COMPUTE = ("pe", "act", "dve", "pool")
SAME_ENGINE_SYNC = True


def _rng(ap):
    sz = _DSZ[ap.dtype]
    steps = ap.ap
    off = int(ap.offset)
    if str(ap.space) == "DRAM":
        ext = sum((c - 1) * s for s, c in steps)
        return (ap.name, off * sz, (off + ext + 1) * sz)
    if str(ap.space) == "PSUM":
        return (ap.name, 0, 2048)
    pstep = steps[0][0]
    lo = off % pstep if pstep > 0 else off
    ext = sum((c - 1) * s for s, c in steps[1:])
    return (ap.name, lo * sz, (lo + ext + 1) * sz)


class Op:
    __slots__ = ("eng", "fn", "deps", "sem", "cnt", "needs_inc", "inc_idx", "is_dma", "seq", "epoch")

    def __init__(self, eng, fn, is_dma=False):
        self.seq = 0
        self.epoch = 0
        self.eng = eng
        self.fn = fn
        self.deps = set()
        self.sem = None
        self.cnt = 0
        self.needs_inc = False
        self.inc_idx = 0
        self.is_dma = is_dma


class DmaSem:
    def __init__(self, handle):
        self.h = handle
        self.n = 0
        self.last = None


class Prog:
    def __init__(self, nc):
        self.nc = nc
        self.streams = {e: [] for e in ("pe", "act", "dve", "pool", "sp")}
        self.acc = {}
        self.dma_sems = []
        self.epoch = 0

    def _add(self, op):
        op.seq = len(self.streams[op.eng])
        op.epoch = self.epoch
        last = {}
        keep = set()
        for d in op.deps:
            if d.is_dma:
                keep.add(d)
            elif d.eng not in last or d.seq > last[d.eng].seq:
                last[d.eng] = d
        op.deps = keep | set(last.values())
        self.streams[op.eng].append(op)

    def new_dma_sem(self, stack, name):
        s = DmaSem(stack.enter_context(self.nc.semaphore(name)))
        self.dma_sems.append(s)
        return s

    def _track(self, op, reads, writes):
        writes = list(writes) + [ap for ap in reads if str(ap.space) == "PSUM"]
        reads = [ap for ap in reads if str(ap.space) != "PSUM"]
        for ap in reads:
            n, lo, hi = _rng(ap)
            a = self.acc.setdefault(n, ([], []))
            for (l, h, o) in a[0]:
                if l < hi and lo < h:
                    op.deps.add(o)
            a[1].append((lo, hi, op))
        for ap in writes:
            n, lo, hi = _rng(ap)
            a = self.acc.setdefault(n, ([], []))
            for (l, h, o) in a[0]:
                if l < hi and lo < h and o is not op:
                    op.deps.add(o)
            for (l, h, o) in a[1]:
                if l < hi and lo < h and o is not op:
                    op.deps.add(o)
            a[0][:] = [t for t in a[0] if not (lo <= t[0] and t[1] <= hi)]
            a[1][:] = [t for t in a[1] if not (lo <= t[0] and t[1] <= hi)]
            a[0].append((lo, hi, op))

    def emit(self, eng, fn, reads=(), writes=()):
        op = Op(eng, fn)
        self._track(op, reads, writes)
        self._add(op)
        return op

    def dma(self, queue, out, in_, sem):
        op = Op(queue, lambda e: e.dma_start(out=out, in_=in_), is_dma=True)
        if sem.last is not None:
            op.deps.add(sem.last)
        sem.n += 1
        sem.last = op
        op.sem = sem
        op.cnt = sem.n
        self._track(op, [in_], [out])
        self._add(op)
        return op

    def mm(self, out, lhsT, rhs, start=True, stop=True):
        return self.emit("pe", lambda e: e.matmul(out, lhsT, rhs, start=start, stop=stop),
                         reads=[lhsT, rhs], writes=[out])

    def tr(self, out, in_, ident):
        return self.emit("pe", lambda e: e.transpose(out, in_, ident), reads=[in_, ident], writes=[out])

    def actf(self, out, in_, func, bias=None, scale=None, accum_out=None):
        kw = {}
        rd = [in_]
        wr = [out]
        if bias is not None:
            kw["bias"] = bias
            if not isinstance(bias, (int, float)):
                rd.append(bias)
        if scale is not None:
            kw["scale"] = scale
            if not isinstance(scale, (int, float)):
                rd.append(scale)
        if accum_out is not None:
            kw["accum_out"] = accum_out
            wr.append(accum_out)
        return self.emit("act", lambda e: e.activation(out, in_, func, **kw), reads=rd, writes=wr)

    def tt(self, eng, out, in0, in1, op):
        return self.emit(eng, lambda e: e.tensor_tensor(out, in0, in1, op), reads=[in0, in1], writes=[out])

    def ts(self, eng, out, in0, s1, op0, s2=None, op1=None):
        rd = [in0] + [s for s in (s1, s2) if s is not None and not isinstance(s, (int, float))]
        kw = {}
        if op1 is not None:
            kw["op1"] = op1
        return self.emit(eng, lambda e: e.tensor_scalar(out, in0, s1, s2, op0, **kw), reads=rd, writes=[out])

    def stt(self, eng, out, in0, scalar, in1, op0, op1):
        rd = [in0, in1] + ([scalar] if not isinstance(scalar, (int, float)) else [])
        return self.emit(eng, lambda e: e.scalar_tensor_tensor(out, in0, scalar, in1, op0, op1),
                         reads=rd, writes=[out])

    def copy(self, eng, out, in_):
        if eng == "act":
            return self.emit("act", lambda e: e.copy(out, in_), reads=[in_], writes=[out])
        return self.emit(eng, lambda e: e.tensor_copy(out, in_), reads=[in_], writes=[out])

    def memset(self, eng, ap, val):
        return self.emit(eng, lambda e: e.memset(ap, val), writes=[ap])

    def finalize(self, sems, block):
        def needs_sem(op, d):
            if d.is_dma:
                return True
            if d.eng == op.eng and not op.is_dma:
                if d.eng == "pe" or not SAME_ENGINE_SYNC:
                    return False
            return True

        for st in self.streams.values():
            for op in st:
                for d in op.deps:
                    if not d.is_dma and needs_sem(op, d):
                        d.needs_inc = True
        self.max_inc = {}
        for eng in COMPUTE:
            k = {}
            for op in self.streams[eng]:
                if not op.is_dma and op.needs_inc:
                    k[op.epoch] = k.get(op.epoch, 0) + 1
                    op.inc_idx = k[op.epoch]
            self.max_inc[eng] = max(list(k.values()) + [0])
        stats = {}
        nc = self.nc
        engobj = {"pe": nc.tensor, "act": nc.scalar, "dve": nc.vector, "pool": nc.gpsimd, "sp": nc.sync}

        def run(eng):
            e = engobj[eng]
            waited = {}
            nw = 0
            for op in self.streams[eng]:
                need = {}
                for d in op.deps:
                    if not needs_sem(op, d):
                        continue
                    if d.is_dma:
                        key, v, h = ("d", id(d.sem)), d.cnt * 16, d.sem.h
                    else:
                        key, v, h = ("c", d.eng, d.epoch), d.inc_idx, sems[d.eng][d.epoch]
                    if v > need.get(key, (0, None))[0]:
                        need[key] = (v, h)
                for key, (v, h) in need.items():
                    if v > waited.get(key, 0):
                        e.wait_ge(h, v)
                        waited[key] = v
                        nw += 1
                ins = op.fn(e)
                if op.is_dma:
                    ins.then_inc(op.sem.h, 16)
                elif op.needs_inc:
                    ins.then_inc(sems[eng][op.epoch], 1)
            if eng == "sp":
                for s in self.dma_sems:
                    if s.n:
                        e.wait_ge(s.h, s.n * 16)
            stats[eng] = (len(self.streams[eng]), nw)

        block.tensor(lambda _e: run("pe"))
        block.scalar(lambda _e: run("act"))
        block.vector(lambda _e: run("dve"))
        block.gpsimd(lambda _e: run("pool"))
        block.sync(lambda _e: run("sp"))
        return stats


D = 1024
T = 512
DFF = 2816
EPS = 1e-6
NJ = DFF // 128
R_SLOTS = 4
EP_TILES = 2
WCOLS = 4096


class Cols:
    def __init__(self):
        self.n = 0
        self.d = {}

    def add(self, name, w):
        self.d[name] = (self.n, w)
        self.n += w


CST = Cols()
for _n, _w in (("gT", 112), ("lbT", 8), ("ogh", 1), ("ogr", 4), ("sinks", 8), ("invb", 1), ("invc", 1),
               ("ident", 128), ("maskU", 64), ("scanmask", 512), ("swam", 256), ("swam0", 256),
               ("decayT", 512), ("gq", 512), ("gk", 512)):
    CST.add(_n, _w)
NCST = CST.n


def unit_list():
    u = []
    for l in range(2):
        u += [(("ffn1_in", l, i), 4096) for i in range(11)]
        u += [(("ffn1_out", l, i), 2816) for i in range(8)]
        if l == 0:
            u += [(("hg", i), 4096) for i in range(4)]
            u += [(("swa", i), 3072) for i in range(2)]
            u += [(("ewout", i), 3072) for i in range(4)]
        else:
            for hh in range(4):
                u += [(("oq", hh), 2048), (("ok", hh), 2048), (("ov", hh), 4096), (("og", hh), 4096)]
            u += [(("owout", i), 4096) for i in range(4)]
        u += [(("ffn2_in", l, i), 4096) for i in range(11)]
        u += [(("ffn2_out", l, i), 2816) for i in range(8)]
        u += [(("pproj", l), 2048), (("pgate", l, 0), 4096), (("pgate", l, 1), 4096)]
    return u


UNITS = unit_list()
NU = len(UNITS)
UIDX = {k: i for i, (k, _) in enumerate(UNITS)}


def _kmaj(W, cols):
    kc = W.shape[0] // 128
    return np.ascontiguousarray(W.reshape(kc, 128, W.shape[1])[:, :, cols].transpose(1, 0, 2)).reshape(128, -1)


def pack_weights(inp):
    wall = np.zeros((NU, 128, WCOLS), np.float32)
    ar = np.arange
    for (key, n) in UNITS:
        i = UIDX[key]
        k0 = key[0]
        if k0 in ("ffn1_in", "ffn2_in"):
            W = inp[k0[:4] + "_w_in"][key[1]]
            u = key[2]
            cols = np.concatenate([ar(256 * u, 256 * u + 256), DFF + ar(256 * u, 256 * u + 256)])
            blk = _kmaj(W, cols)
        elif k0 in ("ffn1_out", "ffn2_out"):
            W = inp[k0[:4] + "_w_out"][key[1]]
            blk = _kmaj(W, ar(128 * key[2], 128 * key[2] + 128))
        elif k0 == "hg":
            W = inp["even_w_in"][0]
            hh = key[1]
            cols = np.concatenate([g * 512 + hh * 128 + ar(128) for g in range(4)])
            blk = _kmaj(W, cols)
        elif k0 == "swa":
            W = inp["even_w_in"][0]
            g = key[1]
            cols = np.concatenate([2048 + 256 * g + ar(256), 2560 + 64 * g + ar(64), 2688 + 64 * g + ar(64)])
            blk = _kmaj(W, cols)
        elif k0 == "ewout":
            W = inp["even_w_out"][0]
            cols = ar(256 * key[1], 256 * key[1] + 256)
            a = _kmaj(W[0:512], cols)
            b = np.zeros((128, 8, 256), np.float32)
            b[0:64] = W[512:1024].reshape(8, 64, 1024)[:, :, cols].transpose(1, 0, 2)
            blk = np.concatenate([a, b.reshape(128, -1)], axis=1)
        elif k0 in ("oq", "ok", "ov", "og"):
            W = inp["odd_w_in"][0]
            hh = key[1]
            base = {"oq": 0, "ok": 1024, "ov": 2048, "og": 4096}[k0]
            wd = 256 if k0 in ("oq", "ok") else 512
            blk = _kmaj(W, base + hh * wd + ar(wd))
        elif k0 == "owout":
            W = inp["odd_w_out"][0]
            blk = _kmaj(W, ar(256 * key[1], 256 * key[1] + 256))
        elif k0 == "pproj":
            blk = _kmaj(inp["ple_w_proj"][key[1]], ar(1024))
        elif k0 == "pgate":
            blk = _kmaj(inp["ple_w_gate"][key[1]], ar(512 * key[2], 512 * key[2] + 512))
        assert blk.shape == (128, n), (key, blk.shape, n)
        wall[i, :, :n] = blk
    return wall


def pack_consts(inp):
    c = np.zeros((128, NCST), np.float32)

    def put(name, arr):
        o, w = CST.d[name]
        arr = np.asarray(arr, np.float32)
        c[: arr.shape[0], o:o + w] = arr.reshape(arr.shape[0], w)

    put("gT", inp["norm_g"].reshape(2, 7, 8, 128).transpose(3, 0, 1, 2).reshape(128, 112))
    put("lbT", inp["hgrn_lb"].reshape(2, 4, 128).transpose(2, 0, 1).reshape(128, 8))
    put("ogh", inp["hgrn_onorm_g"].reshape(128, 1))
    put("ogr", inp["ret_onorm_g"].reshape(4, 128).T)
    put("sinks", np.broadcast_to(inp["attn_sinks"].reshape(1, 8), (128, 8)))
    inv_b = (np.float32(10000.0) ** (-np.arange(0, 64, 2, dtype=np.float32) / np.float32(64))).astype(np.float32)
    inv_c = (np.float32(10000.0) ** (-np.linspace(0.0, 1.0, 128, dtype=np.float32))).astype(np.float32)
    put("invb", np.concatenate([inv_b, inv_b]).reshape(64, 1))
    put("invc", inv_c.reshape(128, 1))
    put("ident", np.eye(128, dtype=np.float32))
    s = np.arange(64)
    put("maskU", (s[:, None] <= s[None, :]).astype(np.float32))
    sm = np.ones(512, np.float32)
    sm[::64] = 0.0
    put("scanmask", np.broadcast_to(sm, (128, 512)))
    qi = np.arange(128)[:, None]
    kj = np.arange(256)[None, :]
    diff = qi + 128 - kj
    valid = (diff >= 0) & (diff < 128)
    put("swam", np.where(valid, 0.0, -30000.0))
    put("swam0", np.where(valid & (kj >= 128), 0.0, -30000.0))
    hh = np.arange(4, dtype=np.float64)
    lg = np.log1p(-np.exp2(-5.0 - hh))
    idx = np.arange(128, dtype=np.float64)
    dcs = idx[None, :] - idx[:, None]
    dec = np.where(dcs[None] >= 0, np.exp(np.maximum(dcs, 0.0)[None] * lg[:, None, None]), 0.0)
    put("decayT", dec.transpose(1, 0, 2).reshape(128, 512))
    put("gq", np.broadcast_to(np.exp((idx + 1.0)[None, :] * lg[:, None]).reshape(1, 512), (128, 512)))
    put("gk", np.broadcast_to(np.exp((127.0 - idx)[None, :] * lg[:, None]).reshape(1, 512), (128, 512)))
    return c


GAMMA128 = [float(np.exp(128.0 * np.log1p(-np.exp2(-5.0 - h)))) for h in range(4)]


def build_nc(NT, nstage=8):
    S = NT * T
    nc = bass.Bass("TRN2", target_bir_lowering=False)
    x_d = nc.dram_tensor("x", [S, D], F32, kind="ExternalInput").ap()
    p_d = nc.dram_tensor("p", [2, S, 256], F32, kind="ExternalInput").ap()
    pos_d = nc.dram_tensor("pos", [1, S], I32, kind="ExternalInput").ap()
    wall_d = nc.dram_tensor("wall", [NU, 128, WCOLS], F32, kind="ExternalInput").ap()
    cst_d = nc.dram_tensor("cst", [128, NCST], F32, kind="ExternalInput").ap()
    out_d = nc.dram_tensor("out", [S, D], F32, kind="ExternalOutput").ap()
    wbf_d = nc.dram_tensor("wbf", [NU, 128, WCOLS], BF16, kind="Internal").ap()

    with ExitStack() as st:
        P = Prog(nc)
        sb = lambda n, s, d: st.enter_context(nc.sbuf_tensor(n, s, d))
        h = sb("h", [128, 8, T], F32)
        xn = sb("xn", [128, 8, T], BF16)
        ybuf = sb("ybuf", [128, 8, T], F32)
        sqb = sb("sqb", [128, 2, T], BF16)
        rstd = sb("rstd", [128, T], F32)
        wring = sb("wring", [128, R_SLOTS, WCOLS], BF16)
        ptile = sb("ptile", [128, 4, 256], F32)
        pTb = sb("pTb", [128, 2, T], BF16)
        cosb = sb("cosb", [64, T], F32)
        sinb = sb("sinb", [64, T], F32)
        cosc = sb("cosc", [128, T], F32)
        sinc = sb("sinc", [128, T], F32)
        retS = sb("retS", [128, 8, 512], F32)
        retSb = sb("retSb", [128, 8, 512], BF16)
        hgS = sb("hgS", [128, 4, 128], F32)
        hgSb = sb("hgSb", [128, 4, 128], BF16)
        kT = sb("kT", [64, 2, 640], BF16)
        vswa = sb("vswa", [128, 5, 128], BF16)
        csb = sb("csb", [128, NCST], F32)
        gsc = sb("gsc", [128, 112], F32)
        lbs = sb("lbs", [128, 12], F32)
        identb = sb("identb", [128, 128], BF16)
        onesb = sb("onesb", [128, 128], BF16)
        smallc = sb("smallc", [128, 16], F32)
        ARENA = 72 * 1024
        arena = sb("arena", [128, ARENA // 2], BF16)
        pb = [st.enter_context(nc.psum_tensor(f"pb{i}", [128, 512], F32)) for i in range(8)]
        NEP = (NT + EP_TILES - 1) // EP_TILES
        sems = {e: [st.enter_context(nc.semaphore(f"s_{e}{i}")) for i in range(NEP)] for e in COMPUTE}
        wsem_all = [[P.new_dma_sem(st, f"w{j}_{i}") for i in range(R_SLOTS)] for j in range(NEP)]
        csem = [P.new_dma_sem(st, f"c{i}") for i in range(8)]
        xsem = P.new_dma_sem(st, "xs")
        osem = P.new_dma_sem(st, "os")
        psem = P.new_dma_sem(st, "ps")
        qsem = P.new_dma_sem(st, "qs")
        ksem = P.new_dma_sem(st, "ks")
        blk = st.enter_context(nc.Block())

        def av(lo, nbytes, dt, inner=None, parts=128):
            a = arena[0:parts, lo // 2:(lo + nbytes) // 2]
            if dt != BF16:
                a = a.bitcast(dt)
            if inner is not None:
                a = a.rearrange("p (a b) -> p a b", b=inner)
            return a

        K = 1024

        def cc(name, lo=0, n=None, parts=128):
            o, w = CST.d[name]
            n = w - lo if n is None else n
            return csb[0:parts, o + lo:o + lo + n]

        identf = cc("ident")
        bank_ctr = [0]

        def nb():
            b = pb[bank_ctr[0] % 7]
            bank_ctr[0] += 1
            return b

        ss_bank = pb[7]

        wctr = [0]

        def wload(key):
            i = UIDX[key]
            n = UNITS[i][1]
            s = wctr[0] % R_SLOTS
            wctr[0] += 1
            P.dma("sp", wring[:, s, 0:n], wbf_d[i, :, 0:n], wsem_all[P.epoch][s])
            return wring[:, s, 0:n]

        P.dma("sp", csb[:], cst_d, ksem)
        for i, (key, n) in enumerate(UNITS):
            P.dma("pool", wbf_d[i, :, 0:n], wall_d[i, :, 0:n], csem[i % 8])
        P.copy("dve", identb[:], identf)
        P.memset("dve", onesb[:], 1.0)
        P.ts("dve", gsc[:], cc("gT"), 0.5, ALU.mult)
        P.tt("dve", lbs[:, 0:4], cc("lbT", 0, 4), cc("lbT", 4, 4), ALU.subtract)
        P.actf(lbs[:, 4:8], lbs[:, 0:4], AF.Sigmoid)
        P.ts("dve", lbs[:, 8:12], lbs[:, 4:8], -1.0, ALU.mult, 1.0, ALU.add)
        for tns in (retS, retSb, hgS, hgSb, vswa):
            P.memset("dve", tns[:], 0.0)
        P.memset("dve", kT[:], 0.0)

        def gcol(l, n, k, scaled=False):
            c = (l * 7 + n) * 8 + k
            return gsc[:, c:c + 1] if scaled else cc("gT", c, 1)

        def rstd_from_ss(ss_ap, out_ap, inv_n):
            P.actf(out_ap, ss_ap, AF.Ln, bias=EPS, scale=inv_n)
            P.actf(out_ap, out_ap, AF.Exp, scale=-0.5)

        h_ss_ready = [False]

        def prenorm(l, n):
            if not h_ss_ready[0]:
                for k in range(8):
                    P.actf(sqb[:, k % 2, :], h[:, k, :], AF.Square)
                    P.mm(ss_bank[:], onesb[:], sqb[:, k % 2, :], start=(k == 0), stop=(k == 7))
            h_ss_ready[0] = False
            rstd_from_ss(ss_bank[:], rstd[:], 1.0 / D)
            for k in range(8):
                P.stt("dve", xn[:, k, :], h[:, k, :], gcol(l, n, k), rstd[:], ALU.mult, ALU.mult)

        pending_ss = []

        def flush_ss():
            for o in pending_ss:
                P.mm(ss_bank[:], onesb[:], sqb[:, o % 2, :], start=(o == 0), stop=(o == 7))
            pending_ss.clear()

        def evac_y(py, o):
            flush_ss()
            P.copy("dve", ybuf[:, o, :], py)
            P.actf(sqb[:, o % 2, :], ybuf[:, o, :], AF.Square)
            pending_ss.append(o)

        def postnorm_res(l, n, half, next_norm=True):
            flush_ss()
            rstd_from_ss(ss_bank[:], rstd[:], 1.0 / D)
            for k in range(8):
                P.tt("dve" if k % 2 else "pool", ybuf[:, k, :], ybuf[:, k, :], rstd[:], ALU.mult)
                P.stt("dve", h[:, k, :], ybuf[:, k, :], gcol(l, n, k, scaled=half), h[:, k, :], ALU.mult, ALU.add)
                if next_norm:
                    P.actf(sqb[:, k % 2, :], h[:, k, :], AF.Square)
                    P.mm(ss_bank[:], onesb[:], sqb[:, k % 2, :], start=(k == 0), stop=(k == 7))
            h_ss_ready[0] = next_norm

        def ffn(l, which, mid_hook=None):
            n0 = 0 if which == 1 else 4
            hid = av(0, 22 * K, BF16, inner=T)
            sg = av(22 * K, 4 * K, F32, inner=T)
            prenorm(l, n0)
            for u in range(11):
                w = wload((f"ffn{which}_in", l, u)).rearrange("p (k c) -> p k c", c=512)
                for jj in range(2):
                    j = 2 * u + jj
                    pg = nb()
                    pu = nb()
                    for k in range(8):
                        P.mm(pg[:], w[:, k, 128 * jj:128 * jj + 128], xn[:, k, :], start=(k == 0), stop=(k == 7))
                    for k in range(8):
                        P.mm(pu[:], w[:, k, 256 + 128 * jj:256 + 128 * jj + 128], xn[:, k, :], start=(k == 0), stop=(k == 7))
                    P.actf(sg[:, j % 2, :], pg[:], AF.Silu)
                    P.tt("dve", hid[:, j, :], sg[:, j % 2, :], pu[:], ALU.mult)
            if mid_hook is not None:
                mid_hook()
            for o in range(8):
                w = wload((f"ffn{which}_out", l, o)).rearrange("p (k c) -> p k c", c=128)
                py = nb()
                for j in range(NJ):
                    P.mm(py[:], w[:, j, :], hid[:, j, :], start=(j == 0), stop=(j == NJ - 1))
                evac_y(py[:], o)
            postnorm_res(l, n0 + 1, True, next_norm=(which == 1))

        def ple(l, t):
            for k in range(8):
                P.copy("act" if k % 2 else "dve", xn[:, k, :], h[:, k, :])
            P.dma("sp", ptile[:], p_d[l, t * T:(t + 1) * T, :].rearrange("(b p) f -> p b f", p=128), psem)
            for c in range(2):
                bk = nb()
                for b in range(4):
                    P.tr(bk[:, 128 * b:128 * b + 128], ptile[:, b, 128 * c:128 * c + 128], identf)
                P.copy("act", pTb[:, c, :], bk[:])
            w = wload(("pproj", l)).rearrange("p (k c) -> p k c", c=1024)
            for o in range(8):
                bk = nb()
                for c in range(2):
                    P.mm(bk[:], w[:, c, 128 * o:128 * o + 128], pTb[:, c, :], start=(c == 0), stop=(c == 1))
                P.copy("dve", ybuf[:, o, :], bk[:])
            sg = av(22 * K, 4 * K, F32, inner=T)
            for u in range(2):
                w = wload(("pgate", l, u)).rearrange("p (k c) -> p k c", c=512)
                for o4 in range(4):
                    o = 4 * u + o4
                    bk = nb()
                    for k in range(8):
                        P.mm(bk[:], w[:, k, 128 * o4:128 * o4 + 128], xn[:, k, :], start=(k == 0), stop=(k == 7))
                    P.actf(sg[:, o % 2, :], bk[:], AF.Sigmoid)
                    P.tt("dve", ybuf[:, o, :], ybuf[:, o, :], sg[:, o % 2, :], ALU.mult)
                    flush_ss()
                    P.actf(sqb[:, o % 2, :], ybuf[:, o, :], AF.Square)
                    pending_ss.append(o)
            postnorm_res(l, 6, False, next_norm=(l == 0))

        xio = av(0, 16 * K, F32, inner=D)

        def load_x(t):
            P.dma("sp", xio, x_d[t * T:(t + 1) * T, :].rearrange("(b p) f -> p b f", p=128), xsem)
            for k in range(8):
                bk = nb()
                for b in range(4):
                    P.tr(bk[:, 128 * b:128 * b + 128], xio[:, b, 128 * k:128 * k + 128], identf)
                P.copy("act" if k % 2 else "dve", h[:, k, :], bk[:])

        def store_out(t):
            for b in range(4):
                for hf in range(2):
                    bk = nb()
                    for kk in range(4):
                        k = 4 * hf + kk
                        P.tr(bk[:, 128 * kk:128 * kk + 128], h[:, k, 128 * b:128 * b + 128], identf)
                    P.copy("act" if hf else "dve", xio[:, b, 512 * hf:512 * hf + 512], bk[:])
            P.dma("sp", out_d[t * T:(t + 1) * T, :].rearrange("(b p) f -> p b f", p=128), xio, osem)

        TWO_PI = float(2 * np.pi)
        MAGIC = 12582912.0
        C1 = 6.28125
        C2 = float(2 * np.pi - 6.28125)
        PI_LO = 3.1415925

        def trig_tables(t):
            posi = av(26 * K, 2 * K, I32)
            posf = av(28 * K, 2 * K, F32)
            ang = av(30 * K, 2 * K, F32)
            nn = av(32 * K, 2 * K, F32)
            r2 = av(34 * K, 2 * K, F32)
            P.dma("sp", posi, pos_d[:, t * T:(t + 1) * T].partition_broadcast(128), qsem)
            P.copy("dve", posf, posi)
            for (inv, parts, cs, sn) in ((cc("invb", parts=64), 64, cosb, sinb), (cc("invc"), 128, cosc, sinc)):
                a = ang[0:parts]
                n_ = nn[0:parts]
                r = r2[0:parts]
                P.ts("dve", a, posf[0:parts], inv, ALU.mult)
                P.ts("dve", n_, a, float(1.0 / TWO_PI), ALU.mult, MAGIC, ALU.add)
                P.ts("dve", n_, n_, -MAGIC, ALU.add)
                P.stt("dve", a, n_, -C1, a, ALU.mult, ALU.add)
                P.stt("dve", a, n_, -C2, a, ALU.mult, ALU.add)
                P.ts("dve", a, a, -PI_LO, ALU.max, PI_LO, ALU.min)
                P.actf(sn[:], a, AF.Sin)
                P.ts("dve", r, a, float(np.pi / 2), ALU.add)
                P.ts("dve", n_, r, PI_LO, ALU.is_gt, TWO_PI, ALU.mult)
                P.tt("dve", r, r, n_, ALU.subtract)
                P.ts("dve", r, r, -PI_LO, ALU.max, PI_LO, ALU.min)
                P.actf(cs[:], r, AF.Sin)

        def even_mixer(t):
            l = 0
            prenorm(l, 2)
            catA = av(0, 4 * K, BF16, inner=T)
            catB = av(4 * K, 8 * K, BF16, inner=T, parts=64)
            qf = av(12 * K, 2 * K, F32)
            fb = av(14 * K, 2 * K, F32)
            kk_ = av(16 * K, 2 * K, F32)
            bb = av(18 * K, 2 * K, F32)
            e1 = av(20 * K, 2 * K, F32)
            e2 = av(22 * K, 2 * K, F32)
            qs = av(24 * K, 1 * K, BF16)
            ks = av(25 * K, 1 * K, BF16)
            kdT = av(26 * K, 1 * K, BF16)
            vtok = av(27 * K, 2 * K, BF16, inner=128, parts=64)
            gs = av(29 * K, 2 * K, F32)
            obuf = av(31 * K, 2 * K, F32)
            decb = av(33 * K, 32, F32)
            kdtok = av(33 * K + 512, 512, BF16, inner=128, parts=64)
            attb = av(34 * K, 256, BF16, inner=64, parts=64)
            lb = lbs[:, 4:8]
            omlb = lbs[:, 8:12]
            maskU = cc("maskU", parts=64)

            qraw = av(35 * K, 8 * K, F32, inner=T, parts=64)
            kraw = av(43 * K, 2 * K, F32, parts=64)
            ra = av(45 * K, 8 * K, F32, inner=T, parts=64)
            rb = av(53 * K, 8 * K, F32, inner=T, parts=64)
            qT = av(61 * K, 4 * K, BF16, inner=T, parts=64)
            smx_ = [av(65 * K, 1 * K, F32), av(68 * K, 1 * K, F32)]
            pexp_ = [av(66 * K, 1 * K, F32), av(69 * K, 1 * K, F32)]
            pnb_ = [av(67 * K, 512, BF16), av(70 * K, 512, BF16)]
            pTt_ = [av(67 * K + 512, 512, BF16, inner=128), av(70 * K + 512, 512, BF16, inner=128)]
            cos4 = cosb[:, :].unsqueeze(1).to_broadcast([64, 4, T])
            sin4 = sinb[:, :].unsqueeze(1).to_broadcast([64, 4, T])

            def hg_head(hh):
                w = wload(("hg", hh)).rearrange("p (k c) -> p k c", c=512)
                pq = nb()
                for k in range(8):
                    P.mm(pq[:], w[:, k, 0:128], xn[:, k, :], start=(k == 0), stop=(k == 7))
                P.actf(qf, pq[:], AF.Silu)
                pf = nb()
                for k in range(8):
                    P.mm(pf[:], w[:, k, 128:256], xn[:, k, :], start=(k == 0), stop=(k == 7))
                P.actf(fb, pf[:], AF.Sigmoid)
                P.ts("dve", fb, fb, omlb[:, hh:hh + 1], ALU.mult, lb[:, hh:hh + 1], ALU.add)
                P.ts("dve", kk_, fb, -1.0, ALU.mult, 1.0, ALU.add)
                P.actf(e1, fb, AF.Ln)
                P.emit("dve", lambda e, o=bb, m=cc("scanmask"), d=e1: e.tensor_tensor_scan(o, m, d, 0.0, ALU.mult, ALU.add),
                       reads=[cc("scanmask"), e1], writes=[bb])
                b3 = bb.rearrange("p (c t) -> p c t", t=64)
                P.actf(e1, bb, AF.Exp)
                P.tt("dve", qs, qf, e1, ALU.mult)
                P.actf(e2, bb, AF.Exp, scale=-1.0)
                P.tt("dve", ks, kk_, e2, ALU.mult)
                P.actf(decb, b3[:, :, 63], AF.Exp)
                e13 = e1.rearrange("p (c t) -> p c t", t=64)
                P.tt("dve", e13, b3[:, :, 63:64].to_broadcast([128, 8, 64]), b3, ALU.subtract)
                P.actf(e1, e1, AF.Exp)
                P.tt("dve", kdT, kk_, e1, ALU.mult)
                for c in range(8):
                    pv = nb()
                    for k in range(8):
                        P.mm(pv[0:64, 0:128], xn[:, k, 64 * c:64 * c + 64], w[:, k, 256:384], start=(k == 0), stop=(k == 7))
                    P.copy("act", vtok[:, c, :], pv[0:64, 0:128])
                pgt = nb()
                for k in range(8):
                    P.mm(pgt[:], w[:, k, 384:512], xn[:, k, :], start=(k == 0), stop=(k == 7))
                P.actf(gs, pgt[:], AF.Silu)
                for c in range(8):
                    cs_ = slice(64 * c, 64 * c + 64)
                    r = c % 2
                    pt_ = nb()
                    ptb = pt_[:].bitcast(BF16)
                    P.tr(ptb[0:64, 0:128], kdT[:, cs_], identb[:])
                    P.copy("act", kdtok[:, r, :], ptb[0:64, 0:128])
                    pa = nb()
                    P.mm(pa[0:64, 0:64], ks[:, cs_], qs[:, cs_])
                    P.tt("dve", attb[:, r, :], pa[0:64, 0:64], maskU, ALU.mult)
                    po = nb()
                    P.mm(po[:, 0:64], vtok[:, c, :], attb[:, r, :], start=True, stop=False)
                    P.mm(po[:, 0:64], hgSb[:, hh, :], qs[:, cs_], start=False, stop=True)
                    P.copy("act", obuf[:, cs_], po[:, 0:64])
                    pu_ = nb()
                    P.mm(pu_[:, 0:128], kdtok[:, r, :], vtok[:, c, :])
                    P.stt("dve", hgS[:, hh, :], hgS[:, hh, :], decb[:, c:c + 1], pu_[:, 0:128], ALU.mult, ALU.add)
                    P.copy("act", hgSb[:, hh, :], hgS[:, hh, :])
                P.actf(sqb[:, 0, :], obuf, AF.Square)
                pn_ = nb()
                P.mm(pn_[:], onesb[:], sqb[:, 0, :])
                rstd_from_ss(pn_[:], e2, 1.0 / 128)
                P.stt("dve", obuf, obuf, cc("ogh"), e2, ALU.mult, ALU.mult)
                P.tt("dve", catA[:, hh, :], obuf, gs, ALU.mult)
            def swa_proj(g):
                w = wload(("swa", g)).rearrange("p (k c) -> p k c", c=384)
                for q4 in range(4):
                    pq = nb()
                    for k in range(8):
                        P.mm(pq[0:64, :], w[:, k, 64 * q4:64 * q4 + 64], xn[:, k, :], start=(k == 0), stop=(k == 7))
                    P.copy("act", qraw[:, q4, :], pq[0:64, :])
                pk = nb()
                for k in range(8):
                    P.mm(pk[0:64, :], w[:, k, 256:320], xn[:, k, :], start=(k == 0), stop=(k == 7))
                P.copy("act", kraw, pk[0:64, :])
                for b in range(4):
                    pv = nb()
                    for k in range(8):
                        P.mm(pv[:, 0:64], xn[:, k, 128 * b:128 * b + 128], w[:, k, 320:384], start=(k == 0), stop=(k == 7))
                    P.copy("act", vswa[:, 1 + b, 64 * g:64 * g + 64], pv[:, 0:64])
                P.tt("dve", ra[:], qraw[:], cos4, ALU.mult)
                P.tt("dve", rb[0:32], qraw[32:64], sin4[32:64], ALU.mult)
                P.tt("dve", rb[32:64], qraw[0:32], sin4[0:32], ALU.mult)
                P.tt("dve", qT[0:32], ra[0:32], rb[0:32], ALU.subtract)
                P.tt("dve", qT[32:64], ra[32:64], rb[32:64], ALU.add)
                ra1 = ra[:, 0, :]
                rb1 = rb[:, 0, :]
                P.tt("dve", ra1, kraw, cosb[:], ALU.mult)
                P.tt("dve", rb1[0:32], kraw[32:64], sinb[32:64], ALU.mult)
                P.tt("dve", rb1[32:64], kraw[0:32], sinb[0:32], ALU.mult)
                P.tt("dve", kT[0:32, g, 128:640], ra1[0:32], rb1[0:32], ALU.subtract)
                P.tt("dve", kT[32:64, g, 128:640], ra1[32:64], rb1[32:64], ALU.add)

            def swa_attn(g):
                its = [(b, q4) for b in range(4) for q4 in range(4)]

                def scores(i):
                    b, q4 = its[i]
                    mask = cc("swam0") if (t == 0 and b == 0) else cc("swam")
                    hq = 4 * g + q4
                    sink = cc("sinks", hq, 1)
                    par = i % 2
                    smx, pexp, pnb = smx_[par], pexp_[par], pnb_[par]
                    sc = smallc[:, 8 * par:8 * par + 8]
                    ps_ = nb()
                    P.mm(ps_[:, 0:256], qT[:, q4, 128 * b:128 * b + 128], kT[:, g, 128 * b:128 * b + 256])
                    P.stt("dve", smx, ps_[:, 0:256], 0.125, mask, ALU.mult, ALU.add)
                    P.emit("dve", lambda e, o=sc[:, 0:1], i_=smx: e.reduce_max(o, i_, AX.X),
                           reads=[smx], writes=[sc[:, 0:1]])
                    P.ts("dve", sc[:, 1:2], sc[:, 0:1], sink, ALU.max, -1.0, ALU.mult)
                    P.memset("dve", sc[:, 2:3], 0.0)
                    P.actf(pexp, smx, AF.Exp, bias=sc[:, 1:2], accum_out=sc[:, 2:3])
                    P.actf(sc[:, 3:4], sink, AF.Exp, bias=sc[:, 1:2])
                    P.tt("dve", sc[:, 4:5], sc[:, 2:3], sc[:, 3:4], ALU.add)
                    P.emit("dve", lambda e, o=sc[:, 5:6], i_=sc[:, 4:5]: e.reciprocal(o, i_),
                           reads=[sc[:, 4:5]], writes=[sc[:, 5:6]])
                    P.ts("dve", pnb, pexp, sc[:, 5:6], ALU.mult)

                def pv(i):
                    b, q4 = its[i]
                    hq = 4 * g + q4
                    par = i % 2
                    pnb, pTt = pnb_[par], pTt_[par]
                    pt_ = nb()
                    ptb = pt_[:].bitcast(BF16)
                    for j in range(2):
                        P.tr(ptb[:, 128 * j:128 * j + 128], pnb[:, 128 * j:128 * j + 128], identb[:])
                    P.copy("act", pTt.rearrange("p a b -> p (a b)"), ptb[:, 0:256])
                    po = nb()
                    for j in range(2):
                        P.mm(po[0:64, 0:128], vswa[:, b + j, 64 * g:64 * g + 64], pTt[:, j, :], start=(j == 0), stop=(j == 1))
                    P.copy("act", catB[:, hq, 128 * b:128 * b + 128], po[0:64, 0:128])

                scores(0)
                for i in range(len(its)):
                    if i + 1 < len(its):
                        scores(i + 1)
                    pv(i)
                P.copy("dve", kT[:, g, 0:128], kT[:, g, 512:640])

            swa_proj(0)
            hg_head(0)
            hg_head(1)
            swa_attn(0)
            swa_proj(1)
            hg_head(2)
            hg_head(3)
            swa_attn(1)
            P.copy("dve", vswa[:, 0, :], vswa[:, 4, :])
            for u in range(4):
                w = wload(("ewout", u))
                wa = w[:, 0:1024].rearrange("p (k c) -> p k c", c=256)
                wb_ = w[0:64, 1024:3072].rearrange("p (k c) -> p k c", c=256)
                for o2 in range(2):
                    o = 2 * u + o2
                    py = nb()
                    for hh in range(4):
                        P.mm(py[:], wa[:, hh, 128 * o2:128 * o2 + 128], catA[:, hh, :], start=(hh == 0), stop=False)
                    for hq in range(8):
                        P.mm(py[:], wb_[:, hq, 128 * o2:128 * o2 + 128], catB[:, hq, :], start=False, stop=(hq == 7))
                    evac_y(py[:], o)
            postnorm_res(l, 3, False)

        def odd_mixer(t):
            l = 1
            prenorm(l, 2)
            cat = av(0, 16 * K, BF16, inner=T)
            qraw = av(16 * K, 4 * K, F32, inner=T)
            kraw = av(20 * K, 4 * K, F32, inner=T)
            ta = av(24 * K, 4 * K, F32, inner=T)
            tb = av(28 * K, 4 * K, F32, inner=T)
            rr = av(32 * K, 4 * K, F32, inner=T)
            qr = av(36 * K, 2 * K, BF16, inner=T)
            qin = av(38 * K, 2 * K, BF16, inner=T)
            krb = av(40 * K, 2 * K, BF16, inner=T)
            kinT = av(42 * K, 2 * K, BF16, inner=T)
            ktok = av(44 * K, 2 * K, BF16, inner=256)
            vtok = av(46 * K, 4 * K, BF16, inner=512)
            gs = av(50 * K, 4 * K, BF16, inner=T)
            obuf = av(54 * K, 8 * K, F32, inner=T)
            attb = av(62 * K, 512, BF16, inner=128)
            e2 = av(63 * K, 2 * K, F32)
            cos2 = cosc[:, :].unsqueeze(1).to_broadcast([128, 2, T])
            sin2 = sinc[:, :].unsqueeze(1).to_broadcast([128, 2, T])

            def rope(raw):
                P.tt("dve", ta[:], raw[:], cos2, ALU.mult)
                P.tt("dve", tb[:], raw[:], sin2, ALU.mult)
                P.tt("dve", rr[:, 0, :], ta[:, 0, :], tb[:, 1, :], ALU.subtract)
                P.tt("dve", rr[:, 1, :], ta[:, 1, :], tb[:, 0, :], ALU.add)

            for hh in range(4):
                gq4 = cc("gq", 128 * hh, 128).unsqueeze(1).to_broadcast([128, 4, 128])
                gk4 = cc("gk", 128 * hh, 128).unsqueeze(1).to_broadcast([128, 4, 128])
                w = wload(("oq", hh)).rearrange("p (k c) -> p k c", c=256)
                for hf in range(2):
                    pq = nb()
                    for k in range(8):
                        P.mm(pq[:], w[:, k, 128 * hf:128 * hf + 128], xn[:, k, :], start=(k == 0), stop=(k == 7))
                    P.copy("act", qraw[:, hf, :], pq[:])
                w = wload(("ok", hh)).rearrange("p (k c) -> p k c", c=256)
                for hf in range(2):
                    pk = nb()
                    for k in range(8):
                        P.mm(pk[:], w[:, k, 128 * hf:128 * hf + 128], xn[:, k, :], start=(k == 0), stop=(k == 7))
                    P.copy("act", kraw[:, hf, :], pk[:])
                w = wload(("ov", hh)).rearrange("p (k c) -> p k c", c=512)
                for c in range(4):
                    pv = nb()
                    for k in range(8):
                        P.mm(pv[:], xn[:, k, 128 * c:128 * c + 128], w[:, k, :], start=(k == 0), stop=(k == 7))
                    P.copy("act", vtok[:, c, :], pv[:])
                w = wload(("og", hh)).rearrange("p (k c) -> p k c", c=512)
                for vc in range(4):
                    pg = nb()
                    for k in range(8):
                        P.mm(pg[:], w[:, k, 128 * vc:128 * vc + 128], xn[:, k, :], start=(k == 0), stop=(k == 7))
                    P.actf(gs[:, vc, :], pg[:], AF.Silu)
                rope(qraw)
                P.copy("act", qr[:], rr[:])
                for hf in range(2):
                    P.tt("dve", qin[:, hf, :].rearrange("p (c t) -> p c t", t=128),
                         rr[:, hf, :].rearrange("p (c t) -> p c t", t=128), gq4, ALU.mult)
                rope(kraw)
                P.ts("dve", rr[:], rr[:], 1.0 / 16.0, ALU.mult)
                P.copy("act", krb[:], rr[:])
                for hf in range(2):
                    P.tt("dve", kinT[:, hf, :].rearrange("p (c t) -> p c t", t=128),
                         rr[:, hf, :].rearrange("p (c t) -> p c t", t=128), gk4, ALU.mult)
                pt_ = nb()
                ptb = pt_[:].bitcast(BF16)
                for c in range(4):
                    for hf in range(2):
                        P.tr(ptb[:, 256 * c + 128 * hf:256 * c + 128 * hf + 128], kinT[:, hf, 128 * c:128 * c + 128], identb[:])
                P.copy("act", ktok.rearrange("p a b -> p (a b)"), ptb[:])
                dT = cc("decayT", 128 * hh, 128)
                for c in range(4):
                    cs_ = slice(128 * c, 128 * c + 128)
                    r = c % 2
                    pa = nb()
                    for hf in range(2):
                        P.mm(pa[:, 0:128], krb[:, hf, cs_], qr[:, hf, cs_], start=(hf == 0), stop=(hf == 1))
                    P.tt("dve", attb[:, r, :], pa[:, 0:128], dT, ALU.mult)
                    po = nb()
                    for vc in range(4):
                        vs = slice(128 * vc, 128 * vc + 128)
                        P.mm(po[:, vs], vtok[:, c, vs], attb[:, r, :], start=True, stop=False)
                        for hf in range(2):
                            P.mm(po[:, vs], retSb[:, 2 * hh + hf, vs], qin[:, hf, cs_], start=False, stop=(hf == 1))
                    P.copy("act", obuf[:, :, cs_], po[:].rearrange("p (a b) -> p a b", b=128))
                    for hf in range(2):
                        pu_ = nb()
                        P.mm(pu_[:], ktok[:, c, 128 * hf:128 * hf + 128], vtok[:, c, :])
                        P.stt("dve", retS[:, 2 * hh + hf, :], retS[:, 2 * hh + hf, :], GAMMA128[hh], pu_[:], ALU.mult, ALU.add)
                        P.copy("act", retSb[:, 2 * hh + hf, :], retS[:, 2 * hh + hf, :])
                pn_ = nb()
                for vc in range(4):
                    P.actf(sqb[:, vc % 2, :], obuf[:, vc, :], AF.Square)
                    P.mm(pn_[:], onesb[:], sqb[:, vc % 2, :], start=(vc == 0), stop=(vc == 3))
                rstd_from_ss(pn_[:], e2, 1.0 / 512)
                for vc in range(4):
                    P.stt("dve", obuf[:, vc, :], obuf[:, vc, :], cc("ogr", vc, 1), e2, ALU.mult, ALU.mult)
                    P.tt("dve", cat[:, 4 * hh + vc, :], obuf[:, vc, :], gs[:, vc, :], ALU.mult)
            for u in range(4):
                w = wload(("owout", u)).rearrange("p (k c) -> p k c", c=256)
                for o2 in range(2):
                    o = 2 * u + o2
                    py = nb()
                    for j in range(16):
                        P.mm(py[:], w[:, j, 128 * o2:128 * o2 + 128], cat[:, j, :], start=(j == 0), stop=(j == 15))
                    evac_y(py[:], o)
            postnorm_res(l, 3, False)

        for t in range(NT):
            P.epoch = t // EP_TILES
            load_x(t)
            if nstage == 0:
                trig_tables(t)
            stage_fns = [lambda: ffn(0, 1, mid_hook=lambda: trig_tables(t)), lambda: even_mixer(t), lambda: ffn(0, 2), lambda: ple(0, t),
                         lambda: ffn(1, 1), lambda: odd_mixer(t), lambda: ffn(1, 2), lambda: ple(1, t)]
            for f in stage_fns[:nstage]:
                f()
            store_out(t)
        stats = P.finalize(sems, blk)
    return nc, stats


_NC_CACHE = {}


def kernel(**inputs):
    NT = inputs["x"].shape[1] // T
    if NT not in _NC_CACHE:
        _NC_CACHE[NT] = build_nc(NT)[0]
    nc = _NC_CACHE[NT]
    wall = pack_weights(inputs)
    cst = pack_consts(inputs)
    x = np.ascontiguousarray(inputs["x"], dtype=np.float32)
    p = np.ascontiguousarray(inputs["p"], dtype=np.float32)
    pos = np.ascontiguousarray(inputs["positions"], dtype=np.int32)
    in_maps = []
    for c in range(8):
        in_maps.append({"x": x[c], "p": np.ascontiguousarray(p[:, c]), "pos": pos[c:c + 1],
                        "wall": wall, "cst": cst})
    res = run_bass_kernel_spmd(nc, in_maps, core_ids=list(range(8)))
    return np.stack([r["out"] for r in res.results], axis=0)
```

```python
from contextlib import ExitStack
import numpy as np
import concourse.bass as bass
import concourse.mybir as mybir
from concourse.bass_utils import run_bass_kernel_spmd

F32 = mybir.dt.float32
BF16 = mybir.dt.bfloat16
I32 = mybir.dt.int32
AF = mybir.ActivationFunctionType
ALU = mybir.AluOpType
AX = mybir.AxisListType

_DSZ = {F32: 4, BF16: 2, I32: 4}
COMPUTE = ("pe", "act", "dve", "pool")
SAME_ENGINE_SYNC = True


def _rng(ap):
    sz = _DSZ[ap.dtype]
    steps = ap.ap
    off = int(ap.offset)
    if str(ap.space) == "DRAM":
        ext = sum((c - 1) * s for s, c in steps)
        return (ap.name, off * sz, (off + ext + 1) * sz)
    if str(ap.space) == "PSUM":
        return (ap.name, 0, 2048)
    pstep = steps[0][0]
    lo = off % pstep if pstep > 0 else off
    ext = sum((c - 1) * s for s, c in steps[1:])
    return (ap.name, lo * sz, (lo + ext + 1) * sz)


class Op:
    __slots__ = ("eng", "fn", "deps", "sem", "cnt", "needs_inc", "inc_idx", "is_dma", "seq", "epoch")

    def __init__(self, eng, fn, is_dma=False):
        self.seq = 0
        self.epoch = 0
        self.eng = eng
        self.fn = fn
        self.deps = set()
        self.sem = None
        self.cnt = 0
        self.needs_inc = False
        self.inc_idx = 0
        self.is_dma = is_dma


class DmaSem:
    def __init__(self, handle):
        self.h = handle
        self.n = 0
        self.last = None


class Prog:
    def __init__(self, nc):
        self.nc = nc
        self.streams = {e: [] for e in ("pe", "act", "dve", "pool", "sp")}
        self.acc = {}
        self.dma_sems = []
        self.epoch = 0

    def _add(self, op):
        op.seq = len(self.streams[op.eng])
        op.epoch = self.epoch
        last = {}
        keep = set()
        for d in op.deps:
            if d.is_dma:
                keep.add(d)
            elif d.eng not in last or d.seq > last[d.eng].seq:
                last[d.eng] = d
        op.deps = keep | set(last.values())
        self.streams[op.eng].append(op)

    def new_dma_sem(self, stack, name):
        s = DmaSem(stack.enter_context(self.nc.semaphore(name)))
        self.dma_sems.append(s)
        return s

    def _track(self, op, reads, writes):
        writes = list(writes) + [ap for ap in reads if str(ap.space) == "PSUM"]
        reads = [ap for ap in reads if str(ap.space) != "PSUM"]
        for ap in reads:
            n, lo, hi = _rng(ap)
            a = self.acc.setdefault(n, ([], []))
            for (l, h, o) in a[0]:
                if l < hi and lo < h:
                    op.deps.add(o)
            a[1].append((lo, hi, op))
        for ap in writes:
            n, lo, hi = _rng(ap)
            a = self.acc.setdefault(n, ([], []))
            for (l, h, o) in a[0]:
                if l < hi and lo < h and o is not op:
                    op.deps.add(o)
            for (l, h, o) in a[1]:
                if l < hi and lo < h and o is not op:
                    op.deps.add(o)
            a[0][:] = [t for t in a[0] if not (lo <= t[0] and t[1] <= hi)]
            a[1][:] = [t for t in a[1] if not (lo <= t[0] and t[1] <= hi)]
            a[0].append((lo, hi, op))

    def emit(self, eng, fn, reads=(), writes=()):
        op = Op(eng, fn)
        self._track(op, reads, writes)
        self._add(op)
        return op

    def dma(self, queue, out, in_, sem):
        op = Op(queue, lambda e: e.dma_start(out=out, in_=in_), is_dma=True)
        if sem.last is not None:
            op.deps.add(sem.last)
        sem.n += 1
        sem.last = op
        op.sem = sem
        op.cnt = sem.n
        self._track(op, [in_], [out])
        self._add(op)
        return op

    def mm(self, out, lhsT, rhs, start=True, stop=True):
        return self.emit("pe", lambda e: e.matmul(out, lhsT, rhs, start=start, stop=stop),
                         reads=[lhsT, rhs], writes=[out])

    def tr(self, out, in_, ident):
        return self.emit("pe", lambda e: e.transpose(out, in_, ident), reads=[in_, ident], writes=[out])

    def actf(self, out, in_, func, bias=None, scale=None, accum_out=None):
        kw = {}
        rd = [in_]
        wr = [out]
        if bias is not None:
            kw["bias"] = bias
            if not isinstance(bias, (int, float)):
                rd.append(bias)
        if scale is not None:
            kw["scale"] = scale
            if not isinstance(scale, (int, float)):
                rd.append(scale)
        if accum_out is not None:
            kw["accum_out"] = accum_out
            wr.append(accum_out)
        return self.emit("act", lambda e: e.activation(out, in_, func, **kw), reads=rd, writes=wr)

    def tt(self, eng, out, in0, in1, op):
        return self.emit(eng, lambda e: e.tensor_tensor(out, in0, in1, op), reads=[in0, in1], writes=[out])

    def ts(self, eng, out, in0, s1, op0, s2=None, op1=None):
        rd = [in0] + [s for s in (s1, s2) if s is not None and not isinstance(s, (int, float))]
        kw = {}
        if op1 is not None:
            kw["op1"] = op1
        return self.emit(eng, lambda e: e.tensor_scalar(out, in0, s1, s2, op0, **kw), reads=rd, writes=[out])

    def stt(self, eng, out, in0, scalar, in1, op0, op1):
        rd = [in0, in1] + ([scalar] if not isinstance(scalar, (int, float)) else [])
        return self.emit(eng, lambda e: e.scalar_tensor_tensor(out, in0, scalar, in1, op0, op1),
                         reads=rd, writes=[out])

    def copy(self, eng, out, in_):
        if eng == "act":
            return self.emit("act", lambda e: e.copy(out, in_), reads=[in_], writes=[out])
        return self.emit(eng, lambda e: e.tensor_copy(out, in_), reads=[in_], writes=[out])

    def memset(self, eng, ap, val):
        return self.emit(eng, lambda e: e.memset(ap, val), writes=[ap])

    def finalize(self, sems, block):
        def needs_sem(op, d):
            if d.is_dma:
                return True
            if d.eng == op.eng and not op.is_dma:
                if d.eng == "pe" or not SAME_ENGINE_SYNC:
                    return False
            return True

        for st in self.streams.values():
            for op in st:
                for d in op.deps:
                    if not d.is_dma and needs_sem(op, d):
                        d.needs_inc = True
        self.max_inc = {}
        for eng in COMPUTE:
            k = {}
            for op in self.streams[eng]:
                if not op.is_dma and op.needs_inc:
                    k[op.epoch] = k.get(op.epoch, 0) + 1
                    op.inc_idx = k[op.epoch]
            self.max_inc[eng] = max(list(k.values()) + [0])
        stats = {}
        nc = self.nc
        engobj = {"pe": nc.tensor, "act": nc.scalar, "dve": nc.vector, "pool": nc.gpsimd, "sp": nc.sync}

        def run(eng):
            e = engobj[eng]
            waited = {}
            nw = 0
            for op in self.streams[eng]:
                need = {}
                for d in op.deps:
                    if not needs_sem(op, d):
                        continue
                    if d.is_dma:
                        key, v, h = ("d", id(d.sem)), d.cnt * 16, d.sem.h
                    else:
                        key, v, h = ("c", d.eng, d.epoch), d.inc_idx, sems[d.eng][d.epoch]
                    if v > need.get(key, (0, None))[0]:
                        need[key] = (v, h)
                for key, (v, h) in need.items():
                    if v > waited.get(key, 0):
                        e.wait_ge(h, v)
                        waited[key] = v
                        nw += 1
                ins = op.fn(e)
                if op.is_dma:
                    ins.then_inc(op.sem.h, 16)
                elif op.needs_inc:
                    ins.then_inc(sems[eng][op.epoch], 1)
            if eng == "sp":
                for s in self.dma_sems:
                    if s.n:
                        e.wait_ge(s.h, s.n * 16)
            stats[eng] = (len(self.streams[eng]), nw)

        block.tensor(lambda _e: run("pe"))
        block.scalar(lambda _e: run("act"))
        block.vector(lambda _e: run("dve"))
        block.gpsimd(lambda _e: run("pool"))
        block.sync(lambda _e: run("sp"))
        return stats


D = 1024
T = 512
DFF = 2816
EPS = 1e-6
NJ = DFF // 128
R_SLOTS = 4
EP_TILES = 2
WCOLS = 4096


class Cols:
    def __init__(self):
        self.n = 0
        self.d = {}

    def add(self, name, w):
        self.d[name] = (self.n, w)
        self.n += w


CST = Cols()
for _n, _w in (("gT", 112), ("lbT", 8), ("ogh", 1), ("ogr", 4), ("sinks", 8), ("invb", 1), ("invc", 1),
               ("ident", 128), ("maskU", 64), ("scanmask", 512), ("swam", 256), ("swam0", 256),
               ("decayT", 512), ("gq", 512), ("gk", 512)):
    CST.add(_n, _w)
NCST = CST.n


def unit_list():
    u = []
    for l in range(2):
        u += [(("ffn1_in", l, i), 4096) for i in range(11)]
        u += [(("ffn1_out", l, i), 2816) for i in range(8)]
        if l == 0:
            u += [(("hg", i), 4096) for i in range(4)]
            u += [(("swa", i), 3072) for i in range(2)]
            u += [(("ewout", i), 3072) for i in range(4)]
        else:
            for hh in range(4):
                u += [(("oq", hh), 2048), (("ok", hh), 2048), (("ov", hh), 4096), (("og", hh), 4096)]
            u += [(("owout", i), 4096) for i in range(4)]
        u += [(("ffn2_in", l, i), 4096) for i in range(11)]
        u += [(("ffn2_out", l, i), 2816) for i in range(8)]
        u += [(("pproj", l), 2048), (("pgate", l, 0), 4096), (("pgate", l, 1), 4096)]
    return u


UNITS = unit_list()
NU = len(UNITS)
UIDX = {k: i for i, (k, _) in enumerate(UNITS)}


def _kmaj(W, cols):
    kc = W.shape[0] // 128
    return np.ascontiguousarray(W.reshape(kc, 128, W.shape[1])[:, :, cols].transpose(1, 0, 2)).reshape(128, -1)


def pack_weights(inp):
    wall = np.zeros((NU, 128, WCOLS), np.float32)
    ar = np.arange
    for (key, n) in UNITS:
        i = UIDX[key]
        k0 = key[0]
        if k0 in ("ffn1_in", "ffn2_in"):
            W = inp[k0[:4] + "_w_in"][key[1]]
            u = key[2]
            cols = np.concatenate([ar(256 * u, 256 * u + 256), DFF + ar(256 * u, 256 * u + 256)])
            blk = _kmaj(W, cols)
        elif k0 in ("ffn1_out", "ffn2_out"):
            W = inp[k0[:4] + "_w_out"][key[1]]
            blk = _kmaj(W, ar(128 * key[2], 128 * key[2] + 128))
        elif k0 == "hg":
            W = inp["even_w_in"][0]
            hh = key[1]
            cols = np.concatenate([g * 512 + hh * 128 + ar(128) for g in range(4)])
            blk = _kmaj(W, cols)
        elif k0 == "swa":
            W = inp["even_w_in"][0]
            g = key[1]
            cols = np.concatenate([2048 + 256 * g + ar(256), 2560 + 64 * g + ar(64), 2688 + 64 * g + ar(64)])
            blk = _kmaj(W, cols)
        elif k0 == "ewout":
            W = inp["even_w_out"][0]
            cols = ar(256 * key[1], 256 * key[1] + 256)
            a = _kmaj(W[0:512], cols)
            b = np.zeros((128, 8, 256), np.float32)
            b[0:64] = W[512:1024].reshape(8, 64, 1024)[:, :, cols].transpose(1, 0, 2)
            blk = np.concatenate([a, b.reshape(128, -1)], axis=1)
        elif k0 in ("oq", "ok", "ov", "og"):
            W = inp["odd_w_in"][0]
            hh = key[1]
            base = {"oq": 0, "ok": 1024, "ov": 2048, "og": 4096}[k0]
            wd = 256 if k0 in ("oq", "ok") else 512
            blk = _kmaj(W, base + hh * wd + ar(wd))
        elif k0 == "owout":
            W = inp["odd_w_out"][0]
            blk = _kmaj(W, ar(256 * key[1], 256 * key[1] + 256))
        elif k0 == "pproj":
            blk = _kmaj(inp["ple_w_proj"][key[1]], ar(1024))
        elif k0 == "pgate":
            blk = _kmaj(inp["ple_w_gate"][key[1]], ar(512 * key[2], 512 * key[2] + 512))
        assert blk.shape == (128, n), (key, blk.shape, n)
        wall[i, :, :n] = blk
    return wall


def pack_consts(inp):
    c = np.zeros((128, NCST), np.float32)

    def put(name, arr):
        o, w = CST.d[name]
        arr = np.asarray(arr, np.float32)
        c[: arr.shape[0], o:o + w] = arr.reshape(arr.shape[0], w)

    put("gT", inp["norm_g"].reshape(2, 7, 8, 128).transpose(3, 0, 1, 2).reshape(128, 112))
    put("lbT", inp["hgrn_lb"].reshape(2, 4, 128).transpose(2, 0, 1).reshape(128, 8))
    put("ogh", inp["hgrn_onorm_g"].reshape(128, 1))
    put("ogr", inp["ret_onorm_g"].reshape(4, 128).T)
    put("sinks", np.broadcast_to(inp["attn_sinks"].reshape(1, 8), (128, 8)))
    inv_b = (np.float32(10000.0) ** (-np.arange(0, 64, 2, dtype=np.float32) / np.float32(64))).astype(np.float32)
    inv_c = (np.float32(10000.0) ** (-np.linspace(0.0, 1.0, 128, dtype=np.float32))).astype(np.float32)
    put("invb", np.concatenate([inv_b, inv_b]).reshape(64, 1))
    put("invc", inv_c.reshape(128, 1))
    put("ident", np.eye(128, dtype=np.float32))
    s = np.arange(64)
    put("maskU", (s[:, None] <= s[None, :]).astype(np.float32))
    sm = np.ones(512, np.float32)
    sm[::64] = 0.0
    put("scanmask", np.broadcast_to(sm, (128, 512)))
    qi = np.arange(128)[:, None]
    kj = np.arange(256)[None, :]
    diff = qi + 128 - kj
    valid = (diff >= 0) & (diff < 128)
    put("swam", np.where(valid, 0.0, -30000.0))
    put("swam0", np.where(valid & (kj >= 128), 0.0, -30000.0))
    hh = np.arange(4, dtype=np.float64)
    lg = np.log1p(-np.exp2(-5.0 - hh))
    idx = np.arange(128, dtype=np.float64)
    dcs = idx[None, :] - idx[:, None]
    dec = np.where(dcs[None] >= 0, np.exp(np.maximum(dcs, 0.0)[None] * lg[:, None, None]), 0.0)
    put("decayT", dec.transpose(1, 0, 2).reshape(128, 512))
    put("gq", np.broadcast_to(np.exp((idx + 1.0)[None, :] * lg[:, None]).reshape(1, 512), (128, 512)))
    put("gk", np.broadcast_to(np.exp((127.0 - idx)[None, :] * lg[:, None]).reshape(1, 512), (128, 512)))
    return c


GAMMA128 = [float(np.exp(128.0 * np.log1p(-np.exp2(-5.0 - h)))) for h in range(4)]


def build_nc(NT, nstage=8):
    S = NT * T
    nc = bass.Bass("TRN2", target_bir_lowering=False)
    x_d = nc.dram_tensor("x", [S, D], F32, kind="ExternalInput").ap()
    p_d = nc.dram_tensor("p", [2, S, 256], F32, kind="ExternalInput").ap()
    pos_d = nc.dram_tensor("pos", [1, S], I32, kind="ExternalInput").ap()
    wall_d = nc.dram_tensor("wall", [NU, 128, WCOLS], F32, kind="ExternalInput").ap()
    cst_d = nc.dram_tensor("cst", [128, NCST], F32, kind="ExternalInput").ap()
    out_d = nc.dram_tensor("out", [S, D], F32, kind="ExternalOutput").ap()
    wbf_d = nc.dram_tensor("wbf", [NU, 128, WCOLS], BF16, kind="Internal").ap()

    with ExitStack() as st:
        P = Prog(nc)
        sb = lambda n, s, d: st.enter_context(nc.sbuf_tensor(n, s, d))
        h = sb("h", [128, 8, T], F32)
        xn = sb("xn", [128, 8, T], BF16)
        ybuf = sb("ybuf", [128, 8, T], F32)
        sqb = sb("sqb", [128, 2, T], BF16)
        rstd = sb("rstd", [128, T], F32)
        wring = sb("wring", [128, R_SLOTS, WCOLS], BF16)
        ptile = sb("ptile", [128, 4, 256], F32)
        pTb = sb("pTb", [128, 2, T], BF16)
        cosb = sb("cosb", [64, T], F32)
        sinb = sb("sinb", [64, T], F32)
        cosc = sb("cosc", [128, T], F32)
        sinc = sb("sinc", [128, T], F32)
        retS = sb("retS", [128, 8, 512], F32)
        retSb = sb("retSb", [128, 8, 512], BF16)
        hgS = sb("hgS", [128, 4, 128], F32)
        hgSb = sb("hgSb", [128, 4, 128], BF16)
        kT = sb("kT", [64, 2, 640], BF16)
        vswa = sb("vswa", [128, 5, 128], BF16)
        csb = sb("csb", [128, NCST], F32)
        gsc = sb("gsc", [128, 112], F32)
        lbs = sb("lbs", [128, 12], F32)
        identb = sb("identb", [128, 128], BF16)
        onesb = sb("onesb", [128, 128], BF16)
        smallc = sb("smallc", [128, 16], F32)
        ARENA = 72 * 1024
        arena = sb("arena", [128, ARENA // 2], BF16)
        pb = [st.enter_context(nc.psum_tensor(f"pb{i}", [128, 512], F32)) for i in range(8)]
        NEP = (NT + EP_TILES - 1) // EP_TILES
        sems = {e: [st.enter_context(nc.semaphore(f"s_{e}{i}")) for i in range(NEP)] for e in COMPUTE}
        wsem_all = [[P.new_dma_sem(st, f"w{j}_{i}") for i in range(R_SLOTS)] for j in range(NEP)]
        csem = [P.new_dma_sem(st, f"c{i}") for i in range(8)]
        xsem = P.new_dma_sem(st, "xs")
        osem = P.new_dma_sem(st, "os")
        psem = P.new_dma_sem(st, "ps")
        qsem = P.new_dma_sem(st, "qs")
        ksem = P.new_dma_sem(st, "ks")
        blk = st.enter_context(nc.Block())

        def av(lo, nbytes, dt, inner=None, parts=128):
            a = arena[0:parts, lo // 2:(lo + nbytes) // 2]
            if dt != BF16:
                a = a.bitcast(dt)
            if inner is not None:
                a = a.rearrange("p (a b) -> p a b", b=inner)
            return a

        K = 1024

        def cc(name, lo=0, n=None, parts=128):
            o, w = CST.d[name]
            n = w - lo if n is None else n
            return csb[0:parts, o + lo:o + lo + n]

        identf = cc("ident")
        bank_ctr = [0]

        def nb():
            b = pb[bank_ctr[0] % 7]
            bank_ctr[0] += 1
            return b

        ss_bank = pb[7]

        wctr = [0]

        def wload(key):
            i = UIDX[key]
            n = UNITS[i][1]
            s = wctr[0] % R_SLOTS
            wctr[0] += 1
            P.dma("sp", wring[:, s, 0:n], wbf_d[i, :, 0:n], wsem_all[P.epoch][s])
            return wring[:, s, 0:n]

        P.dma("sp", csb[:], cst_d, ksem)
        for i, (key, n) in enumerate(UNITS):
            P.dma("pool", wbf_d[i, :, 0:n], wall_d[i, :, 0:n], csem[i % 8])
        P.copy("dve", identb[:], identf)
        P.memset("dve", onesb[:], 1.0)
        P.ts("dve", gsc[:], cc("gT"), 0.5, ALU.mult)
        P.tt("dve", lbs[:, 0:4], cc("lbT", 0, 4), cc("lbT", 4, 4), ALU.subtract)
        P.actf(lbs[:, 4:8], lbs[:, 0:4], AF.Sigmoid)
        P.ts("dve", lbs[:, 8:12], lbs[:, 4:8], -1.0, ALU.mult, 1.0, ALU.add)
        for tns in (retS, retSb, hgS, hgSb, vswa):
            P.memset("dve", tns[:], 0.0)
        P.memset("dve", kT[:], 0.0)

        def gcol(l, n, k, scaled=False):
            c = (l * 7 + n) * 8 + k
            return gsc[:, c:c + 1] if scaled else cc("gT", c, 1)

        def rstd_from_ss(ss_ap, out_ap, inv_n):
            P.actf(out_ap, ss_ap, AF.Ln, bias=EPS, scale=inv_n)
            P.actf(out_ap, out_ap, AF.Exp, scale=-0.5)

        h_ss_ready = [False]

        def prenorm(l, n):
            if not h_ss_ready[0]:
                for k in range(8):
                    P.actf(sqb[:, k % 2, :], h[:, k, :], AF.Square)
                    P.mm(ss_bank[:], onesb[:], sqb[:, k % 2, :], start=(k == 0), stop=(k == 7))
            h_ss_ready[0] = False
            rstd_from_ss(ss_bank[:], rstd[:], 1.0 / D)
            for k in range(8):
                P.stt("dve", xn[:, k, :], h[:, k, :], gcol(l, n, k), rstd[:], ALU.mult, ALU.mult)

        pending_ss = []

        def flush_ss():
            for o in pending_ss:
                P.mm(ss_bank[:], onesb[:], sqb[:, o % 2, :], start=(o == 0), stop=(o == 7))
            pending_ss.clear()

        def evac_y(py, o):
            flush_ss()
            P.copy("dve", ybuf[:, o, :], py)
            P.actf(sqb[:, o % 2, :], ybuf[:, o, :], AF.Square)
            pending_ss.append(o)

        def postnorm_res(l, n, half, next_norm=True):
            flush_ss()
            rstd_from_ss(ss_bank[:], rstd[:], 1.0 / D)
            for k in range(8):
                P.tt("dve" if k % 2 else "pool", ybuf[:, k, :], ybuf[:, k, :], rstd[:], ALU.mult)
                P.stt("dve", h[:, k, :], ybuf[:, k, :], gcol(l, n, k, scaled=half), h[:, k, :], ALU.mult, ALU.add)
                if next_norm:
                    P.actf(sqb[:, k % 2, :], h[:, k, :], AF.Square)
                    P.mm(ss_bank[:], onesb[:], sqb[:, k % 2, :], start=(k == 0), stop=(k == 7))
            h_ss_ready[0] = next_norm

        def ffn(l, which, mid_hook=None):
            n0 = 0 if which == 1 else 4
            hid = av(0, 22 * K, BF16, inner=T)
            sg = av(22 * K, 4 * K, F32, inner=T)
            prenorm(l, n0)
            for u in range(11):
                w = wload((f"ffn{which}_in", l, u)).rearrange("p (k c) -> p k c", c=512)
                for jj in range(2):
                    j = 2 * u + jj
                    pg = nb()
                    pu = nb()
                    for k in range(8):
                        P.mm(pg[:], w[:, k, 128 * jj:128 * jj + 128], xn[:, k, :], start=(k == 0), stop=(k == 7))
                    for k in range(8):
                        P.mm(pu[:], w[:, k, 256 + 128 * jj:256 + 128 * jj + 128], xn[:, k, :], start=(k == 0), stop=(k == 7))
                    P.actf(sg[:, j % 2, :], pg[:], AF.Silu)
                    P.tt("dve", hid[:, j, :], sg[:, j % 2, :], pu[:], ALU.mult)
            if mid_hook is not None:
                mid_hook()
            for o in range(8):
                w = wload((f"ffn{which}_out", l, o)).rearrange("p (k c) -> p k c", c=128)
                py = nb()
                for j in range(NJ):
                    P.mm(py[:], w[:, j, :], hid[:, j, :], start=(j == 0), stop=(j == NJ - 1))
                evac_y(py[:], o)
            postnorm_res(l, n0 + 1, True, next_norm=(which == 1))

        def ple(l, t):
            for k in range(8):
                P.copy("act" if k % 2 else "dve", xn[:, k, :], h[:, k, :])
            P.dma("sp", ptile[:], p_d[l, t * T:(t + 1) * T, :].rearrange("(b p) f -> p b f", p=128), psem)
            for c in range(2):
                bk = nb()
                for b in range(4):
                    P.tr(bk[:, 128 * b:128 * b + 128], ptile[:, b, 128 * c:128 * c + 128], identf)
                P.copy("act", pTb[:, c, :], bk[:])
            w = wload(("pproj", l)).rearrange("p (k c) -> p k c", c=1024)
            for o in range(8):
                bk = nb()
                for c in range(2):
                    P.mm(bk[:], w[:, c, 128 * o:128 * o + 128], pTb[:, c, :], start=(c == 0), stop=(c == 1))
                P.copy("dve", ybuf[:, o, :], bk[:])
            sg = av(22 * K, 4 * K, F32, inner=T)
            for u in range(2):
                w = wload(("pgate", l, u)).rearrange("p (k c) -> p k c", c=512)
                for o4 in range(4):
                    o = 4 * u + o4
                    bk = nb()
                    for k in range(8):
                        P.mm(bk[:], w[:, k, 128 * o4:128 * o4 + 128], xn[:, k, :], start=(k == 0), stop=(k == 7))
                    P.actf(sg[:, o % 2, :], bk[:], AF.Sigmoid)
                    P.tt("dve", ybuf[:, o, :], ybuf[:, o, :], sg[:, o % 2, :], ALU.mult)
                    flush_ss()
                    P.actf(sqb[:, o % 2, :], ybuf[:, o, :], AF.Square)
                    pending_ss.append(o)
            postnorm_res(l, 6, False, next_norm=(l == 0))

        xio = av(0, 16 * K, F32, inner=D)

        xin = ybuf[:].rearrange("p a b -> p (a b)").rearrange("p (a b) -> p a b", b=D)

        def load_x_dma(t):
            P.dma("sp", xin, x_d[t * T:(t + 1) * T, :].rearrange("(b p) f -> p b f", p=128), xsem)

        def load_x(t):
            for k in range(8):
                bk = nb()
                for b in range(4):
                    P.tr(bk[:, 128 * b:128 * b + 128], xin[:, b, 128 * k:128 * k + 128], identf)
                P.copy("act" if k % 2 else "dve", h[:, k, :], bk[:])

        def store_out(t):
            for b in range(4):
                for hf in range(2):
                    bk = nb()
                    for kk in range(4):
                        k = 4 * hf + kk
                        P.tr(bk[:, 128 * kk:128 * kk + 128], h[:, k, 128 * b:128 * b + 128], identf)
                    P.copy("act" if hf else "dve", xio[:, b, 512 * hf:512 * hf + 512], bk[:])
            P.dma("sp", out_d[t * T:(t + 1) * T, :].rearrange("(b p) f -> p b f", p=128), xio, osem)

        TWO_PI = float(2 * np.pi)
        MAGIC = 12582912.0
        C1 = 6.28125
        C2 = float(2 * np.pi - 6.28125)
        PI_LO = 3.1415925

        def trig_tables(t):
            posi = av(26 * K, 2 * K, I32)
            posf = av(28 * K, 2 * K, F32)
            ang = av(30 * K, 2 * K, F32)
            nn = av(32 * K, 2 * K, F32)
            r2 = av(34 * K, 2 * K, F32)
            P.dma("sp", posi, pos_d[:, t * T:(t + 1) * T].partition_broadcast(128), qsem)
            P.copy("dve", posf, posi)
            for (inv, parts, cs, sn) in ((cc("invb", parts=64), 64, cosb, sinb), (cc("invc"), 128, cosc, sinc)):
                a = ang[0:parts]
                n_ = nn[0:parts]
                r = r2[0:parts]
                P.ts("dve", a, posf[0:parts], inv, ALU.mult)
                P.ts("dve", n_, a, float(1.0 / TWO_PI), ALU.mult, MAGIC, ALU.add)
                P.ts("dve", n_, n_, -MAGIC, ALU.add)
                P.stt("dve", a, n_, -C1, a, ALU.mult, ALU.add)
                P.stt("dve", a, n_, -C2, a, ALU.mult, ALU.add)
                P.ts("dve", a, a, -PI_LO, ALU.max, PI_LO, ALU.min)
                P.actf(sn[:], a, AF.Sin)
                P.ts("dve", r, a, float(np.pi / 2), ALU.add)
                P.ts("dve", n_, r, PI_LO, ALU.is_gt, TWO_PI, ALU.mult)
                P.tt("dve", r, r, n_, ALU.subtract)
                P.ts("dve", r, r, -PI_LO, ALU.max, PI_LO, ALU.min)
                P.actf(cs[:], r, AF.Sin)

        def even_mixer(t):
            l = 0
            prenorm(l, 2)
            catA = av(0, 4 * K, BF16, inner=T)
            catB = av(4 * K, 8 * K, BF16, inner=T, parts=64)
            qf = av(12 * K, 2 * K, F32)
            fb = av(14 * K, 2 * K, F32)
            kk_ = av(16 * K, 2 * K, F32)
            bb = av(18 * K, 2 * K, F32)
            e1 = av(20 * K, 2 * K, F32)
            e2 = av(22 * K, 2 * K, F32)
            qs = av(24 * K, 1 * K, BF16)
            ks = av(25 * K, 1 * K, BF16)
            kdT = av(26 * K, 1 * K, BF16)
            vtok = av(27 * K, 2 * K, BF16, inner=128, parts=64)
            gs = av(29 * K, 2 * K, F32)
            obuf = av(31 * K, 2 * K, F32)
            decb = av(33 * K, 32, F32)
            kdtok = av(33 * K + 512, 512, BF16, inner=128, parts=64)
            attb = av(34 * K, 256, BF16, inner=64, parts=64)
            lb = lbs[:, 4:8]
            omlb = lbs[:, 8:12]
            maskU = cc("maskU", parts=64)

            qraw = av(35 * K, 8 * K, F32, inner=T, parts=64)
            kraw = av(43 * K, 2 * K, F32, parts=64)
            ra = av(45 * K, 8 * K, F32, inner=T, parts=64)
            rb = av(53 * K, 8 * K, F32, inner=T, parts=64)
            qT = av(61 * K, 4 * K, BF16, inner=T, parts=64)
            smx_ = [av(65 * K, 1 * K, F32), av(68 * K, 1 * K, F32)]
            pexp_ = [av(66 * K, 1 * K, F32), av(69 * K, 1 * K, F32)]
            pnb_ = [av(67 * K, 512, BF16), av(70 * K, 512, BF16)]
            pTt_ = [av(67 * K + 512, 512, BF16, inner=128), av(70 * K + 512, 512, BF16, inner=128)]
            cos4 = cosb[:, :].unsqueeze(1).to_broadcast([64, 4, T])
            sin4 = sinb[:, :].unsqueeze(1).to_broadcast([64, 4, T])

            def hg_head(hh):
                w = wload(("hg", hh)).rearrange("p (k c) -> p k c", c=512)
                pq = nb()
                for k in range(8):
                    P.mm(pq[:], w[:, k, 0:128], xn[:, k, :], start=(k == 0), stop=(k == 7))
                P.actf(qf, pq[:], AF.Silu)
                pf = nb()
                for k in range(8):
                    P.mm(pf[:], w[:, k, 128:256], xn[:, k, :], start=(k == 0), stop=(k == 7))
                P.actf(fb, pf[:], AF.Sigmoid)
                P.ts("dve", fb, fb, omlb[:, hh:hh + 1], ALU.mult, lb[:, hh:hh + 1], ALU.add)
                P.ts("dve", kk_, fb, -1.0, ALU.mult, 1.0, ALU.add)
                P.actf(e1, fb, AF.Ln)
                P.emit("dve", lambda e, o=bb, m=cc("scanmask"), d=e1: e.tensor_tensor_scan(o, m, d, 0.0, ALU.mult, ALU.add),
                       reads=[cc("scanmask"), e1], writes=[bb])
                b3 = bb.rearrange("p (c t) -> p c t", t=64)
                P.actf(e1, bb, AF.Exp)
                P.tt("dve", qs, qf, e1, ALU.mult)
                P.actf(e2, bb, AF.Exp, scale=-1.0)
                P.tt("dve", ks, kk_, e2, ALU.mult)
                P.actf(decb, b3[:, :, 63], AF.Exp)
                e13 = e1.rearrange("p (c t) -> p c t", t=64)
                P.tt("dve", e13, b3[:, :, 63:64].to_broadcast([128, 8, 64]), b3, ALU.subtract)
                P.actf(e1, e1, AF.Exp)
                P.tt("dve", kdT, kk_, e1, ALU.mult)
                for c in range(8):
                    pv = nb()
                    for k in range(8):
                        P.mm(pv[0:64, 0:128], xn[:, k, 64 * c:64 * c + 64], w[:, k, 256:384], start=(k == 0), stop=(k == 7))
                    P.copy("act", vtok[:, c, :], pv[0:64, 0:128])
                pgt = nb()
                for k in range(8):
                    P.mm(pgt[:], w[:, k, 384:512], xn[:, k, :], start=(k == 0), stop=(k == 7))
                P.actf(gs, pgt[:], AF.Silu)
                for c in range(8):
                    cs_ = slice(64 * c, 64 * c + 64)
                    r = c % 2
                    pt_ = nb()
                    ptb = pt_[:].bitcast(BF16)
                    P.tr(ptb[0:64, 0:128], kdT[:, cs_], identb[:])
                    P.copy("act", kdtok[:, r, :], ptb[0:64, 0:128])
                    pa = nb()
                    P.mm(pa[0:64, 0:64], ks[:, cs_], qs[:, cs_])
                    P.tt("dve", attb[:, r, :], pa[0:64, 0:64], maskU, ALU.mult)
                    po = nb()
                    P.mm(po[:, 0:64], vtok[:, c, :], attb[:, r, :], start=True, stop=False)
                    P.mm(po[:, 0:64], hgSb[:, hh, :], qs[:, cs_], start=False, stop=True)
                    P.copy("act", obuf[:, cs_], po[:, 0:64])
                    pu_ = nb()
                    P.mm(pu_[:, 0:128], kdtok[:, r, :], vtok[:, c, :])
                    P.stt("dve", hgS[:, hh, :], hgS[:, hh, :], decb[:, c:c + 1], pu_[:, 0:128], ALU.mult, ALU.add)
                    P.copy("act", hgSb[:, hh, :], hgS[:, hh, :])
                P.actf(sqb[:, 0, :], obuf, AF.Square)
                pn_ = nb()
                P.mm(pn_[:], onesb[:], sqb[:, 0, :])
                rstd_from_ss(pn_[:], e2, 1.0 / 128)
                P.stt("dve", obuf, obuf, cc("ogh"), e2, ALU.mult, ALU.mult)
                P.tt("dve", catA[:, hh, :], obuf, gs, ALU.mult)
            def swa_proj(g):
                w = wload(("swa", g)).rearrange("p (k c) -> p k c", c=384)
                for q4 in range(4):
                    pq = nb()
                    for k in range(8):
                        P.mm(pq[0:64, :], w[:, k, 64 * q4:64 * q4 + 64], xn[:, k, :], start=(k == 0), stop=(k == 7))
                    P.copy("act", qraw[:, q4, :], pq[0:64, :])
                pk = nb()
                for k in range(8):
                    P.mm(pk[0:64, :], w[:, k, 256:320], xn[:, k, :], start=(k == 0), stop=(k == 7))
                P.copy("act", kraw, pk[0:64, :])
                for b in range(4):
                    pv = nb()
                    for k in range(8):
                        P.mm(pv[:, 0:64], xn[:, k, 128 * b:128 * b + 128], w[:, k, 320:384], start=(k == 0), stop=(k == 7))
                    P.copy("act", vswa[:, 1 + b, 64 * g:64 * g + 64], pv[:, 0:64])
                P.tt("dve", ra[:], qraw[:], cos4, ALU.mult)
                P.tt("dve", rb[0:32], qraw[32:64], sin4[32:64], ALU.mult)
                P.tt("dve", rb[32:64], qraw[0:32], sin4[0:32], ALU.mult)
                P.tt("dve", qT[0:32], ra[0:32], rb[0:32], ALU.subtract)
                P.tt("dve", qT[32:64], ra[32:64], rb[32:64], ALU.add)
                ra1 = ra[:, 0, :]
                rb1 = rb[:, 0, :]
                P.tt("dve", ra1, kraw, cosb[:], ALU.mult)
                P.tt("dve", rb1[0:32], kraw[32:64], sinb[32:64], ALU.mult)
                P.tt("dve", rb1[32:64], kraw[0:32], sinb[0:32], ALU.mult)
                P.tt("dve", kT[0:32, g, 128:640], ra1[0:32], rb1[0:32], ALU.subtract)
                P.tt("dve", kT[32:64, g, 128:640], ra1[32:64], rb1[32:64], ALU.add)

            def swa_attn(g):
                its = [(b, q4) for b in range(4) for q4 in range(4)]

                def scores(i):
                    b, q4 = its[i]
                    mask = cc("swam0") if (t == 0 and b == 0) else cc("swam")
                    hq = 4 * g + q4
                    sink = cc("sinks", hq, 1)
                    par = i % 2
                    smx, pexp, pnb = smx_[par], pexp_[par], pnb_[par]
                    sc = smallc[:, 8 * par:8 * par + 8]
                    ps_ = nb()
                    P.mm(ps_[:, 0:256], qT[:, q4, 128 * b:128 * b + 128], kT[:, g, 128 * b:128 * b + 256])
                    P.stt("dve", smx, ps_[:, 0:256], 0.125, mask, ALU.mult, ALU.add)
                    P.emit("dve", lambda e, o=sc[:, 0:1], i_=smx: e.reduce_max(o, i_, AX.X),
                           reads=[smx], writes=[sc[:, 0:1]])
                    P.ts("dve", sc[:, 1:2], sc[:, 0:1], sink, ALU.max, -1.0, ALU.mult)
                    P.memset("dve", sc[:, 2:3], 0.0)
                    P.actf(pexp, smx, AF.Exp, bias=sc[:, 1:2], accum_out=sc[:, 2:3])
                    P.actf(sc[:, 3:4], sink, AF.Exp, bias=sc[:, 1:2])
                    P.tt("dve", sc[:, 4:5], sc[:, 2:3], sc[:, 3:4], ALU.add)
                    P.emit("dve", lambda e, o=sc[:, 5:6], i_=sc[:, 4:5]: e.reciprocal(o, i_),
                           reads=[sc[:, 4:5]], writes=[sc[:, 5:6]])
                    P.ts("dve", pnb, pexp, sc[:, 5:6], ALU.mult)

                def pv(i):
                    b, q4 = its[i]
                    hq = 4 * g + q4
                    par = i % 2
                    pnb, pTt = pnb_[par], pTt_[par]
                    pt_ = nb()
                    ptb = pt_[:].bitcast(BF16)
                    for j in range(2):
                        P.tr(ptb[:, 128 * j:128 * j + 128], pnb[:, 128 * j:128 * j + 128], identb[:])
                    P.copy("act", pTt.rearrange("p a b -> p (a b)"), ptb[:, 0:256])
                    po = nb()
                    for j in range(2):
                        P.mm(po[0:64, 0:128], vswa[:, b + j, 64 * g:64 * g + 64], pTt[:, j, :], start=(j == 0), stop=(j == 1))
                    P.copy("act", catB[:, hq, 128 * b:128 * b + 128], po[0:64, 0:128])

                scores(0)
                for i in range(len(its)):
                    if i + 1 < len(its):
                        scores(i + 1)
                    pv(i)
                P.copy("dve", kT[:, g, 0:128], kT[:, g, 512:640])

            swa_proj(0)
            hg_head(0)
            hg_head(1)
            swa_attn(0)
            swa_proj(1)
            hg_head(2)
            hg_head(3)
            swa_attn(1)
            P.copy("dve", vswa[:, 0, :], vswa[:, 4, :])
            for u in range(4):
                w = wload(("ewout", u))
                wa = w[:, 0:1024].rearrange("p (k c) -> p k c", c=256)
                wb_ = w[0:64, 1024:3072].rearrange("p (k c) -> p k c", c=256)
                for o2 in range(2):
                    o = 2 * u + o2
                    py = nb()
                    for hh in range(4):
                        P.mm(py[:], wa[:, hh, 128 * o2:128 * o2 + 128], catA[:, hh, :], start=(hh == 0), stop=False)
                    for hq in range(8):
                        P.mm(py[:], wb_[:, hq, 128 * o2:128 * o2 + 128], catB[:, hq, :], start=False, stop=(hq == 7))
                    evac_y(py[:], o)
            postnorm_res(l, 3, False)

        def odd_mixer(t):
            l = 1
            prenorm(l, 2)
            cat = av(0, 16 * K, BF16, inner=T)
            qraw = av(16 * K, 4 * K, F32, inner=T)
            kraw = av(20 * K, 4 * K, F32, inner=T)
            ta = av(24 * K, 4 * K, F32, inner=T)
            tb = av(28 * K, 4 * K, F32, inner=T)
            rr = av(32 * K, 4 * K, F32, inner=T)
            qr = av(36 * K, 2 * K, BF16, inner=T)
            qin = av(38 * K, 2 * K, BF16, inner=T)
            krb = av(40 * K, 2 * K, BF16, inner=T)
            kinT = av(42 * K, 2 * K, BF16, inner=T)
            ktok = av(44 * K, 2 * K, BF16, inner=256)
            vtok = av(46 * K, 4 * K, BF16, inner=512)
            gs = av(50 * K, 4 * K, BF16, inner=T)
            obuf = av(54 * K, 8 * K, F32, inner=T)
            attb = av(62 * K, 512, BF16, inner=128)
            e2 = av(63 * K, 2 * K, F32)
            cos2 = cosc[:, :].unsqueeze(1).to_broadcast([128, 2, T])
            sin2 = sinc[:, :].unsqueeze(1).to_broadcast([128, 2, T])

            def rope(raw):
                P.tt("dve", ta[:], raw[:], cos2, ALU.mult)
                P.tt("dve", tb[:], raw[:], sin2, ALU.mult)
                P.tt("dve", rr[:, 0, :], ta[:, 0, :], tb[:, 1, :], ALU.subtract)
                P.tt("dve", rr[:, 1, :], ta[:, 1, :], tb[:, 0, :], ALU.add)

            for hh in range(4):
                gq4 = cc("gq", 128 * hh, 128).unsqueeze(1).to_broadcast([128, 4, 128])
                gk4 = cc("gk", 128 * hh, 128).unsqueeze(1).to_broadcast([128, 4, 128])
                w = wload(("oq", hh)).rearrange("p (k c) -> p k c", c=256)
                for hf in range(2):
                    pq = nb()
                    for k in range(8):
                        P.mm(pq[:], w[:, k, 128 * hf:128 * hf + 128], xn[:, k, :], start=(k == 0), stop=(k == 7))
                    P.copy("act", qraw[:, hf, :], pq[:])
                w = wload(("ok", hh)).rearrange("p (k c) -> p k c", c=256)
                for hf in range(2):
                    pk = nb()
                    for k in range(8):
                        P.mm(pk[:], w[:, k, 128 * hf:128 * hf + 128], xn[:, k, :], start=(k == 0), stop=(k == 7))
                    P.copy("act", kraw[:, hf, :], pk[:])
                w = wload(("ov", hh)).rearrange("p (k c) -> p k c", c=512)
                for c in range(4):
                    pv = nb()
                    for k in range(8):
                        P.mm(pv[:], xn[:, k, 128 * c:128 * c + 128], w[:, k, :], start=(k == 0), stop=(k == 7))
                    P.copy("act", vtok[:, c, :], pv[:])
                w = wload(("og", hh)).rearrange("p (k c) -> p k c", c=512)
                for vc in range(4):
                    pg = nb()
                    for k in range(8):
                        P.mm(pg[:], w[:, k, 128 * vc:128 * vc + 128], xn[:, k, :], start=(k == 0), stop=(k == 7))
                    P.actf(gs[:, vc, :], pg[:], AF.Silu)
                rope(qraw)
                P.copy("act", qr[:], rr[:])
                for hf in range(2):
                    P.tt("dve", qin[:, hf, :].rearrange("p (c t) -> p c t", t=128),
                         rr[:, hf, :].rearrange("p (c t) -> p c t", t=128), gq4, ALU.mult)
                rope(kraw)
                P.ts("dve", rr[:], rr[:], 1.0 / 16.0, ALU.mult)
                P.copy("act", krb[:], rr[:])
                for hf in range(2):
                    P.tt("dve", kinT[:, hf, :].rearrange("p (c t) -> p c t", t=128),
                         rr[:, hf, :].rearrange("p (c t) -> p c t", t=128), gk4, ALU.mult)
                pt_ = nb()
                ptb = pt_[:].bitcast(BF16)
                for c in range(4):
                    for hf in range(2):
                        P.tr(ptb[:, 256 * c + 128 * hf:256 * c + 128 * hf + 128], kinT[:, hf, 128 * c:128 * c + 128], identb[:])
                P.copy("act", ktok.rearrange("p a b -> p (a b)"), ptb[:])
                dT = cc("decayT", 128 * hh, 128)
                for c in range(4):
                    cs_ = slice(128 * c, 128 * c + 128)
                    r = c % 2
                    pa = nb()
                    for hf in range(2):
                        P.mm(pa[:, 0:128], krb[:, hf, cs_], qr[:, hf, cs_], start=(hf == 0), stop=(hf == 1))
                    P.tt("dve", attb[:, r, :], pa[:, 0:128], dT, ALU.mult)
                    po = nb()
                    for vc in range(4):
                        vs = slice(128 * vc, 128 * vc + 128)
                        P.mm(po[:, vs], vtok[:, c, vs], attb[:, r, :], start=True, stop=False)
                        for hf in range(2):
                            P.mm(po[:, vs], retSb[:, 2 * hh + hf, vs], qin[:, hf, cs_], start=False, stop=(hf == 1))
                    P.copy("act", obuf[:, :, cs_], po[:].rearrange("p (a b) -> p a b", b=128))
                    for hf in range(2):
                        pu_ = nb()
                        P.mm(pu_[:], ktok[:, c, 128 * hf:128 * hf + 128], vtok[:, c, :])
                        P.stt("dve", retS[:, 2 * hh + hf, :], retS[:, 2 * hh + hf, :], GAMMA128[hh], pu_[:], ALU.mult, ALU.add)
                        P.copy("act", retSb[:, 2 * hh + hf, :], retS[:, 2 * hh + hf, :])
                pn_ = nb()
                for vc in range(4):
                    P.actf(sqb[:, vc % 2, :], obuf[:, vc, :], AF.Square)
                    P.mm(pn_[:], onesb[:], sqb[:, vc % 2, :], start=(vc == 0), stop=(vc == 3))
                rstd_from_ss(pn_[:], e2, 1.0 / 512)
                for vc in range(4):
                    P.stt("dve", obuf[:, vc, :], obuf[:, vc, :], cc("ogr", vc, 1), e2, ALU.mult, ALU.mult)
                    P.tt("dve", cat[:, 4 * hh + vc, :], obuf[:, vc, :], gs[:, vc, :], ALU.mult)
            for u in range(4):
                w = wload(("owout", u)).rearrange("p (k c) -> p k c", c=256)
                for o2 in range(2):
                    o = 2 * u + o2
                    py = nb()
                    for j in range(16):
                        P.mm(py[:], w[:, j, 128 * o2:128 * o2 + 128], cat[:, j, :], start=(j == 0), stop=(j == 15))
                    evac_y(py[:], o)
            postnorm_res(l, 3, False)

        load_x_dma(0)
        for t in range(NT):
            P.epoch = t // EP_TILES
            load_x(t)
            if nstage == 0:
                trig_tables(t)
            stage_fns = [lambda: ffn(0, 1, mid_hook=lambda: trig_tables(t)), lambda: even_mixer(t), lambda: ffn(0, 2), lambda: ple(0, t),
                         lambda: ffn(1, 1), lambda: odd_mixer(t), lambda: ffn(1, 2), lambda: ple(1, t)]
            for f in stage_fns[:nstage]:
                f()
            if t + 1 < NT:
                load_x_dma(t + 1)
            store_out(t)
        stats = P.finalize(sems, blk)
    return nc, stats


_NC_CACHE = {}


def kernel(**inputs):
    NT = inputs["x"].shape[1] // T
    if NT not in _NC_CACHE:
        _NC_CACHE[NT] = build_nc(NT)[0]
    nc = _NC_CACHE[NT]
    wall = pack_weights(inputs)
    cst = pack_consts(inputs)
    x = np.ascontiguousarray(inputs["x"], dtype=np.float32)
    p = np.ascontiguousarray(inputs["p"], dtype=np.float32)
    pos = np.ascontiguousarray(inputs["positions"], dtype=np.int32)
    in_maps = []
    for c in range(8):
        in_maps.append({"x": x[c], "p": np.ascontiguousarray(p[:, c]), "pos": pos[c:c + 1],
                        "wall": wall, "cst": cst})
    res = run_bass_kernel_spmd(nc, in_maps, core_ids=list(range(8)))
    return np.stack([r["out"] for r in res.results], axis=0)
```
